# Optimizing a Trainium2 kernel written in Bass

```python
import math
import jax, jax.numpy as jnp
from jax import lax
import numpy as np

D_MODEL = 1024
BATCH = 8
SEQ = 4096
DEPTH = 2

HEAD_DIM = 64
MOBA_HEADS = D_MODEL // 128
MOBA_WIDTH = MOBA_HEADS * HEAD_DIM
DIFF_HEADS = D_MODEL // 256
DIFF_QK_WIDTH = DIFF_HEADS * 2 * HEAD_DIM
DIFF_VDIM = 2 * HEAD_DIM
DIFF_WIDTH = DIFF_HEADS * DIFF_VDIM
MIX_WIDTH = MOBA_WIDTH + DIFF_WIDTH
IN_WIDTH = 4 * MOBA_WIDTH + 2 * DIFF_QK_WIDTH + 2 * DIFF_WIDTH
MOBA_BLOCK = 256
MOBA_TOPK = 3
MOBA_QCHUNK = 32
DIFF_QBLOCK = 128
NORM_EPS = 1e-6
NEG_INF = -1e30

kernel_name = "hymba_moba_diffattn_alibi_block"


def rms_norm(x, g):
    xf = x.astype(jnp.float32)
    y = xf * lax.rsqrt(jnp.mean(xf * xf, axis=-1, keepdims=True) + NORM_EPS)
    return (y * g.astype(jnp.float32)).astype(x.dtype)


def alibi_slopes(n_heads):
    return jnp.asarray(2.0 ** (-8.0 * np.arange(1, n_heads + 1) / n_heads), dtype=jnp.float32)


def moba_attention(q, k, v, slopes):
    B, H, S, dh = q.shape
    nb = S // MOBA_BLOCK
    kb = k.reshape(B, H, nb, MOBA_BLOCK, dh)
    vb = v.reshape(B, H, nb, MOBA_BLOCK, dh)
    scale = dh ** -0.5
    topk = min(MOBA_TOPK, nb - 1)
    q_blk = jnp.arange(S) // MOBA_BLOCK
    if topk > 0:
        k_mean = jnp.mean(kb.astype(jnp.float32), axis=3)
        gate = jnp.einsum('bhsd,bhnd->bhsn', q.astype(jnp.float32), k_mean)
        past = jnp.arange(nb)[None, :] < q_blk[:, None]
        gate = jnp.where(past[None, None], gate, NEG_INF)
        _, sel_idx = lax.top_k(gate, topk)
    bi = jnp.arange(B)[:, None, None, None]
    hi = jnp.arange(H)[None, :, None, None]
    slope4 = slopes[None, :, None, None]

    def chunk(c):
        t0 = c * MOBA_QCHUNK
        qc = lax.dynamic_slice_in_dim(q, t0, MOBA_QCHUNK, axis=2)
        tq = t0 + jnp.arange(MOBA_QCHUNK)
        own = t0 // MOBA_BLOCK
        k_own = lax.dynamic_index_in_dim(kb, own, axis=2, keepdims=False)
        v_own = lax.dynamic_index_in_dim(vb, own, axis=2, keepdims=False)
        s_own = own * MOBA_BLOCK + jnp.arange(MOBA_BLOCK)
        dist_own = (tq[:, None] - s_own[None, :]).astype(jnp.float32)
        sc_own = jnp.einsum('bhqd,bhkd->bhqk', qc, k_own).astype(jnp.float32) * scale - slope4 * dist_own
        sc_own = jnp.where((s_own[None, :] <= tq[:, None])[None, None], sc_own, NEG_INF)
        if topk == 0:
            p = jax.nn.softmax(sc_own, axis=-1)
            return jnp.einsum('bhqk,bhkd->bhqd', p.astype(v.dtype), v_own)
        ic = lax.dynamic_slice_in_dim(sel_idx, t0, MOBA_QCHUNK, axis=2)
        k_sel = kb[bi, hi, ic]
        v_sel = vb[bi, hi, ic]
        s_sel = ic[..., None] * MOBA_BLOCK + jnp.arange(MOBA_BLOCK)
        dist_sel = (tq[None, None, :, None, None] - s_sel).astype(jnp.float32)
        sc_sel = jnp.einsum('bhqd,bhqjkd->bhqjk', qc, k_sel).astype(jnp.float32) * scale
        sc_sel = sc_sel - slopes[None, :, None, None, None] * dist_sel
        sc_sel = jnp.where((ic < own)[..., None], sc_sel, NEG_INF)
        n_sel = topk * MOBA_BLOCK
        scores = jnp.concatenate([sc_sel.reshape(B, H, MOBA_QCHUNK, n_sel), sc_own], axis=-1)
        p = jax.nn.softmax(scores, axis=-1).astype(v.dtype)
        out = jnp.einsum('bhqk,bhqkd->bhqd', p[..., :n_sel],
                         v_sel.reshape(B, H, MOBA_QCHUNK, n_sel, dh))
        return out + jnp.einsum('bhqk,bhkd->bhqd', p[..., n_sel:], v_own)

    outs = lax.map(chunk, jnp.arange(S // MOBA_QCHUNK))
    return outs.transpose(1, 2, 0, 3, 4).reshape(B, H, S, dh)


def diff_attention(q, k, v, lam, slopes):
    B, H, S, _, dh = q.shape
    scale = dh ** -0.5
    sk = jnp.arange(S)
    slope5 = slopes[None, :, None, None, None]

    def block(c):
        t0 = c * DIFF_QBLOCK
        qc = lax.dynamic_slice_in_dim(q, t0, DIFF_QBLOCK, axis=2)
        tq = t0 + jnp.arange(DIFF_QBLOCK)
        sc = jnp.einsum('bhqcd,bhkcd->bhcqk', qc, k).astype(jnp.float32) * scale
        dist = (tq[:, None] - sk[None, :]).astype(jnp.float32)
        sc = jnp.where((sk[None, :] <= tq[:, None])[None, None, None], sc - slope5 * dist, NEG_INF)
        p = jax.nn.softmax(sc, axis=-1)
        a = p[:, :, 0] - lam * p[:, :, 1]
        return jnp.einsum('bhqk,bhkd->bhqd', a.astype(v.dtype), v)

    outs = lax.map(block, jnp.arange(S // DIFF_QBLOCK))
    return outs.transpose(1, 2, 0, 3, 4).reshape(B, H, S, v.shape[-1])


def setup_inputs(seed: int = 0) -> dict:
    key = jax.random.key(seed)
    ks = jax.random.split(key, 13)
    f32 = jnp.float32
    nrm = lambda k, shape: jax.random.normal(k, shape, dtype=f32)
    return {
        "x": nrm(ks[0], (BATCH, SEQ, D_MODEL)),
        "norm_g": 1.0 + 0.02 * nrm(ks[1], (DEPTH, D_MODEL)),
        "w_in": nrm(ks[2], (DEPTH, D_MODEL, IN_WIDTH)) * D_MODEL ** -0.5,
        "moba_q_norm": 1.0 + 0.02 * nrm(ks[3], (DEPTH, HEAD_DIM)),
        "moba_k_norm": 1.0 + 0.02 * nrm(ks[4], (DEPTH, HEAD_DIM)),
        "diff_q_norm": 1.0 + 0.02 * nrm(ks[5], (DEPTH, HEAD_DIM)),
        "diff_k_norm": 1.0 + 0.02 * nrm(ks[6], (DEPTH, HEAD_DIM)),
        "lambda_q1": 0.1 * nrm(ks[7], (DEPTH, HEAD_DIM)),
        "lambda_k1": 0.1 * nrm(ks[8], (DEPTH, HEAD_DIM)),
        "lambda_q2": 0.1 * nrm(ks[9], (DEPTH, HEAD_DIM)),
        "lambda_k2": 0.1 * nrm(ks[10], (DEPTH, HEAD_DIM)),
        "diff_subln": 1.0 + 0.02 * nrm(ks[11], (DEPTH, DIFF_VDIM)),
        "w_out": nrm(ks[12], (DEPTH, MIX_WIDTH, D_MODEL)) * MIX_WIDTH ** -0.5,
    }


def reference(x, norm_g, w_in, moba_q_norm, moba_k_norm, diff_q_norm, diff_k_norm,
              lambda_q1, lambda_k1, lambda_q2, lambda_k2, diff_subln, w_out):
    B, S, _ = x.shape
    s_pad = -(-S // MOBA_BLOCK) * MOBA_BLOCK
    moba_slopes = alibi_slopes(MOBA_HEADS)
    diff_slopes = alibi_slopes(DIFF_HEADS)
    split_at = list(np.cumsum([MOBA_WIDTH] * 4 + [DIFF_QK_WIDTH, DIFF_QK_WIDTH, DIFF_WIDTH]))
    for layer in range(DEPTH):
        h = rms_norm(x, norm_g[layer])
        proj = jnp.einsum('bsd,de->bse', h, w_in[layer])
        proj = jnp.pad(proj, ((0, 0), (0, s_pad - S), (0, 0)))
        mq, mk, mv, mg, dq, dk, dv, dg = jnp.split(proj, split_at, axis=-1)

        to_heads = lambda t: t.reshape(B, s_pad, MOBA_HEADS, HEAD_DIM).transpose(0, 2, 1, 3)
        mq = rms_norm(to_heads(mq), moba_q_norm[layer])
        mk = rms_norm(to_heads(mk), moba_k_norm[layer])
        m_out = moba_attention(mq, mk, to_heads(mv), moba_slopes)
        m_out = m_out.transpose(0, 2, 1, 3).reshape(B, s_pad, MOBA_WIDTH) * jax.nn.silu(mg)

        dq = rms_norm(dq.reshape(B, s_pad, DIFF_HEADS, 2, HEAD_DIM).transpose(0, 2, 1, 3, 4), diff_q_norm[layer])
        dk = rms_norm(dk.reshape(B, s_pad, DIFF_HEADS, 2, HEAD_DIM).transpose(0, 2, 1, 3, 4), diff_k_norm[layer])
        dv = dv.reshape(B, s_pad, DIFF_HEADS, DIFF_VDIM).transpose(0, 2, 1, 3)
        lambda_init = 0.8 - 0.6 * math.exp(-0.3 * layer)
        lam = (jnp.exp(jnp.sum(lambda_q1[layer] * lambda_k1[layer]))
               - jnp.exp(jnp.sum(lambda_q2[layer] * lambda_k2[layer])) + lambda_init)
        d_out = diff_attention(dq, dk, dv, lam, diff_slopes)
        d_out = rms_norm(d_out, diff_subln[layer]) * (1.0 - lambda_init)
        d_out = d_out.transpose(0, 2, 1, 3).reshape(B, s_pad, DIFF_WIDTH) * jax.nn.silu(dg)

        mixed = jnp.concatenate([m_out, d_out], axis=-1)[:, :S]
        x = x + jnp.einsum('bse,ed->bsd', mixed, w_out[layer])
    return x
```

```python
import math
import numpy as np
import concourse.bass as bass
import concourse.mybir as mybir
from concourse.bass_utils import run_bass_kernel_spmd

F32 = mybir.dt.float32
BF16 = mybir.dt.bfloat16
AF = mybir.ActivationFunctionType
ALU = mybir.AluOpType
AX = mybir.AxisListType

D = 1024
NCH = 8
EPS = 1e-6
NEGM = -30000.0
KR = 84
MOBA_SLOPES = [2.0 ** (-8.0 * i / 8) for i in range(1, 9)]
DIFF_SLOPES = [2.0 ** (-8.0 * i / 4) for i in range(1, 5)]
NDSEM = 8


class Prog:
    def __init__(self):
        self.ops = []
        self.last_w = {}
        self.readers = {}
        self.dma_hist = {"sp": [], "pool": []}

    def add(self, eng, fn, reads=(), writes=(), dma=False):
        idx = len(self.ops)
        raw = set()
        deps = set()
        for r in reads:
            if r in self.last_w:
                raw.add(self.last_w[r])
        for w in writes:
            if w in self.last_w:
                deps.add(self.last_w[w])
            for rd in self.readers.get(w, ()):
                deps.add(rd)
        deps |= raw
        keep = set()
        for j in deps:
            oj = self.ops[j]
            if oj["dma"]:
                keep.add(j)
            elif oj["eng"] != eng:
                keep.add(j)
            elif (j in raw) and eng != "pe" and not dma:
                keep.add(j)
            elif dma:
                keep.add(j)
        op = dict(eng=eng, fn=fn, dma=dma, deps=keep, sig=dma, sem=None, val=None)
        if dma:
            h = self.dma_hist[eng]
            n = len(h)
            if n >= NDSEM:
                op["deps"].add(h[n - NDSEM])
            op["slot"] = n % NDSEM
            op["val"] = 16 * (n // NDSEM + 1)
            h.append(idx)
        for w in writes:
            self.last_w[w] = idx
            self.readers[w] = []
        for r in reads:
            self.readers.setdefault(r, []).append(idx)
        self.ops.append(op)
        return idx

    def emit(self, nc, block, sems, dsems):
        ops = self.ops
        for op in ops:
            for j in op["deps"]:
                ops[j]["sig"] = True
        cnt = {e: 0 for e in sems}
        for op in ops:
            if op["dma"]:
                op["sem"] = dsems[op["eng"]][op["slot"]]
            elif op["sig"]:
                cnt[op["eng"]] += 1
                op["sem"] = sems[op["eng"]]
                op["val"] = cnt[op["eng"]]

        def run(engname, eng):
            waited = {}
            for op in ops:
                if op["eng"] != engname:
                    continue
                need = {}
                for j in op["deps"]:
                    oj = ops[j]
                    key = id(oj["sem"])
                    if waited.get(key, 0) >= oj["val"]:
                        continue
                    if key not in need or need[key][1] < oj["val"]:
                        need[key] = (oj["sem"], oj["val"])
                for key, (sem, val) in need.items():
                    eng.wait_ge(sem, val)
                    waited[key] = val
                if op["fn"] is None:
                    continue
                ins = op["fn"](eng)
                if op["dma"]:
                    ins.then_inc(op["sem"], 16)
                elif op["sig"]:
                    ins.then_inc(op["sem"], 1)

        @block.tensor
        def _(e):
            run("pe", e)

        @block.scalar
        def _(e):
            run("act", e)

        @block.vector
        def _(e):
            run("dve", e)

        @block.gpsimd
        def _(e):
            run("pool", e)

        @block.sync
        def _(e):
            run("sp", e)


def build(S, L, lambda_inits):
    NT = S // 128
    NG = S // 512
    NB = S // 256
    assert S % 512 == 0 and NT <= 32
    nc = bass.Bass("TRN2", target_bir_lowering=False)

    def dram(name, shape, dt, kind):
        return nc.dram_tensor(name, list(shape), dt, kind=kind).ap()

    x_d = dram("x", [S, D], F32, "ExternalInput")
    w_in_d = dram("w_in_r", [L, 8, 128, NCH * 512], F32, "ExternalInput")
    w_out_d = dram("w_out_r", [L, 128, NCH * D], F32, "ExternalInput")
    ng_d = dram("norm_g", [L, D], F32, "ExternalInput")
    gcol_d = dram("gcol", [L, 128, 4], F32, "ExternalInput")
    sub_d = dram("subln", [L, 128], F32, "ExternalInput")
    lamv_d = dram("lamv", [L, 256], F32, "ExternalInput")
    ident_d = dram("c_ident", [128, 128], F32, "ExternalInput")
    cm_d = dram("c_cmask", [128, 128], F32, "ExternalInput")
    bones_d = dram("c_bones", [128, 128], F32, "ExternalInput")
    kaug_d = dram("c_kaug", [20, S], F32, "ExternalInput")
    qaug_d = dram("c_qaug", [12, 4, S], F32, "ExternalInput")
    gb_d = dram("c_gb", [1, NT * 16], F32, "ExternalInput")
    out_d = dram("out", [S, D], F32, "ExternalOutput")
    x1_d = dram("x1_scratch", [S, D], F32, "Internal")
    mix_d = dram("mix_scratch", [S, D], BF16, "Internal")

    from contextlib import ExitStack
    es = ExitStack()

    def sb(name, shape, dt):
        return es.enter_context(nc.sbuf_tensor(name, list(shape), dt))

    def pst(name, shape, dt):
        return es.enter_context(nc.psum_tensor(name, list(shape), dt))

    with es:
        hT = sb("hT", [128, NCH, S], BF16)
        xt = [sb(f"xt{i}", [128, D], F32) for i in range(2)]
        hb = [sb(f"hb{i}", [128, D], BF16) for i in range(2)]
        junk = sb("junk", [128, D], BF16)
        gbc = sb("gbc", [128, D], F32)
        wbf = sb("wbf", [128, NCH, 512], BF16)
        wout = sb("wout", [128, NCH, D], BF16)
        QT = [sb(f"QT{i}", [128, S], BF16) for i in range(2)]
        KT = [sb(f"KT{i}", [128, S], BF16) for i in range(2)]
        Vt = sb("Vt", [128, NT, 130], BF16)
        Gt = sb("Gt", [128, NT, 128], BF16)
        gtmp = [sb(f"gtmp{i}", [128, 128], F32) for i in range(2)]
        sq = [sb(f"sq{i}", [128, 512], BF16) for i in range(2)]
        rs = [sb(f"rs{i}", [128, 512], F32) for i in range(2)]
        NPT = 3
        PT = [sb(f"PT{i}", [128, 512], BF16) for i in range(NPT)]
        gm = sb("gm", [128, NT, 16], F32)
        top8 = sb("top8", [128, NT, 8], F32)
        thr = sb("thr", [128, NT], F32)
        sel = sb("sel", [128, NT, 16], F32)
        selb = sb("selb", [128, NT, 16], BF16)
        km = sb("km", [64, 16], F32)
        kmb = sb("kmb", [64, 16], BF16)
        mo = [sb(f"mo{i}", [128, 4, 128], BF16) for i in range(2)]
        abuf = sb("abuf", [128, 4, 128], F32)
        dbuf = [sb(f"dbuf{i}", [128, 128], F32) for i in range(2)]
        small = sb("small", [128, 64], F32)
        mt = [sb(f"mt{i}", [128, D], BF16) for i in range(2)]
        mT = [sb(f"mT{i}", [128, NCH, 128], BF16) for i in range(2)]
        ident = sb("ident", [128, 128], BF16)
        cmask = sb("cmask", [128, 128], BF16)
        bones = sb("bones", [128, 128], BF16)
        gbt = sb("gbt", [128, NT * 16], F32)
        gcol = sb("gcol_s", [128, 4], F32)
        qcol = sb("qcol_s", [128, 4], F32)
        subbc = sb("subbc", [128, 128], F32)
        lamv = sb("lamv_s", [128, 256], F32)
        lamt = sb("lamt", [128, 8], F32)

        ps = [pst(f"ps{i}", [128, 512], F32) for i in range(7)]
        tp = pst("tp", [128, 1024], BF16)

        sems = {e: es.enter_context(nc.semaphore(f"sem_{e}")) for e in ["pe", "act", "dve", "pool", "sp"]}
        dsems = {q: [es.enter_context(nc.semaphore(f"dsem_{q}{i}")) for i in range(NDSEM)] for q in ["sp", "pool"]}
        block = es.enter_context(nc.Block())

        P = Prog()
        cnt = {"s": 0, "pt": 0, "small": 0, "pj": 0}

        def psr(k):
            return ("ps", k)

        def scol():
            c = cnt["small"] % 64
            cnt["small"] += 1
            return c

        P.add("pool", lambda e: e.dma_start(out=ident[:], in_=ident_d[:, :]), writes=["ident"], dma=True)
        P.add("pool", lambda e: e.dma_start(out=cmask[:], in_=cm_d[:, :]), writes=["cmask"], dma=True)
        P.add("pool", lambda e: e.dma_start(out=bones[:], in_=bones_d[:, :]), writes=["bones"], dma=True)
        P.add("sp", lambda e: e.dma_start(out=gbt[:], in_=gb_d[:, :].to_broadcast([128, NT * 16])), writes=["gbt"], dma=True)
        for i in range(2):
            P.add("pool", lambda e, i=i: e.dma_start(out=KT[i][64:84, :], in_=kaug_d[:, :]),
                  writes=[("KTaug", i)], dma=True)
        P.add("dve", lambda e: e.memset(kmb[:], 0.0), writes=["kmb"])

        def layer(l):
            src = x_d if l == 0 else x1_d
            dst = out_d if l == L - 1 else x1_d
            srcn = "xd" if l == 0 else "x1d"
            dstn = "outd" if l == L - 1 else "x1d"
            li = lambda_inits[l]

            P.add("sp", lambda e: e.dma_start(out=gbc[:], in_=ng_d[l:l + 1, :].to_broadcast([128, D])),
                  writes=["gbc"], dma=True)
            P.add("sp", lambda e: e.dma_start(out=gcol[:], in_=gcol_d[l, :, :]), writes=["gcol"], dma=True)
            P.add("sp", lambda e: e.dma_start(out=subbc[:], in_=sub_d[l:l + 1, :].to_broadcast([128, 128])),
                  writes=["subbc"], dma=True)
            P.add("sp", lambda e: e.dma_start(out=lamv[:], in_=lamv_d[l:l + 1, :].to_broadcast([128, 256])),
                  writes=["lamv"], dma=True)
            P.add("pool", lambda e: e.dma_start(out=wout[:].rearrange("p c n -> p (c n)"), in_=w_out_d[l, :, :]),
                  writes=["wout"], dma=True)
            P.add("dve", lambda e: e.tensor_scalar(out=qcol[:], in0=gcol[:], scalar1=0.125, scalar2=None, op0=ALU.mult),
                  reads=["gcol"], writes=["qcol"])
            P.add("dve", lambda e: e.tensor_scalar(out=subbc[:], in0=subbc[:], scalar1=float(1.0 - li), scalar2=None,
                                                   op0=ALU.mult), reads=["subbc"], writes=["subbc"])
            P.add("dve", lambda e: e.tensor_tensor(out=lamv[:, 0:64], in0=lamv[:, 0:64], in1=lamv[:, 64:128], op=ALU.mult),
                  reads=["lamv"], writes=["lamv"])
            P.add("dve", lambda e: e.tensor_tensor(out=lamv[:, 128:192], in0=lamv[:, 128:192], in1=lamv[:, 192:256],
                                                   op=ALU.mult), reads=["lamv"], writes=["lamv"])
            P.add("dve", lambda e: e.tensor_reduce(out=lamt[:, 0:1], in_=lamv[:, 0:64], axis=AX.X, op=ALU.add),
                  reads=["lamv"], writes=["lamt"])
            P.add("dve", lambda e: e.tensor_reduce(out=lamt[:, 1:2], in_=lamv[:, 128:192], axis=AX.X, op=ALU.add),
                  reads=["lamv", "lamt"], writes=["lamt"])
            P.add("act", lambda e: e.activation(out=lamt[:, 2:4], in_=lamt[:, 0:2], func=AF.Exp),
                  reads=["lamt"], writes=["lamt"])
            P.add("dve", lambda e: e.scalar_tensor_tensor(out=lamt[:, 4:5], in0=lamt[:, 3:4], scalar=float(-li),
                                                          in1=lamt[:, 2:3], op0=ALU.add, op1=ALU.subtract),
                  reads=["lamt"], writes=["neglam"])

            for t in range(NT):
                b = t % 2
                P.add("sp", lambda e, t=t, b=b: e.dma_start(out=xt[b][:], in_=src[t * 128:(t + 1) * 128, :]),
                      reads=[(srcn, t)], writes=[("xt", b)], dma=True)
                c0 = scol()
                P.add("act", lambda e, b=b, c0=c0: e.activation(out=junk[:], in_=xt[b][:], func=AF.Square,
                                                               accum_out=small[:, c0:c0 + 1]),
                      reads=[("xt", b)], writes=["junk", ("small", c0)])
                P.add("act", lambda e, c0=c0: e.activation(out=small[:, c0:c0 + 1], in_=small[:, c0:c0 + 1], func=AF.Ln,
                                                          scale=1.0 / D, bias=EPS),
                      reads=[("small", c0)], writes=[("small", c0)])
                P.add("act", lambda e, c0=c0: e.activation(out=small[:, c0:c0 + 1], in_=small[:, c0:c0 + 1], func=AF.Exp,
                                                          scale=-0.5),
                      reads=[("small", c0)], writes=[("small", c0)])
                P.add("dve", lambda e, b=b, c0=c0: e.scalar_tensor_tensor(out=hb[b][:], in0=xt[b][:],
                                                                         scalar=small[:, c0:c0 + 1], in1=gbc[:],
                                                                         op0=ALU.mult, op1=ALU.mult),
                      reads=[("xt", b), ("small", c0), "gbc"], writes=[("hb", b)])
                for c in range(NCH):
                    P.add("pe", lambda e, b=b, c=c: e.transpose(tp[:, c * 128:(c + 1) * 128], hb[b][:, c * 128:(c + 1) * 128],
                                                                ident[:]),
                          reads=[("hb", b), "ident"], writes=["tp"])
                P.add("act", lambda e, t=t: e.copy(out=hT[:, :, t * 128:(t + 1) * 128],
                                                  in_=tp[:, :].rearrange("p (c n) -> p c n", n=128)),
                      writes=["tp", ("hT", t // 4)])

            for u in range(8):
                unit(l, u, li)

            for t in range(NT):
                b = t % 2
                P.add("sp", lambda e, t=t, b=b: e.dma_start(out=mt[b][:], in_=mix_d[t * 128:(t + 1) * 128, :]),
                      reads=[("mixd", t // 4)], writes=[("mt", b)], dma=True)
                P.add("sp", lambda e, t=t, b=b: e.dma_start(out=xt[b][:], in_=src[t * 128:(t + 1) * 128, :]),
                      reads=[(srcn, t)], writes=[("xt", b)], dma=True)
                for c in range(NCH):
                    P.add("pe", lambda e, b=b, c=c: e.transpose(tp[:, c * 128:(c + 1) * 128], mt[b][:, c * 128:(c + 1) * 128],
                                                                ident[:]),
                          reads=[("mt", b), "ident"], writes=["tp"])
                P.add("act", lambda e, b=b: e.copy(out=mT[b][:, :, :], in_=tp[:, :].rearrange("p (c n) -> p c n", n=128)),
                      writes=["tp", ("mT", b)])
                for half in range(2):
                    bank = 6 if half == 0 else 0
                    for c in range(NCH):
                        P.add("pe", lambda e, b=b, c=c, half=half, bank=bank: e.matmul(
                            ps[bank][:, :], lhsT=mT[b][:, c, :], rhs=wout[:, c, half * 512:(half + 1) * 512],
                            start=(c == 0), stop=(c == NCH - 1)),
                            reads=[("mT", b), "wout"], writes=[psr(bank)])
                    P.add("dve", lambda e, b=b, half=half, bank=bank: e.tensor_tensor(
                        out=xt[b][:, half * 512:(half + 1) * 512], in0=xt[b][:, half * 512:(half + 1) * 512],
                        in1=ps[bank][:, :], op=ALU.add),
                        reads=[("xt", b)], writes=[psr(bank), ("xt", b)])
                P.add("sp", lambda e, t=t, b=b: e.dma_start(out=dst[t * 128:(t + 1) * 128, :], in_=xt[b][:]),
                      reads=[("xt", b)], writes=[(dstn, t)], dma=True)

        def unit(l, u, li):
            moba = u < 4
            dvw = 65 if moba else 129
            heads = [2 * u, 2 * u + 1] if moba else [8 + (u - 4), 8 + (u - 4)]
            P.add("pool", lambda e: e.dma_start(out=wbf[:].rearrange("p c n -> p (c n)"), in_=w_in_d[l, u, :, :]),
                  writes=["wbf"], dma=True)
            for i in range(2):
                P.add("pool", lambda e, i=i: e.dma_start(out=QT[i][80:84, :], in_=qaug_d[heads[i], :, :]),
                      writes=[("QTaug", i)], dma=True)
            if moba:
                P.add("dve", lambda e: e.memset(Vt[:, :, 64:65], 1.0), writes=["Vt"])
                P.add("dve", lambda e: e.memset(Vt[:, :, 129:130], 1.0), writes=["Vt"])
            else:
                P.add("dve", lambda e: e.memset(Vt[:, :, 128:129], 1.0), writes=["Vt"])
                if u == 4:
                    for i in range(2):
                        P.add("dve", lambda e, i=i: e.memset(QT[i][64:80, :], 0.0), writes=[("QTsel", i)])

            for which in range(2):
                T = QT if which == 0 else KT
                tname = "QT" if which == 0 else "KT"
                colt = qcol if which == 0 else gcol
                cidx = (0 if moba else 2) + which
                for g in range(NG):
                    pj = 6 if (cnt["pj"] % 2 == 0) else 1
                    cnt["pj"] += 1
                    b = g % 2
                    for c in range(NCH):
                        P.add("pe", lambda e, c=c, g=g, pj=pj, which=which: e.matmul(
                            ps[pj][:, :], lhsT=wbf[:, c, which * 128:(which + 1) * 128],
                            rhs=hT[:, c, g * 512:(g + 1) * 512], start=(c == 0), stop=(c == NCH - 1)),
                            reads=["wbf", ("hT", g)], writes=[psr(pj)])
                    P.add("act", lambda e, b=b, pj=pj: e.activation(out=sq[b][:], in_=ps[pj][:, :], func=AF.Square),
                          writes=[psr(pj), ("sq", b)])
                    P.add("pe", lambda e, b=b: e.matmul(ps[0][:, :], lhsT=bones[:], rhs=sq[b][:], start=True, stop=True),
                          reads=["bones", ("sq", b)], writes=[psr(0)])
                    P.add("act", lambda e, b=b: e.activation(out=rs[b][:], in_=ps[0][:, :], func=AF.Ln, scale=1.0 / 64,
                                                            bias=EPS),
                          writes=[psr(0), ("rs", b)])
                    P.add("act", lambda e, b=b: e.activation(out=rs[b][:], in_=rs[b][:], func=AF.Exp, scale=-0.5),
                          reads=[("rs", b)], writes=[("rs", b)])
                    for i in range(2):
                        P.add("dve", lambda e, i=i, b=b, g=g, pj=pj, T=T, colt=colt, cidx=cidx: e.scalar_tensor_tensor(
                            out=T[i][0:64, g * 512:(g + 1) * 512], in0=ps[pj][i * 64:(i + 1) * 64, :],
                            scalar=colt[i * 64:(i + 1) * 64, cidx:cidx + 1], in1=rs[b][i * 64:(i + 1) * 64, :],
                            op0=ALU.mult, op1=ALU.mult),
                            reads=[("rs", b), "qcol", "gcol"], writes=[psr(pj), (tname, i, g)])

            for t in range(NT):
                bank = 2 + (t % 2)
                b = t % 2
                for c in range(NCH):
                    P.add("pe", lambda e, c=c, t=t, bank=bank: e.matmul(
                        ps[bank][:, 0:256], lhsT=hT[:, c, t * 128:(t + 1) * 128], rhs=wbf[:, c, 256:512],
                        start=(c == 0), stop=(c == NCH - 1)),
                        reads=["wbf", ("hT", t // 4)], writes=[psr(bank)])
                if moba:
                    P.add("dve", lambda e, t=t, bank=bank: e.tensor_copy(
                        out=Vt[:, t, :].rearrange("p (i c) -> p i c", c=65)[:, :, 0:64],
                        in_=ps[bank][:, 0:128].rearrange("p (i c) -> p i c", c=64)),
                        writes=[psr(bank), "Vt"])
                    P.add("act", lambda e, t=t, bank=bank: e.activation(out=Gt[:, t, :], in_=ps[bank][:, 128:256], func=AF.Silu),
                          writes=[psr(bank), "Gt"])
                else:
                    P.add("dve", lambda e, t=t, bank=bank: e.tensor_copy(out=Vt[:, t, 0:128], in_=ps[bank][:, 0:128]),
                          writes=[psr(bank), "Vt"])
                    P.add("act", lambda e, b=b, bank=bank: e.activation(out=gtmp[b][:], in_=ps[bank][:, 128:256], func=AF.Silu),
                          writes=[psr(bank), ("gtmp", b)])
                    P.add("dve", lambda e, t=t, b=b: e.tensor_tensor(out=Gt[:, t, :], in0=gtmp[b][:], in1=subbc[:], op=ALU.mult),
                          reads=[("gtmp", b), "subbc"], writes=["Gt"])

            if moba:
                for i in range(2):
                    P.add("dve", lambda e, i=i: e.tensor_reduce(
                        out=km[:, 0:NB], in_=KT[i][0:64, :].rearrange("p (n s) -> p n s", s=256), axis=AX.X, op=ALU.add),
                        reads=[("KT", i, g) for g in range(NG)], writes=["km"])
                    P.add("dve", lambda e: e.tensor_scalar(out=kmb[:, 0:NB], in0=km[:, 0:NB], scalar1=1.0 / 256, scalar2=None,
                                                           op0=ALU.mult), reads=["km"], writes=["kmb"])
                    for t in range(NT):
                        P.add("pe", lambda e, i=i, t=t: e.matmul(ps[6][:, t * 16:(t + 1) * 16],
                                                                 lhsT=QT[i][0:64, t * 128:(t + 1) * 128], rhs=kmb[:, :],
                                                                 start=True, stop=True),
                              reads=[("QT", i, t // 4), "kmb"], writes=[psr(6)])
                    P.add("dve", lambda e: e.tensor_tensor(out=gm[:].rearrange("p t n -> p (t n)"), in0=ps[6][:, 0:NT * 16],
                                                           in1=gbt[:], op=ALU.add),
                          reads=["gbt"], writes=[psr(6), "gm"])
                    for t in range(NT):
                        P.add("dve", lambda e, t=t: e.max(out=top8[:, t, :], in_=gm[:, t, :]), reads=["gm"], writes=["top8"])
                    P.add("dve", lambda e: e.tensor_scalar(out=thr[:], in0=top8[:, :, 3], scalar1=-1e29, scalar2=None,
                                                           op0=ALU.max), reads=["top8"], writes=["thr"])
                    P.add("dve", lambda e: e.tensor_tensor(out=sel[:], in0=gm[:], in1=thr[:].unsqueeze(2).to_broadcast([128, NT, 16]),
                                                           op=ALU.is_ge), reads=["gm", "thr"], writes=["sel"])
                    P.add("dve", lambda e: e.tensor_scalar(out=selb[:], in0=sel[:], scalar1=-NEGM, scalar2=NEGM,
                                                           op0=ALU.mult, op1=ALU.add), reads=["sel"], writes=["selb"])
                    for t0 in range(0, NT, 8):
                        for t in range(t0, t0 + 8):
                            P.add("pe", lambda e, t=t, t0=t0: e.transpose(tp[0:16, (t - t0) * 128:(t - t0 + 1) * 128],
                                                                          selb[:, t, :], ident[:]),
                                  reads=["selb", "ident"], writes=["tp"])
                        P.add("act", lambda e, i=i, t0=t0: e.copy(out=QT[i][64:80, t0 * 128:(t0 + 8) * 128], in_=tp[0:16, :]),
                              writes=["tp", ("QTsel", i)])

            for g in range(NG):
                for i in range(2):
                    oset = (g * 2 + i) % 2
                    ob = [2 + 2 * oset, 3 + 2 * oset]
                    nkt = 4 * (g + 1)
                    for kt in range(nkt):
                        j = kt - 4 * g
                        s0 = max(0, j)
                        sbk = cnt["s"] % 2
                        cnt["s"] += 1
                        slot = cnt["pt"] % NPT
                        cnt["pt"] += 1
                        P.add("pe", lambda e, i=i, kt=kt, g=g, s0=s0, sbk=sbk, j=j: e.matmul(
                            ps[sbk][:, s0 * 128:512], lhsT=KT[i][0:KR, kt * 128:(kt + 1) * 128],
                            rhs=QT[i][0:KR, g * 512 + s0 * 128:(g + 1) * 512], start=True, stop=(j < 0)),
                            reads=[("KT", i, kt // 4), ("KTaug", i), ("QT", i, g), ("QTaug", i), ("QTsel", i)],
                            writes=[psr(sbk)])
                        if j >= 0:
                            P.add("pe", lambda e, sbk=sbk, j=j: e.matmul(ps[sbk][:, j * 128:(j + 1) * 128], lhsT=ident[:],
                                                                         rhs=cmask[:], start=False, stop=True),
                                  reads=["ident", "cmask"], writes=[psr(sbk)])
                        P.add("act", lambda e, sbk=sbk, slot=slot, s0=s0: e.activation(
                            out=PT[slot][:, s0 * 128:512], in_=ps[sbk][:, s0 * 128:512], func=AF.Exp),
                            writes=[psr(sbk), ("PT", slot)])
                        for s in range(s0, 4):
                            bank = ob[s // 2]
                            col = (s % 2) * dvw
                            vlo = i * 65 if moba else 0
                            P.add("pe", lambda e, slot=slot, s=s, bank=bank, col=col, kt=kt, vlo=vlo, g=g: e.matmul(
                                ps[bank][:, col:col + dvw], lhsT=PT[slot][:, s * 128:(s + 1) * 128],
                                rhs=Vt[:, kt, vlo:vlo + dvw], start=(kt == 0 and s % 2 == 0), stop=(kt == 4 * g + s),
                                skip_group_check=True),
                                reads=[("PT", slot), "Vt"], writes=[psr(bank)])
                    epilogue(u, g, i, ob, dvw, moba)

        def epilogue(u, g, i, ob, dvw, moba):
            mb = g % 2
            for s in range(4):
                t = 4 * g + s
                bank = ob[s // 2]
                col = (s % 2) * dvw
                c0 = scol()
                if moba:
                    P.add("dve", lambda e, bank=bank, col=col, c0=c0: e.reciprocal(out=small[:, c0:c0 + 1],
                                                                                   in_=ps[bank][:, col + 64:col + 65]),
                          writes=[psr(bank), ("small", c0)])
                    P.add("dve", lambda e, bank=bank, col=col, c0=c0, s=s, t=t, mb=mb, i=i: e.scalar_tensor_tensor(
                        out=mo[mb][:, s, i * 64:(i + 1) * 64], in0=ps[bank][:, col:col + 64], scalar=small[:, c0:c0 + 1],
                        in1=Gt[:, t, i * 64:(i + 1) * 64], op0=ALU.mult, op1=ALU.mult),
                        reads=[("small", c0), "Gt"], writes=[psr(bank), ("mo", mb)])
                elif i == 0:
                    P.add("dve", lambda e, bank=bank, col=col, c0=c0: e.reciprocal(out=small[:, c0:c0 + 1],
                                                                                   in_=ps[bank][:, col + 128:col + 129]),
                          writes=[psr(bank), ("small", c0)])
                    P.add("dve", lambda e, bank=bank, col=col, c0=c0, s=s: e.tensor_scalar(
                        out=abuf[:, s, :], in0=ps[bank][:, col:col + 128], scalar1=small[:, c0:c0 + 1], scalar2=None,
                        op0=ALU.mult),
                        reads=[("small", c0)], writes=[psr(bank), ("abuf", s)])
                else:
                    c1 = scol()
                    db = s % 2
                    P.add("dve", lambda e, bank=bank, col=col, c0=c0: e.reciprocal(out=small[:, c0:c0 + 1],
                                                                                   in_=ps[bank][:, col + 128:col + 129]),
                          writes=[psr(bank), ("small", c0)])
                    P.add("dve", lambda e, c0=c0: e.tensor_tensor(out=small[:, c0:c0 + 1], in0=small[:, c0:c0 + 1],
                                                                 in1=lamt[:, 4:5], op=ALU.mult),
                          reads=[("small", c0), "neglam"], writes=[("small", c0)])
                    P.add("dve", lambda e, bank=bank, col=col, c0=c0, s=s, db=db: e.scalar_tensor_tensor(
                        out=dbuf[db][:], in0=ps[bank][:, col:col + 128], scalar=small[:, c0:c0 + 1], in1=abuf[:, s, :],
                        op0=ALU.mult, op1=ALU.add),
                        reads=[("small", c0), ("abuf", s)], writes=[psr(bank), ("dbuf", db)])
                    P.add("act", lambda e, db=db, c1=c1: e.activation(out=junk[:, 0:128], in_=dbuf[db][:], func=AF.Square,
                                                                     accum_out=small[:, c1:c1 + 1]),
                          reads=[("dbuf", db)], writes=["junk", ("small", c1)])
                    P.add("act", lambda e, c1=c1: e.activation(out=small[:, c1:c1 + 1], in_=small[:, c1:c1 + 1], func=AF.Ln,
                                                              scale=1.0 / 128, bias=EPS),
                          reads=[("small", c1)], writes=[("small", c1)])
                    P.add("act", lambda e, c1=c1: e.activation(out=small[:, c1:c1 + 1], in_=small[:, c1:c1 + 1], func=AF.Exp,
                                                              scale=-0.5),
                          reads=[("small", c1)], writes=[("small", c1)])
                    P.add("dve", lambda e, db=db, c1=c1, s=s, t=t, mb=mb: e.scalar_tensor_tensor(
                        out=mo[mb][:, s, :], in0=dbuf[db][:], scalar=small[:, c1:c1 + 1], in1=Gt[:, t, :],
                        op0=ALU.mult, op1=ALU.mult),
                        reads=[("dbuf", db), ("small", c1), "Gt"], writes=[("mo", mb)])
            if i == 1:
                P.add("sp", lambda e, g=g, mb=mb, u=u: e.dma_start(
                    out=mix_d[g * 512:(g + 1) * 512, u * 128:(u + 1) * 128].rearrange("(s p) c -> p s c", p=128),
                    in_=mo[mb][:, :, :]),
                    reads=[("mo", mb)], writes=[("mixd", g)], dma=True)

        for l in range(L):
            layer(l)
        P.add("sp", None, reads=[("outd", t) for t in range(NT)])
        P.emit(nc, block, sems, dsems)
    return nc


def _host_layout(S, inputs):
    w_in = np.asarray(inputs["w_in"], dtype=np.float32)
    w_out = np.asarray(inputs["w_out"], dtype=np.float32)
    L = w_in.shape[0]
    NT = S // 128
    w_in_r = np.empty((L, 8, 128, NCH, 512), np.float32)
    for l in range(L):
        wl = w_in[l].reshape(NCH, 128, 4096)
        for u in range(8):
            base = 0 if u < 4 else 2048
            idx = u % 4
            for k in range(4):
                c0 = base + k * 512 + idx * 128
                w_in_r[l, u, :, :, k * 128:(k + 1) * 128] = wl[:, :, c0:c0 + 128].transpose(1, 0, 2)
    w_in_r = np.ascontiguousarray(w_in_r.reshape(L, 8, 128, NCH * 512))
    w_out_r = np.ascontiguousarray(w_out.reshape(L, NCH, 128, D).transpose(0, 2, 1, 3).reshape(L, 128, NCH * D))
    gcol = np.stack([np.stack([np.tile(np.asarray(inputs[k], np.float32)[l], 2) for k in
                               ("moba_q_norm", "moba_k_norm", "diff_q_norm", "diff_k_norm")], axis=1)
                     for l in range(L)], axis=0).astype(np.float32)
    lamv = np.concatenate([np.asarray(inputs[k], np.float32) for k in
                           ("lambda_q1", "lambda_k1", "lambda_q2", "lambda_k2")], axis=1)
    pos = np.arange(S)
    ident = np.eye(128, dtype=np.float32)
    kk = np.arange(128)[:, None]
    qq = np.arange(128)[None, :]
    cmask = np.where(kk <= qq, 0.0, NEGM).astype(np.float32)
    bones = np.kron(np.eye(2, dtype=np.float32), np.ones((64, 64), np.float32))
    kaug = np.zeros((20, S), np.float32)
    kaug[0:16] = (pos[None, :] // 256 == np.arange(16)[:, None]).astype(np.float32)
    kaug[16] = pos // 128
    kaug[17] = pos % 128
    kaug[18] = 1.0
    kaug[19] = 1.0
    qaug = np.zeros((12, 4, S), np.float32)
    for h, sl in enumerate(MOBA_SLOPES + DIFF_SLOPES):
        qaug[h, 0] = sl * 128.0
        qaug[h, 1] = sl
        qaug[h, 2] = -sl * 128.0 * (pos // 128)
        qaug[h, 3] = -sl * (pos % 128)
    gb = np.zeros((NT, 16), np.float32)
    for t in range(NT):
        own = t // 2
        gb[t, own] = 1e30
        gb[t, own + 1:] = -1e30
    common = {
        "w_in_r": w_in_r, "w_out_r": w_out_r,
        "norm_g": np.ascontiguousarray(np.asarray(inputs["norm_g"], np.float32)),
        "gcol": np.ascontiguousarray(gcol),
        "subln": np.ascontiguousarray(np.asarray(inputs["diff_subln"], np.float32)),
        "lamv": np.ascontiguousarray(lamv),
        "c_ident": ident, "c_cmask": cmask, "c_bones": bones, "c_kaug": kaug, "c_qaug": qaug,
        "c_gb": gb.reshape(1, NT * 16),
    }
    return L, common


def kernel(x, norm_g, w_in, moba_q_norm, moba_k_norm, diff_q_norm, diff_k_norm,
           lambda_q1, lambda_k1, lambda_q2, lambda_k2, diff_subln, w_out):
    inputs = dict(x=x, norm_g=norm_g, w_in=w_in, moba_q_norm=moba_q_norm, moba_k_norm=moba_k_norm,
                  diff_q_norm=diff_q_norm, diff_k_norm=diff_k_norm, lambda_q1=lambda_q1, lambda_k1=lambda_k1,
                  lambda_q2=lambda_q2, lambda_k2=lambda_k2, diff_subln=diff_subln, w_out=w_out)
    x = np.asarray(x, dtype=np.float32)
    B, S, _ = x.shape
    L, common = _host_layout(S, inputs)
    lambda_inits = [0.8 - 0.6 * math.exp(-0.3 * l) for l in range(L)]
    nc = build(S, L, lambda_inits)
    in_maps = [dict(common, x=np.ascontiguousarray(x[b])) for b in range(B)]
    res = run_bass_kernel_spmd(nc, in_maps, core_ids=list(range(B)))
    return np.stack([np.asarray(r["out"], dtype=np.float32) for r in res.results], axis=0)
```

```python
import math
import numpy as np
import concourse.bass as bass
import concourse.mybir as mybir
from concourse.bass_utils import run_bass_kernel_spmd

F32 = mybir.dt.float32
BF16 = mybir.dt.bfloat16
AF = mybir.ActivationFunctionType
ALU = mybir.AluOpType
AX = mybir.AxisListType

D = 1024
NCH = 8
EPS = 1e-6
NEGM = -30000.0
KR = 84
MOBA_SLOPES = [2.0 ** (-8.0 * i / 8) for i in range(1, 9)]
DIFF_SLOPES = [2.0 ** (-8.0 * i / 4) for i in range(1, 5)]
NDSEM = 8
ALIBI_FAR = 100.0


class Prog:
    def __init__(self):
        self.ops = []
        self.last_w = {}
        self.readers = {}
        self.dma_hist = {"sp": [], "pool": []}

    def add(self, eng, fn, reads=(), writes=(), dma=False):
        idx = len(self.ops)
        raw = set()
        deps = set()
        for r in reads:
            if r in self.last_w:
                raw.add(self.last_w[r])
        for w in writes:
            if w in self.last_w:
                deps.add(self.last_w[w])
            for rd in self.readers.get(w, ()):
                deps.add(rd)
        deps |= raw
        keep = set()
        for j in deps:
            oj = self.ops[j]
            if oj["dma"]:
                keep.add(j)
            elif oj["eng"] != eng:
                keep.add(j)
            elif (j in raw) and eng != "pe" and not dma:
                keep.add(j)
            elif dma:
                keep.add(j)
        op = dict(eng=eng, fn=fn, dma=dma, deps=keep, sig=dma, sem=None, val=None)
        if dma:
            h = self.dma_hist[eng]
            n = len(h)
            if n >= NDSEM:
                op["deps"].add(h[n - NDSEM])
            op["slot"] = n % NDSEM
            op["val"] = 16 * (n // NDSEM + 1)
            h.append(idx)
        for w in writes:
            self.last_w[w] = idx
            self.readers[w] = []
        for r in reads:
            self.readers.setdefault(r, []).append(idx)
        self.ops.append(op)
        return idx

    def emit(self, nc, block, sems, dsems):
        ops = self.ops
        for op in ops:
            for j in op["deps"]:
                ops[j]["sig"] = True
        cnt = {e: 0 for e in sems}
        for op in ops:
            if op["dma"]:
                op["sem"] = dsems[op["eng"]][op["slot"]]
            elif op["sig"]:
                cnt[op["eng"]] += 1
                op["sem"] = sems[op["eng"]]
                op["val"] = cnt[op["eng"]]

        def run(engname, eng):
            waited = {}
            for op in ops:
                if op["eng"] != engname:
                    continue
                need = {}
                for j in op["deps"]:
                    oj = ops[j]
                    key = id(oj["sem"])
                    if waited.get(key, 0) >= oj["val"]:
                        continue
                    if key not in need or need[key][1] < oj["val"]:
                        need[key] = (oj["sem"], oj["val"])
                for key, (sem, val) in need.items():
                    eng.wait_ge(sem, val)
                    waited[key] = val
                if op["fn"] is None:
                    continue
                ins = op["fn"](eng)
                if op["dma"]:
                    ins.then_inc(op["sem"], 16)
                elif op["sig"]:
                    ins.then_inc(op["sem"], 1)

        @block.tensor
        def _(e):
            run("pe", e)

        @block.scalar
        def _(e):
            run("act", e)

        @block.vector
        def _(e):
            run("dve", e)

        @block.gpsimd
        def _(e):
            run("pool", e)

        @block.sync
        def _(e):
            run("sp", e)


def build(S, L, lambda_inits):
    NT = S // 128
    NG = S // 512
    NB = S // 256
    assert S % 512 == 0 and NT <= 32
    nc = bass.Bass("TRN2", target_bir_lowering=False)

    def dram(name, shape, dt, kind):
        return nc.dram_tensor(name, list(shape), dt, kind=kind).ap()

    x_d = dram("x", [S, D], F32, "ExternalInput")
    w_in_d = dram("w_in_r", [L, 8, 128, NCH * 512], F32, "ExternalInput")
    w_out_d = dram("w_out_r", [L, 128, NCH * D], F32, "ExternalInput")
    ng_d = dram("norm_g", [L, D], F32, "ExternalInput")
    gcol_d = dram("gcol", [L, 128, 4], F32, "ExternalInput")
    sub_d = dram("subln", [L, 128], F32, "ExternalInput")
    lamv_d = dram("lamv", [L, 256], F32, "ExternalInput")
    ident_d = dram("c_ident", [128, 128], F32, "ExternalInput")
    cm_d = dram("c_cmask", [128, 128], F32, "ExternalInput")
    bones_d = dram("c_bones", [128, 128], F32, "ExternalInput")
    kaug_d = dram("c_kaug", [20, S], F32, "ExternalInput")
    qaug_d = dram("c_qaug", [12, 4, S], F32, "ExternalInput")
    gb_d = dram("c_gb", [1, NT * 16], F32, "ExternalInput")
    out_d = dram("out", [S, D], F32, "ExternalOutput")
    x1_d = dram("x1_scratch", [S, D], F32, "Internal")
    mix_d = dram("mix_scratch", [S, D], BF16, "Internal")

    from contextlib import ExitStack
    es = ExitStack()

    def sb(name, shape, dt):
        return es.enter_context(nc.sbuf_tensor(name, list(shape), dt))

    def pst(name, shape, dt):
        return es.enter_context(nc.psum_tensor(name, list(shape), dt))

    with es:
        hT = sb("hT", [128, NCH, S], BF16)
        xt = [sb(f"xt{i}", [128, D], F32) for i in range(2)]
        hb = [sb(f"hb{i}", [128, D], BF16) for i in range(2)]
        junk = sb("junk", [128, D], BF16)
        gbc = sb("gbc", [128, D], F32)
        wbf = sb("wbf", [128, NCH, 512], BF16)
        wout = sb("wout", [128, NCH, D], BF16)
        QT = [sb(f"QT{i}", [128, S], BF16) for i in range(2)]
        KT = [sb(f"KT{i}", [128, S], BF16) for i in range(2)]
        Vt = sb("Vt", [128, NT, 130], BF16)
        Gt = sb("Gt", [128, NT, 128], BF16)
        gtmp = [sb(f"gtmp{i}", [128, 128], F32) for i in range(2)]
        sq = [sb(f"sq{i}", [128, 512], BF16) for i in range(2)]
        rs = [sb(f"rs{i}", [128, 512], F32) for i in range(2)]
        NPT = 4
        PT = [sb(f"PT{i}", [128, 512], BF16) for i in range(NPT)]
        gm = sb("gm", [128, NT, 16], F32)
        top8 = sb("top8", [128, NT, 8], F32)
        thr = sb("thr", [128, NT], F32)
        sel = sb("sel", [128, NT, 16], F32)
        selb = sb("selb", [128, NT, 16], BF16)
        km = sb("km", [64, 16], F32)
        kmb = sb("kmb", [64, 16], BF16)
        mo = [sb(f"mo{i}", [128, 4, 128], BF16) for i in range(2)]
        abuf = sb("abuf", [128, 4, 128], F32)
        dbuf = [sb(f"dbuf{i}", [128, 128], F32) for i in range(2)]
        small = sb("small", [128, 64], F32)
        mt = [sb(f"mt{i}", [128, D], BF16) for i in range(2)]
        mT = [sb(f"mT{i}", [128, NCH, 128], BF16) for i in range(2)]
        ident = sb("ident", [128, 128], BF16)
        cmask = sb("cmask", [128, 128], BF16)
        bones = sb("bones", [128, 128], BF16)
        gbt = sb("gbt", [128, NT * 16], F32)
        gcol = sb("gcol_s", [128, 4], F32)
        qcol = sb("qcol_s", [128, 4], F32)
        subbc = sb("subbc", [128, 128], F32)
        lamv = sb("lamv_s", [128, 256], F32)
        lamt = sb("lamt", [128, 8], F32)

        ps = [pst(f"ps{i}", [128, 512], F32) for i in range(7)]
        tp = pst("tp", [128, 1024], BF16)

        sems = {e: es.enter_context(nc.semaphore(f"sem_{e}")) for e in ["pe", "act", "dve", "pool", "sp"]}
        dsems = {q: [es.enter_context(nc.semaphore(f"dsem_{q}{i}")) for i in range(NDSEM)] for q in ["sp", "pool"]}
        block = es.enter_context(nc.Block())

        P = Prog()
        cnt = {"s": 0, "pt": 0, "small": 0, "pj": 0}

        def psr(k):
            return ("ps", k)

        def scol():
            c = cnt["small"] % 64
            cnt["small"] += 1
            return c

        P.add("pool", lambda e: e.dma_start(out=ident[:], in_=ident_d[:, :]), writes=["ident"], dma=True)
        P.add("pool", lambda e: e.dma_start(out=cmask[:], in_=cm_d[:, :]), writes=["cmask"], dma=True)
        P.add("pool", lambda e: e.dma_start(out=bones[:], in_=bones_d[:, :]), writes=["bones"], dma=True)
        P.add("sp", lambda e: e.dma_start(out=gbt[:], in_=gb_d[:, :].to_broadcast([128, NT * 16])), writes=["gbt"], dma=True)
        for i in range(2):
            P.add("pool", lambda e, i=i: e.dma_start(out=KT[i][64:84, :], in_=kaug_d[:, :]),
                  writes=[("KTaug", i)], dma=True)
        P.add("dve", lambda e: e.memset(kmb[:], 0.0), writes=["kmb"])

        def layer(l):
            src = x_d if l == 0 else x1_d
            dst = out_d if l == L - 1 else x1_d
            srcn = "xd" if l == 0 else "x1d"
            dstn = "outd" if l == L - 1 else "x1d"
            li = lambda_inits[l]

            P.add("sp", lambda e: e.dma_start(out=gbc[:], in_=ng_d[l:l + 1, :].to_broadcast([128, D])),
                  writes=["gbc"], dma=True)
            P.add("sp", lambda e: e.dma_start(out=gcol[:], in_=gcol_d[l, :, :]), writes=["gcol"], dma=True)
            P.add("sp", lambda e: e.dma_start(out=subbc[:], in_=sub_d[l:l + 1, :].to_broadcast([128, 128])),
                  writes=["subbc"], dma=True)
            P.add("sp", lambda e: e.dma_start(out=lamv[:], in_=lamv_d[l:l + 1, :].to_broadcast([128, 256])),
                  writes=["lamv"], dma=True)
            P.add("pool", lambda e: e.dma_start(out=wout[:].rearrange("p c n -> p (c n)"), in_=w_out_d[l, :, :]),
                  writes=["wout"], dma=True)
            P.add("dve", lambda e: e.tensor_scalar(out=qcol[:], in0=gcol[:], scalar1=0.125, scalar2=None, op0=ALU.mult),
                  reads=["gcol"], writes=["qcol"])
            P.add("dve", lambda e: e.tensor_scalar(out=subbc[:], in0=subbc[:], scalar1=float(1.0 - li), scalar2=None,
                                                   op0=ALU.mult), reads=["subbc"], writes=["subbc"])
            P.add("dve", lambda e: e.tensor_tensor(out=lamv[:, 0:64], in0=lamv[:, 0:64], in1=lamv[:, 64:128], op=ALU.mult),
                  reads=["lamv"], writes=["lamv"])
            P.add("dve", lambda e: e.tensor_tensor(out=lamv[:, 128:192], in0=lamv[:, 128:192], in1=lamv[:, 192:256],
                                                   op=ALU.mult), reads=["lamv"], writes=["lamv"])
            P.add("dve", lambda e: e.tensor_reduce(out=lamt[:, 0:1], in_=lamv[:, 0:64], axis=AX.X, op=ALU.add),
                  reads=["lamv"], writes=["lamt"])
            P.add("dve", lambda e: e.tensor_reduce(out=lamt[:, 1:2], in_=lamv[:, 128:192], axis=AX.X, op=ALU.add),
                  reads=["lamv", "lamt"], writes=["lamt"])
            P.add("act", lambda e: e.activation(out=lamt[:, 2:4], in_=lamt[:, 0:2], func=AF.Exp),
                  reads=["lamt"], writes=["lamt"])
            P.add("dve", lambda e: e.scalar_tensor_tensor(out=lamt[:, 4:5], in0=lamt[:, 3:4], scalar=float(-li),
                                                          in1=lamt[:, 2:3], op0=ALU.add, op1=ALU.subtract),
                  reads=["lamt"], writes=["neglam"])

            for t in range(NT):
                b = t % 2
                P.add("sp", lambda e, t=t, b=b: e.dma_start(out=xt[b][:], in_=src[t * 128:(t + 1) * 128, :]),
                      reads=[(srcn, t)], writes=[("xt", b)], dma=True)
                c0 = scol()
                P.add("act", lambda e, b=b, c0=c0: e.activation(out=junk[:], in_=xt[b][:], func=AF.Square,
                                                               accum_out=small[:, c0:c0 + 1]),
                      reads=[("xt", b)], writes=["junk", ("small", c0)])
                P.add("act", lambda e, c0=c0: e.activation(out=small[:, c0:c0 + 1], in_=small[:, c0:c0 + 1], func=AF.Ln,
                                                          scale=1.0 / D, bias=EPS),
                      reads=[("small", c0)], writes=[("small", c0)])
                P.add("act", lambda e, c0=c0: e.activation(out=small[:, c0:c0 + 1], in_=small[:, c0:c0 + 1], func=AF.Exp,
                                                          scale=-0.5),
                      reads=[("small", c0)], writes=[("small", c0)])
                P.add("dve", lambda e, b=b, c0=c0: e.scalar_tensor_tensor(out=hb[b][:], in0=xt[b][:],
                                                                         scalar=small[:, c0:c0 + 1], in1=gbc[:],
                                                                         op0=ALU.mult, op1=ALU.mult),
                      reads=[("xt", b), ("small", c0), "gbc"], writes=[("hb", b)])
                for c in range(NCH):
                    P.add("pe", lambda e, b=b, c=c: e.transpose(tp[:, c * 128:(c + 1) * 128], hb[b][:, c * 128:(c + 1) * 128],
                                                                ident[:]),
                          reads=[("hb", b), "ident"], writes=["tp"])
                P.add("act", lambda e, t=t: e.copy(out=hT[:, :, t * 128:(t + 1) * 128],
                                                  in_=tp[:, :].rearrange("p (c n) -> p c n", n=128)),
                      writes=["tp", ("hT", t // 4)])

            for u in range(8):
                unit(l, u, li)

            for t in range(NT):
                b = t % 2
                P.add("sp", lambda e, t=t, b=b: e.dma_start(out=mt[b][:], in_=mix_d[t * 128:(t + 1) * 128, :]),
                      reads=[("mixd", t // 4)], writes=[("mt", b)], dma=True)
                P.add("sp", lambda e, t=t, b=b: e.dma_start(out=xt[b][:], in_=src[t * 128:(t + 1) * 128, :]),
                      reads=[(srcn, t)], writes=[("xt", b)], dma=True)
                for c in range(NCH):
                    P.add("pe", lambda e, b=b, c=c: e.transpose(tp[:, c * 128:(c + 1) * 128], mt[b][:, c * 128:(c + 1) * 128],
                                                                ident[:]),
                          reads=[("mt", b), "ident"], writes=["tp"])
                P.add("act", lambda e, b=b: e.copy(out=mT[b][:, :, :], in_=tp[:, :].rearrange("p (c n) -> p c n", n=128)),
                      writes=["tp", ("mT", b)])
                for half in range(2):
                    bank = 6 if half == 0 else 0
                    for c in range(NCH):
                        P.add("pe", lambda e, b=b, c=c, half=half, bank=bank: e.matmul(
                            ps[bank][:, :], lhsT=mT[b][:, c, :], rhs=wout[:, c, half * 512:(half + 1) * 512],
                            start=(c == 0), stop=(c == NCH - 1)),
                            reads=[("mT", b), "wout"], writes=[psr(bank)])
                    P.add("dve", lambda e, b=b, half=half, bank=bank: e.tensor_tensor(
                        out=xt[b][:, half * 512:(half + 1) * 512], in0=xt[b][:, half * 512:(half + 1) * 512],
                        in1=ps[bank][:, :], op=ALU.add),
                        reads=[("xt", b)], writes=[psr(bank), ("xt", b)])
                P.add("sp", lambda e, t=t, b=b: e.dma_start(out=dst[t * 128:(t + 1) * 128, :], in_=xt[b][:]),
                      reads=[("xt", b)], writes=[(dstn, t)], dma=True)

        def unit(l, u, li):
            moba = u < 4
            dvw = 65 if moba else 129
            heads = [2 * u, 2 * u + 1] if moba else [8 + (u - 4), 8 + (u - 4)]
            P.add("pool", lambda e: e.dma_start(out=wbf[:].rearrange("p c n -> p (c n)"), in_=w_in_d[l, u, :, :]),
                  writes=["wbf"], dma=True)
            for i in range(2):
                P.add("pool", lambda e, i=i: e.dma_start(out=QT[i][80:84, :], in_=qaug_d[heads[i], :, :]),
                      writes=[("QTaug", i)], dma=True)
            if moba:
                P.add("dve", lambda e: e.memset(Vt[:, :, 64:65], 1.0), writes=["Vt"])
                P.add("dve", lambda e: e.memset(Vt[:, :, 129:130], 1.0), writes=["Vt"])
            else:
                P.add("dve", lambda e: e.memset(Vt[:, :, 128:129], 1.0), writes=["Vt"])
                if u == 4:
                    for i in range(2):
                        P.add("dve", lambda e, i=i: e.memset(QT[i][64:80, :], 0.0), writes=[("QTsel", i)])

            for which in range(2):
                T = QT if which == 0 else KT
                tname = "QT" if which == 0 else "KT"
                colt = qcol if which == 0 else gcol
                cidx = (0 if moba else 2) + which
                for g in range(NG):
                    pj = 6 if (cnt["pj"] % 2 == 0) else 1
                    cnt["pj"] += 1
                    b = g % 2
                    for c in range(NCH):
                        P.add("pe", lambda e, c=c, g=g, pj=pj, which=which: e.matmul(
                            ps[pj][:, :], lhsT=wbf[:, c, which * 128:(which + 1) * 128],
                            rhs=hT[:, c, g * 512:(g + 1) * 512], start=(c == 0), stop=(c == NCH - 1)),
                            reads=["wbf", ("hT", g)], writes=[psr(pj)])
                    P.add("act", lambda e, b=b, pj=pj: e.activation(out=sq[b][:], in_=ps[pj][:, :], func=AF.Square),
                          writes=[psr(pj), ("sq", b)])
                    P.add("pe", lambda e, b=b: e.matmul(ps[0][:, :], lhsT=bones[:], rhs=sq[b][:], start=True, stop=True),
                          reads=["bones", ("sq", b)], writes=[psr(0)])
                    P.add("act", lambda e, b=b: e.activation(out=rs[b][:], in_=ps[0][:, :], func=AF.Ln, scale=1.0 / 64,
                                                            bias=EPS),
                          writes=[psr(0), ("rs", b)])
                    P.add("act", lambda e, b=b: e.activation(out=rs[b][:], in_=rs[b][:], func=AF.Exp, scale=-0.5),
                          reads=[("rs", b)], writes=[("rs", b)])
                    for i in range(2):
                        P.add("dve", lambda e, i=i, b=b, g=g, pj=pj, T=T, colt=colt, cidx=cidx: e.scalar_tensor_tensor(
                            out=T[i][0:64, g * 512:(g + 1) * 512], in0=ps[pj][i * 64:(i + 1) * 64, :],
                            scalar=colt[i * 64:(i + 1) * 64, cidx:cidx + 1], in1=rs[b][i * 64:(i + 1) * 64, :],
                            op0=ALU.mult, op1=ALU.mult),
                            reads=[("rs", b), "qcol", "gcol"], writes=[psr(pj), (tname, i, g)])

            for t in range(NT):
                bank = 2 + (t % 2)
                b = t % 2
                for c in range(NCH):
                    P.add("pe", lambda e, c=c, t=t, bank=bank: e.matmul(
                        ps[bank][:, 0:256], lhsT=hT[:, c, t * 128:(t + 1) * 128], rhs=wbf[:, c, 256:512],
                        start=(c == 0), stop=(c == NCH - 1)),
                        reads=["wbf", ("hT", t // 4)], writes=[psr(bank)])
                if moba:
                    P.add("dve", lambda e, t=t, bank=bank: e.tensor_copy(
                        out=Vt[:, t, :].rearrange("p (i c) -> p i c", c=65)[:, :, 0:64],
                        in_=ps[bank][:, 0:128].rearrange("p (i c) -> p i c", c=64)),
                        writes=[psr(bank), "Vt"])
                    P.add("act", lambda e, t=t, bank=bank: e.activation(out=Gt[:, t, :], in_=ps[bank][:, 128:256], func=AF.Silu),
                          writes=[psr(bank), "Gt"])
                else:
                    P.add("dve", lambda e, t=t, bank=bank: e.tensor_copy(out=Vt[:, t, 0:128], in_=ps[bank][:, 0:128]),
                          writes=[psr(bank), "Vt"])
                    P.add("act", lambda e, b=b, bank=bank: e.activation(out=gtmp[b][:], in_=ps[bank][:, 128:256], func=AF.Silu),
                          writes=[psr(bank), ("gtmp", b)])
                    P.add("dve", lambda e, t=t, b=b: e.tensor_tensor(out=Gt[:, t, :], in0=gtmp[b][:], in1=subbc[:], op=ALU.mult),
                          reads=[("gtmp", b), "subbc"], writes=["Gt"])

            if moba:
                for i in range(2):
                    P.add("dve", lambda e, i=i: e.tensor_reduce(
                        out=km[:, 0:NB], in_=KT[i][0:64, :].rearrange("p (n s) -> p n s", s=256), axis=AX.X, op=ALU.add),
                        reads=[("KT", i, g) for g in range(NG)], writes=["km"])
                    P.add("dve", lambda e: e.tensor_scalar(out=kmb[:, 0:NB], in0=km[:, 0:NB], scalar1=1.0 / 256, scalar2=None,
                                                           op0=ALU.mult), reads=["km"], writes=["kmb"])
                    for t in range(NT):
                        P.add("pe", lambda e, i=i, t=t: e.matmul(ps[6][:, t * 16:(t + 1) * 16],
                                                                 lhsT=QT[i][0:64, t * 128:(t + 1) * 128], rhs=kmb[:, :],
                                                                 start=True, stop=True),
                              reads=[("QT", i, t // 4), "kmb"], writes=[psr(6)])
                    P.add("dve", lambda e: e.tensor_tensor(out=gm[:].rearrange("p t n -> p (t n)"), in0=ps[6][:, 0:NT * 16],
                                                           in1=gbt[:], op=ALU.add),
                          reads=["gbt"], writes=[psr(6), "gm"])
                    for t in range(NT):
                        P.add("dve", lambda e, t=t: e.max(out=top8[:, t, :], in_=gm[:, t, :]), reads=["gm"], writes=["top8"])
                    P.add("dve", lambda e: e.tensor_scalar(out=thr[:], in0=top8[:, :, 3], scalar1=-1e29, scalar2=None,
                                                           op0=ALU.max), reads=["top8"], writes=["thr"])
                    P.add("dve", lambda e: e.tensor_tensor(out=sel[:], in0=gm[:], in1=thr[:].unsqueeze(2).to_broadcast([128, NT, 16]),
                                                           op=ALU.is_ge), reads=["gm", "thr"], writes=["sel"])
                    P.add("dve", lambda e: e.tensor_scalar(out=selb[:], in0=sel[:], scalar1=-NEGM, scalar2=NEGM,
                                                           op0=ALU.mult, op1=ALU.add), reads=["sel"], writes=["selb"])
                    for t0 in range(0, NT, 8):
                        for t in range(t0, t0 + 8):
                            P.add("pe", lambda e, t=t, t0=t0: e.transpose(tp[0:16, (t - t0) * 128:(t - t0 + 1) * 128],
                                                                          selb[:, t, :], ident[:]),
                                  reads=["selb", "ident"], writes=["tp"])
                        P.add("act", lambda e, i=i, t0=t0: e.copy(out=QT[i][64:80, t0 * 128:(t0 + 8) * 128], in_=tp[0:16, :]),
                              writes=["tp", ("QTsel", i)])

            slopes_all = MOBA_SLOPES + DIFF_SLOPES
            iters = []
            for g in range(NG):
                for i in range(2):
                    sl = slopes_all[heads[i]]
                    kt_first = 0
                    while kt_first < 4 * g and sl * (g * 512 - (kt_first * 128 + 127)) > ALIBI_FAR:
                        kt_first += 1
                    for kt in range(kt_first, 4 * (g + 1)):
                        iters.append(dict(g=g, i=i, kt=kt, first=(kt == kt_first), last=(kt == 4 * g + 3)))
            SB = [0, 1, 6]
            PIPE = 2

            def emit_S(n):
                it = iters[n]
                g, i, kt = it["g"], it["i"], it["kt"]
                j = kt - 4 * g
                s0 = max(0, j)
                sbk = SB[n % 3]
                P.add("pe", lambda e: e.matmul(
                    ps[sbk][:, s0 * 128:512], lhsT=KT[i][0:KR, kt * 128:(kt + 1) * 128],
                    rhs=QT[i][0:KR, g * 512 + s0 * 128:(g + 1) * 512], start=True, stop=(j < 0)),
                    reads=[("KT", i, kt // 4), ("KTaug", i), ("QT", i, g), ("QTaug", i), ("QTsel", i)],
                    writes=[psr(sbk)])
                if j >= 0:
                    P.add("pe", lambda e: e.matmul(ps[sbk][:, j * 128:(j + 1) * 128], lhsT=ident[:],
                                                   rhs=cmask[:], start=False, stop=True),
                          reads=["ident", "cmask"], writes=[psr(sbk)])

            def emit_rest(n):
                it = iters[n]
                g, i, kt = it["g"], it["i"], it["kt"]
                j = kt - 4 * g
                s0 = max(0, j)
                sbk = SB[n % 3]
                slot = n % NPT
                oset = (g * 2 + i) % 2
                ob = [2 + 2 * oset, 3 + 2 * oset]
                P.add("act", lambda e: e.activation(
                    out=PT[slot][:, s0 * 128:512], in_=ps[sbk][:, s0 * 128:512], func=AF.Exp),
                    writes=[psr(sbk), ("PT", slot)])
                for s in range(s0, 4):
                    bank = ob[s // 2]
                    col = (s % 2) * dvw
                    vlo = i * 65 if moba else 0
                    P.add("pe", lambda e, s=s, bank=bank, col=col, vlo=vlo: e.matmul(
                        ps[bank][:, col:col + dvw], lhsT=PT[slot][:, s * 128:(s + 1) * 128],
                        rhs=Vt[:, kt, vlo:vlo + dvw], start=(it["first"] and s % 2 == 0), stop=(kt == 4 * g + s),
                        skip_group_check=True),
                        reads=[("PT", slot), "Vt"], writes=[psr(bank)])
                if it["last"]:
                    epilogue(u, g, i, ob, dvw, moba)

            NI = len(iters)
            for n in range(min(PIPE, NI)):
                emit_S(n)
            for n in range(NI):
                if n + PIPE < NI:
                    emit_S(n + PIPE)
                emit_rest(n)

        def epilogue(u, g, i, ob, dvw, moba):
            mb = g % 2
            for s in range(4):
                t = 4 * g + s
                bank = ob[s // 2]
                col = (s % 2) * dvw
                c0 = scol()
                if moba:
                    P.add("dve", lambda e, bank=bank, col=col, c0=c0: e.reciprocal(out=small[:, c0:c0 + 1],
                                                                                   in_=ps[bank][:, col + 64:col + 65]),
                          writes=[psr(bank), ("small", c0)])
                    P.add("dve", lambda e, bank=bank, col=col, c0=c0, s=s, t=t, mb=mb, i=i: e.scalar_tensor_tensor(
                        out=mo[mb][:, s, i * 64:(i + 1) * 64], in0=ps[bank][:, col:col + 64], scalar=small[:, c0:c0 + 1],
                        in1=Gt[:, t, i * 64:(i + 1) * 64], op0=ALU.mult, op1=ALU.mult),
                        reads=[("small", c0), "Gt"], writes=[psr(bank), ("mo", mb)])
                elif i == 0:
                    P.add("dve", lambda e, bank=bank, col=col, c0=c0: e.reciprocal(out=small[:, c0:c0 + 1],
                                                                                   in_=ps[bank][:, col + 128:col + 129]),
                          writes=[psr(bank), ("small", c0)])
                    P.add("dve", lambda e, bank=bank, col=col, c0=c0, s=s: e.tensor_scalar(
                        out=abuf[:, s, :], in0=ps[bank][:, col:col + 128], scalar1=small[:, c0:c0 + 1], scalar2=None,
                        op0=ALU.mult),
                        reads=[("small", c0)], writes=[psr(bank), ("abuf", s)])
                else:
                    c1 = scol()
                    db = s % 2
                    P.add("dve", lambda e, bank=bank, col=col, c0=c0: e.reciprocal(out=small[:, c0:c0 + 1],
                                                                                   in_=ps[bank][:, col + 128:col + 129]),
                          writes=[psr(bank), ("small", c0)])
                    P.add("dve", lambda e, c0=c0: e.tensor_tensor(out=small[:, c0:c0 + 1], in0=small[:, c0:c0 + 1],
                                                                 in1=lamt[:, 4:5], op=ALU.mult),
                          reads=[("small", c0), "neglam"], writes=[("small", c0)])
                    P.add("dve", lambda e, bank=bank, col=col, c0=c0, s=s, db=db: e.scalar_tensor_tensor(
                        out=dbuf[db][:], in0=ps[bank][:, col:col + 128], scalar=small[:, c0:c0 + 1], in1=abuf[:, s, :],
                        op0=ALU.mult, op1=ALU.add),
                        reads=[("small", c0), ("abuf", s)], writes=[psr(bank), ("dbuf", db)])
                    P.add("act", lambda e, db=db, c1=c1: e.activation(out=junk[:, 0:128], in_=dbuf[db][:], func=AF.Square,
                                                                     accum_out=small[:, c1:c1 + 1]),
                          reads=[("dbuf", db)], writes=["junk", ("small", c1)])
                    P.add("act", lambda e, c1=c1: e.activation(out=small[:, c1:c1 + 1], in_=small[:, c1:c1 + 1], func=AF.Ln,
                                                              scale=1.0 / 128, bias=EPS),
                          reads=[("small", c1)], writes=[("small", c1)])
                    P.add("act", lambda e, c1=c1: e.activation(out=small[:, c1:c1 + 1], in_=small[:, c1:c1 + 1], func=AF.Exp,
                                                              scale=-0.5),
                          reads=[("small", c1)], writes=[("small", c1)])
                    P.add("dve", lambda e, db=db, c1=c1, s=s, t=t, mb=mb: e.scalar_tensor_tensor(
                        out=mo[mb][:, s, :], in0=dbuf[db][:], scalar=small[:, c1:c1 + 1], in1=Gt[:, t, :],
                        op0=ALU.mult, op1=ALU.mult),
                        reads=[("dbuf", db), ("small", c1), "Gt"], writes=[("mo", mb)])
            if i == 1:
                P.add("sp", lambda e, g=g, mb=mb, u=u: e.dma_start(
                    out=mix_d[g * 512:(g + 1) * 512, u * 128:(u + 1) * 128].rearrange("(s p) c -> p s c", p=128),
                    in_=mo[mb][:, :, :]),
                    reads=[("mo", mb)], writes=[("mixd", g)], dma=True)

        for l in range(L):
            layer(l)
        P.add("sp", None, reads=[("outd", t) for t in range(NT)])
        P.emit(nc, block, sems, dsems)
    return nc


def _host_layout(S, inputs):
    w_in = np.asarray(inputs["w_in"], dtype=np.float32)
    w_out = np.asarray(inputs["w_out"], dtype=np.float32)
    L = w_in.shape[0]
    NT = S // 128
    w_in_r = np.empty((L, 8, 128, NCH, 512), np.float32)
    for l in range(L):
        wl = w_in[l].reshape(NCH, 128, 4096)
        for u in range(8):
            base = 0 if u < 4 else 2048
            idx = u % 4
            for k in range(4):
                c0 = base + k * 512 + idx * 128
                w_in_r[l, u, :, :, k * 128:(k + 1) * 128] = wl[:, :, c0:c0 + 128].transpose(1, 0, 2)
    w_in_r = np.ascontiguousarray(w_in_r.reshape(L, 8, 128, NCH * 512))
    w_out_r = np.ascontiguousarray(w_out.reshape(L, NCH, 128, D).transpose(0, 2, 1, 3).reshape(L, 128, NCH * D))
    gcol = np.stack([np.stack([np.tile(np.asarray(inputs[k], np.float32)[l], 2) for k in
                               ("moba_q_norm", "moba_k_norm", "diff_q_norm", "diff_k_norm")], axis=1)
                     for l in range(L)], axis=0).astype(np.float32)
    lamv = np.concatenate([np.asarray(inputs[k], np.float32) for k in
                           ("lambda_q1", "lambda_k1", "lambda_q2", "lambda_k2")], axis=1)
    pos = np.arange(S)
    ident = np.eye(128, dtype=np.float32)
    kk = np.arange(128)[:, None]
    qq = np.arange(128)[None, :]
    cmask = np.where(kk <= qq, 0.0, NEGM).astype(np.float32)
    bones = np.kron(np.eye(2, dtype=np.float32), np.ones((64, 64), np.float32))
    kaug = np.zeros((20, S), np.float32)
    kaug[0:16] = (pos[None, :] // 256 == np.arange(16)[:, None]).astype(np.float32)
    kaug[16] = pos // 128
    kaug[17] = pos % 128
    kaug[18] = 1.0
    kaug[19] = 1.0
    qaug = np.zeros((12, 4, S), np.float32)
    for h, sl in enumerate(MOBA_SLOPES + DIFF_SLOPES):
        qaug[h, 0] = sl * 128.0
        qaug[h, 1] = sl
        qaug[h, 2] = -sl * 128.0 * (pos // 128)
        qaug[h, 3] = -sl * (pos % 128)
    gb = np.zeros((NT, 16), np.float32)
    for t in range(NT):
        own = t // 2
        gb[t, own] = 1e30
        gb[t, own + 1:] = -1e30
    common = {
        "w_in_r": w_in_r, "w_out_r": w_out_r,
        "norm_g": np.ascontiguousarray(np.asarray(inputs["norm_g"], np.float32)),
        "gcol": np.ascontiguousarray(gcol),
        "subln": np.ascontiguousarray(np.asarray(inputs["diff_subln"], np.float32)),
        "lamv": np.ascontiguousarray(lamv),
        "c_ident": ident, "c_cmask": cmask, "c_bones": bones, "c_kaug": kaug, "c_qaug": qaug,
        "c_gb": gb.reshape(1, NT * 16),
    }
    return L, common


def kernel(x, norm_g, w_in, moba_q_norm, moba_k_norm, diff_q_norm, diff_k_norm,
           lambda_q1, lambda_k1, lambda_q2, lambda_k2, diff_subln, w_out):
    inputs = dict(x=x, norm_g=norm_g, w_in=w_in, moba_q_norm=moba_q_norm, moba_k_norm=moba_k_norm,
                  diff_q_norm=diff_q_norm, diff_k_norm=diff_k_norm, lambda_q1=lambda_q1, lambda_k1=lambda_k1,
                  lambda_q2=lambda_q2, lambda_k2=lambda_k2, diff_subln=diff_subln, w_out=w_out)
    x = np.asarray(x, dtype=np.float32)
    B, S, _ = x.shape
    L, common = _host_layout(S, inputs)
    lambda_inits = [0.8 - 0.6 * math.exp(-0.3 * l) for l in range(L)]
    nc = build(S, L, lambda_inits)
    in_maps = [dict(common, x=np.ascontiguousarray(x[b])) for b in range(B)]
    res = run_bass_kernel_spmd(nc, in_maps, core_ids=list(range(B)))
    return np.stack([np.asarray(r["out"], dtype=np.float32) for r in res.results], axis=0)
```

```python
import math
import numpy as np
import concourse.bass as bass
import concourse.mybir as mybir
from concourse.bass_utils import run_bass_kernel_spmd

F32 = mybir.dt.float32
BF16 = mybir.dt.bfloat16
AF = mybir.ActivationFunctionType
ALU = mybir.AluOpType
AX = mybir.AxisListType

D = 1024
NCH = 8
EPS = 1e-6
NEGM = -30000.0
KR = 84
MOBA_SLOPES = [2.0 ** (-8.0 * i / 8) for i in range(1, 9)]
DIFF_SLOPES = [2.0 ** (-8.0 * i / 4) for i in range(1, 5)]
NDSEM = 8
ALIBI_FAR = 100.0


class Prog:
    def __init__(self):
        self.ops = []
        self.last_w = {}
        self.readers = {}
        self.dma_hist = {"sp": [], "pool": []}

    def add(self, eng, fn, reads=(), writes=(), dma=False):
        idx = len(self.ops)
        raw = set()
        deps = set()
        for r in reads:
            if r in self.last_w:
                raw.add(self.last_w[r])
        for w in writes:
            if w in self.last_w:
                deps.add(self.last_w[w])
            for rd in self.readers.get(w, ()):
                deps.add(rd)
        deps |= raw
        keep = set()
        for j in deps:
            oj = self.ops[j]
            if oj["dma"]:
                keep.add(j)
            elif oj["eng"] != eng:
                keep.add(j)
            elif (j in raw) and eng != "pe" and not dma:
                keep.add(j)
            elif dma:
                keep.add(j)
        op = dict(eng=eng, fn=fn, dma=dma, deps=keep, sig=dma, sem=None, val=None)
        if dma:
            h = self.dma_hist[eng]
            n = len(h)
            if n >= NDSEM:
                op["deps"].add(h[n - NDSEM])
            op["slot"] = n % NDSEM
            op["val"] = 16 * (n // NDSEM + 1)
            h.append(idx)
        for w in writes:
            self.last_w[w] = idx
            self.readers[w] = []
        for r in reads:
            self.readers.setdefault(r, []).append(idx)
        self.ops.append(op)
        return idx

    def emit(self, nc, block, sems, dsems):
        ops = self.ops
        for op in ops:
            for j in op["deps"]:
                ops[j]["sig"] = True
        cnt = {e: 0 for e in sems}
        for op in ops:
            if op["dma"]:
                op["sem"] = dsems[op["eng"]][op["slot"]]
            elif op["sig"]:
                cnt[op["eng"]] += 1
                op["sem"] = sems[op["eng"]]
                op["val"] = cnt[op["eng"]]

        def run(engname, eng):
            waited = {}
            for op in ops:
                if op["eng"] != engname:
                    continue
                need = {}
                for j in op["deps"]:
                    oj = ops[j]
                    key = id(oj["sem"])
                    if waited.get(key, 0) >= oj["val"]:
                        continue
                    if key not in need or need[key][1] < oj["val"]:
                        need[key] = (oj["sem"], oj["val"])
                for key, (sem, val) in need.items():
                    eng.wait_ge(sem, val)
                    waited[key] = val
                if op["fn"] is None:
                    continue
                ins = op["fn"](eng)
                if op["dma"]:
                    ins.then_inc(op["sem"], 16)
                elif op["sig"]:
                    ins.then_inc(op["sem"], 1)

        @block.tensor
        def _(e):
            run("pe", e)

        @block.scalar
        def _(e):
            run("act", e)

        @block.vector
        def _(e):
            run("dve", e)

        @block.gpsimd
        def _(e):
            run("pool", e)

        @block.sync
        def _(e):
            run("sp", e)


def build(S, L, lambda_inits):
    NT = S // 128
    NG = S // 512
    NB = S // 256
    assert S % 512 == 0 and NT <= 32
    nc = bass.Bass("TRN2", target_bir_lowering=False)

    def dram(name, shape, dt, kind):
        return nc.dram_tensor(name, list(shape), dt, kind=kind).ap()

    x_d = dram("x", [S, D], F32, "ExternalInput")
    w_in_d = dram("w_in_r", [L, 8, 128, NCH * 512], F32, "ExternalInput")
    w_out_d = dram("w_out_r", [L, 128, NCH * D], F32, "ExternalInput")
    ng_d = dram("norm_g", [L, D], F32, "ExternalInput")
    gcol_d = dram("gcol", [L, 128, 4], F32, "ExternalInput")
    sub_d = dram("subln", [L, 128], F32, "ExternalInput")
    lamv_d = dram("lamv", [L, 256], F32, "ExternalInput")
    ident_d = dram("c_ident", [128, 128], F32, "ExternalInput")
    cm_d = dram("c_cmask", [128, 128], F32, "ExternalInput")
    bones_d = dram("c_bones", [128, 128], F32, "ExternalInput")
    kaug_d = dram("c_kaug", [20, S], F32, "ExternalInput")
    qaug_d = dram("c_qaug", [12, 4, S], F32, "ExternalInput")
    gb_d = dram("c_gb", [1, NT * 16], F32, "ExternalInput")
    out_d = dram("out", [S, D], F32, "ExternalOutput")
    x1_d = dram("x1_scratch", [S, D], F32, "Internal")
    mix_d = dram("mix_scratch", [S, D], BF16, "Internal")

    from contextlib import ExitStack
    es = ExitStack()

    def sb(name, shape, dt):
        return es.enter_context(nc.sbuf_tensor(name, list(shape), dt))

    def pst(name, shape, dt):
        return es.enter_context(nc.psum_tensor(name, list(shape), dt))

    with es:
        hT = sb("hT", [128, NCH, S], BF16)
        xt = [sb(f"xt{i}", [128, D], F32) for i in range(2)]
        hb = [sb(f"hb{i}", [128, D], BF16) for i in range(2)]
        junk = sb("junk", [128, D], BF16)
        gbc = sb("gbc", [128, D], F32)
        wbf = sb("wbf", [128, NCH, 512], BF16)
        wout = sb("wout", [128, NCH, D], BF16)
        QT = [sb(f"QT{i}", [128, S], BF16) for i in range(2)]
        KT = [sb(f"KT{i}", [128, S], BF16) for i in range(2)]
        Vt = sb("Vt", [128, NT, 130], BF16)
        Gt = sb("Gt", [128, NT, 128], BF16)
        gtmp = [sb(f"gtmp{i}", [128, 128], F32) for i in range(2)]
        sq = [sb(f"sq{i}", [128, 512], BF16) for i in range(2)]
        rs = [sb(f"rs{i}", [128, 512], F32) for i in range(2)]
        NPT = 4
        PT = [sb(f"PT{i}", [128, 512], BF16) for i in range(NPT)]
        gm = [sb(f"gm{i}", [128, NT, 16], F32) for i in range(2)]
        top8 = sb("top8", [128, NT, 8], F32)
        thr = sb("thr", [128, NT], F32)
        sel = sb("sel", [128, NT, 16], F32)
        selb = [sb(f"selb{i}", [128, NT, 16], BF16) for i in range(2)]
        km = [sb(f"km{i}", [64, 16], F32) for i in range(2)]
        kmb = [sb(f"kmb{i}", [64, 16], BF16) for i in range(2)]
        mo = [sb(f"mo{i}", [128, 4, 128], BF16) for i in range(2)]
        abuf = sb("abuf", [128, 4, 128], F32)
        dbuf = [sb(f"dbuf{i}", [128, 128], F32) for i in range(2)]
        small = sb("small", [128, 64], F32)
        mt = [sb(f"mt{i}", [128, D], BF16) for i in range(2)]
        mT = [sb(f"mT{i}", [128, NCH, 128], BF16) for i in range(2)]
        ident = sb("ident", [128, 128], BF16)
        cmask = sb("cmask", [128, 128], BF16)
        bones = sb("bones", [128, 128], BF16)
        gbt = sb("gbt", [128, NT * 16], F32)
        gcol = sb("gcol_s", [128, 4], F32)
        qcol = sb("qcol_s", [128, 4], F32)
        subbc = sb("subbc", [128, 128], F32)
        lamv = sb("lamv_s", [128, 256], F32)
        lamt = sb("lamt", [128, 8], F32)

        ps = [pst(f"ps{i}", [128, 512], F32) for i in range(7)]
        tp = pst("tp", [128, 1024], BF16)

        sems = {e: es.enter_context(nc.semaphore(f"sem_{e}")) for e in ["pe", "act", "dve", "pool", "sp"]}
        dsems = {q: [es.enter_context(nc.semaphore(f"dsem_{q}{i}")) for i in range(NDSEM)] for q in ["sp", "pool"]}
        block = es.enter_context(nc.Block())

        P = Prog()
        cnt = {"s": 0, "pt": 0, "small": 0, "pj": 0}

        def psr(k):
            return ("ps", k)

        def scol():
            c = cnt["small"] % 64
            cnt["small"] += 1
            return c

        P.add("pool", lambda e: e.dma_start(out=ident[:], in_=ident_d[:, :]), writes=["ident"], dma=True)
        P.add("pool", lambda e: e.dma_start(out=cmask[:], in_=cm_d[:, :]), writes=["cmask"], dma=True)
        P.add("pool", lambda e: e.dma_start(out=bones[:], in_=bones_d[:, :]), writes=["bones"], dma=True)
        P.add("sp", lambda e: e.dma_start(out=gbt[:], in_=gb_d[:, :].to_broadcast([128, NT * 16])), writes=["gbt"], dma=True)
        for i in range(2):
            P.add("pool", lambda e, i=i: e.dma_start(out=KT[i][64:84, :], in_=kaug_d[:, :]),
                  writes=[("KTaug", i)], dma=True)
        for i in range(2):
            P.add("dve", lambda e, i=i: e.memset(kmb[i][:], 0.0), writes=[("kmb", i)])

        def layer(l):
            src = x_d if l == 0 else x1_d
            dst = out_d if l == L - 1 else x1_d
            srcn = "xd" if l == 0 else "x1d"
            dstn = "outd" if l == L - 1 else "x1d"
            li = lambda_inits[l]

            P.add("sp", lambda e: e.dma_start(out=gbc[:], in_=ng_d[l:l + 1, :].to_broadcast([128, D])),
                  writes=["gbc"], dma=True)
            P.add("sp", lambda e: e.dma_start(out=gcol[:], in_=gcol_d[l, :, :]), writes=["gcol"], dma=True)
            P.add("sp", lambda e: e.dma_start(out=subbc[:], in_=sub_d[l:l + 1, :].to_broadcast([128, 128])),
                  writes=["subbc"], dma=True)
            P.add("sp", lambda e: e.dma_start(out=lamv[:], in_=lamv_d[l:l + 1, :].to_broadcast([128, 256])),
                  writes=["lamv"], dma=True)
            P.add("pool", lambda e: e.dma_start(out=wout[:].rearrange("p c n -> p (c n)"), in_=w_out_d[l, :, :]),
                  writes=["wout"], dma=True)
            P.add("dve", lambda e: e.tensor_scalar(out=qcol[:], in0=gcol[:], scalar1=0.125, scalar2=None, op0=ALU.mult),
                  reads=["gcol"], writes=["qcol"])
            P.add("dve", lambda e: e.tensor_scalar(out=subbc[:], in0=subbc[:], scalar1=float(1.0 - li), scalar2=None,
                                                   op0=ALU.mult), reads=["subbc"], writes=["subbc"])
            P.add("dve", lambda e: e.tensor_tensor(out=lamv[:, 0:64], in0=lamv[:, 0:64], in1=lamv[:, 64:128], op=ALU.mult),
                  reads=["lamv"], writes=["lamv"])
            P.add("dve", lambda e: e.tensor_tensor(out=lamv[:, 128:192], in0=lamv[:, 128:192], in1=lamv[:, 192:256],
                                                   op=ALU.mult), reads=["lamv"], writes=["lamv"])
            P.add("dve", lambda e: e.tensor_reduce(out=lamt[:, 0:1], in_=lamv[:, 0:64], axis=AX.X, op=ALU.add),
                  reads=["lamv"], writes=["lamt"])
            P.add("dve", lambda e: e.tensor_reduce(out=lamt[:, 1:2], in_=lamv[:, 128:192], axis=AX.X, op=ALU.add),
                  reads=["lamv", "lamt"], writes=["lamt"])
            P.add("act", lambda e: e.activation(out=lamt[:, 2:4], in_=lamt[:, 0:2], func=AF.Exp),
                  reads=["lamt"], writes=["lamt"])
            P.add("dve", lambda e: e.scalar_tensor_tensor(out=lamt[:, 4:5], in0=lamt[:, 3:4], scalar=float(-li),
                                                          in1=lamt[:, 2:3], op0=ALU.add, op1=ALU.subtract),
                  reads=["lamt"], writes=["neglam"])

            for t in range(NT):
                b = t % 2
                P.add("sp", lambda e, t=t, b=b: e.dma_start(out=xt[b][:], in_=src[t * 128:(t + 1) * 128, :]),
                      reads=[(srcn, t)], writes=[("xt", b)], dma=True)
                c0 = scol()
                P.add("act", lambda e, b=b, c0=c0: e.activation(out=junk[:], in_=xt[b][:], func=AF.Square,
                                                               accum_out=small[:, c0:c0 + 1]),
                      reads=[("xt", b)], writes=["junk", ("small", c0)])
                P.add("act", lambda e, c0=c0: e.activation(out=small[:, c0:c0 + 1], in_=small[:, c0:c0 + 1], func=AF.Ln,
                                                          scale=1.0 / D, bias=EPS),
                      reads=[("small", c0)], writes=[("small", c0)])
                P.add("act", lambda e, c0=c0: e.activation(out=small[:, c0:c0 + 1], in_=small[:, c0:c0 + 1], func=AF.Exp,
                                                          scale=-0.5),
                      reads=[("small", c0)], writes=[("small", c0)])
                P.add("dve", lambda e, b=b, c0=c0: e.scalar_tensor_tensor(out=hb[b][:], in0=xt[b][:],
                                                                         scalar=small[:, c0:c0 + 1], in1=gbc[:],
                                                                         op0=ALU.mult, op1=ALU.mult),
                      reads=[("xt", b), ("small", c0), "gbc"], writes=[("hb", b)])
                for c in range(NCH):
                    P.add("pe", lambda e, b=b, c=c: e.transpose(tp[:, c * 128:(c + 1) * 128], hb[b][:, c * 128:(c + 1) * 128],
                                                                ident[:]),
                          reads=[("hb", b), "ident"], writes=["tp"])
                P.add("act", lambda e, t=t: e.copy(out=hT[:, :, t * 128:(t + 1) * 128],
                                                  in_=tp[:, :].rearrange("p (c n) -> p c n", n=128)),
                      writes=["tp", ("hT", t // 4)])

            for u in range(8):
                unit(l, u, li)

            for t in range(NT):
                b = t % 2
                P.add("sp", lambda e, t=t, b=b: e.dma_start(out=mt[b][:], in_=mix_d[t * 128:(t + 1) * 128, :]),
                      reads=[("mixd", t // 4)], writes=[("mt", b)], dma=True)
                P.add("sp", lambda e, t=t, b=b: e.dma_start(out=xt[b][:], in_=src[t * 128:(t + 1) * 128, :]),
                      reads=[(srcn, t)], writes=[("xt", b)], dma=True)
                for c in range(NCH):
                    P.add("pe", lambda e, b=b, c=c: e.transpose(tp[:, c * 128:(c + 1) * 128], mt[b][:, c * 128:(c + 1) * 128],
                                                                ident[:]),
                          reads=[("mt", b), "ident"], writes=["tp"])
                P.add("act", lambda e, b=b: e.copy(out=mT[b][:, :, :], in_=tp[:, :].rearrange("p (c n) -> p c n", n=128)),
                      writes=["tp", ("mT", b)])
                for half in range(2):
                    bank = ([6, 0] if t % 2 == 0 else [1, 2])[half]
                    for c in range(NCH):
                        P.add("pe", lambda e, b=b, c=c, half=half, bank=bank: e.matmul(
                            ps[bank][:, :], lhsT=mT[b][:, c, :], rhs=wout[:, c, half * 512:(half + 1) * 512],
                            start=(c == 0), stop=(c == NCH - 1)),
                            reads=[("mT", b), "wout"], writes=[psr(bank)])
                    P.add("dve", lambda e, b=b, half=half, bank=bank: e.tensor_tensor(
                        out=xt[b][:, half * 512:(half + 1) * 512], in0=xt[b][:, half * 512:(half + 1) * 512],
                        in1=ps[bank][:, :], op=ALU.add),
                        reads=[("xt", b)], writes=[psr(bank), ("xt", b)])
                P.add("sp", lambda e, t=t, b=b: e.dma_start(out=dst[t * 128:(t + 1) * 128, :], in_=xt[b][:]),
                      reads=[("xt", b)], writes=[(dstn, t)], dma=True)

        def unit(l, u, li):
            moba = u < 4
            dvw = 65 if moba else 129
            heads = [2 * u, 2 * u + 1] if moba else [8 + (u - 4), 8 + (u - 4)]
            P.add("pool", lambda e: e.dma_start(out=wbf[:].rearrange("p c n -> p (c n)"), in_=w_in_d[l, u, :, :]),
                  writes=["wbf"], dma=True)
            for i in range(2):
                P.add("pool", lambda e, i=i: e.dma_start(out=QT[i][80:84, :], in_=qaug_d[heads[i], :, :]),
                      writes=[("QTaug", i)], dma=True)
            if moba:
                P.add("dve", lambda e: e.memset(Vt[:, :, 64:65], 1.0), writes=["Vt"])
                P.add("dve", lambda e: e.memset(Vt[:, :, 129:130], 1.0), writes=["Vt"])
            else:
                P.add("dve", lambda e: e.memset(Vt[:, :, 128:129], 1.0), writes=["Vt"])
                if u == 4:
                    for i in range(2):
                        P.add("dve", lambda e, i=i: e.memset(QT[i][64:80, :], 0.0), writes=[("QTsel", i)])

            groups = [(which, g) for which in range(2) for g in range(NG)]

            def emit_proj(n):
                which, g = groups[n]
                pj = 6 if n % 2 == 0 else 1
                for c in range(NCH):
                    P.add("pe", lambda e, c=c: e.matmul(
                        ps[pj][:, :], lhsT=wbf[:, c, which * 128:(which + 1) * 128],
                        rhs=hT[:, c, g * 512:(g + 1) * 512], start=(c == 0), stop=(c == NCH - 1)),
                        reads=["wbf", ("hT", g)], writes=[psr(pj)])

            def emit_chain(n):
                which, g = groups[n]
                pj = 6 if n % 2 == 0 else 1
                b = n % 2
                T = QT if which == 0 else KT
                tname = "QT" if which == 0 else "KT"
                colt = qcol if which == 0 else gcol
                cidx = (0 if moba else 2) + which
                P.add("act", lambda e: e.activation(out=sq[b][:], in_=ps[pj][:, :], func=AF.Square),
                      writes=[psr(pj), ("sq", b)])
                P.add("pe", lambda e: e.matmul(ps[0][:, :], lhsT=bones[:], rhs=sq[b][:], start=True, stop=True),
                      reads=["bones", ("sq", b)], writes=[psr(0)])
                P.add("act", lambda e: e.activation(out=rs[b][:], in_=ps[0][:, :], func=AF.Ln, scale=1.0 / 64, bias=EPS),
                      writes=[psr(0), ("rs", b)])
                P.add("act", lambda e: e.activation(out=rs[b][:], in_=rs[b][:], func=AF.Exp, scale=-0.5),
                      reads=[("rs", b)], writes=[("rs", b)])
                for i in range(2):
                    P.add("dve", lambda e, i=i: e.scalar_tensor_tensor(
                        out=T[i][0:64, g * 512:(g + 1) * 512], in0=ps[pj][i * 64:(i + 1) * 64, :],
                        scalar=colt[i * 64:(i + 1) * 64, cidx:cidx + 1], in1=rs[b][i * 64:(i + 1) * 64, :],
                        op0=ALU.mult, op1=ALU.mult),
                        reads=[("rs", b), "qcol", "gcol"], writes=[psr(pj), (tname, i, g)])

            emit_proj(0)
            for n in range(len(groups)):
                if n + 1 < len(groups):
                    emit_proj(n + 1)
                emit_chain(n)

            side = []

            def sel_stage1(i):
                P.add("dve", lambda e: e.tensor_reduce(
                    out=km[i][:, 0:NB], in_=KT[i][0:64, :].rearrange("p (n s) -> p n s", s=256), axis=AX.X, op=ALU.add),
                    reads=[("KT", i, g) for g in range(NG)], writes=[("km", i)])
                P.add("dve", lambda e: e.tensor_scalar(out=kmb[i][:, 0:NB], in0=km[i][:, 0:NB], scalar1=1.0 / 256,
                                                       scalar2=None, op0=ALU.mult),
                      reads=[("km", i)], writes=[("kmb", i)])

            def sel_gate(i):
                gbank = 6 if i == 0 else 0
                for t in range(NT):
                    P.add("pe", lambda e, t=t: e.matmul(ps[gbank][:, t * 16:(t + 1) * 16],
                                                        lhsT=QT[i][0:64, t * 128:(t + 1) * 128], rhs=kmb[i][:, :],
                                                        start=True, stop=True),
                          reads=[("QT", i, t // 4), ("kmb", i)], writes=[psr(gbank)])
                P.add("dve", lambda e: e.tensor_tensor(out=gm[i][:].rearrange("p t n -> p (t n)"),
                                                       in0=ps[gbank][:, 0:NT * 16], in1=gbt[:], op=ALU.add),
                      reads=["gbt"], writes=[psr(gbank), ("gm", i)])

            def sel_chain(i):
                th = []
                for t in range(NT):
                    th.append(lambda t=t: P.add("dve", lambda e: e.max(out=top8[:, t, :], in_=gm[i][:, t, :]),
                                                reads=[("gm", i)], writes=["top8"]))
                th.append(lambda: P.add("dve", lambda e: e.tensor_scalar(out=thr[:], in0=top8[:, :, 3], scalar1=-1e29,
                                                                         scalar2=None, op0=ALU.max),
                                        reads=["top8"], writes=["thr"]))
                th.append(lambda: P.add("dve", lambda e: e.tensor_tensor(
                    out=sel[:], in0=gm[i][:], in1=thr[:].unsqueeze(2).to_broadcast([128, NT, 16]), op=ALU.is_ge),
                    reads=[("gm", i), "thr"], writes=["sel"]))
                th.append(lambda: P.add("dve", lambda e: e.tensor_scalar(out=selb[i][:], in0=sel[:], scalar1=-NEGM,
                                                                         scalar2=NEGM, op0=ALU.mult, op1=ALU.add),
                                        reads=["sel"], writes=[("selb", i)]))
                return th

            def sel_final(i):
                for t0 in range(0, NT, 8):
                    for t in range(t0, t0 + 8):
                        P.add("pe", lambda e, t=t, t0=t0: e.transpose(tp[0:16, (t - t0) * 128:(t - t0 + 1) * 128],
                                                                      selb[i][:, t, :], ident[:]),
                              reads=[("selb", i), "ident"], writes=["tp"])
                    P.add("act", lambda e, t0=t0: e.copy(out=QT[i][64:80, t0 * 128:(t0 + 8) * 128], in_=tp[0:16, :]),
                          writes=["tp", ("QTsel", i)])

            if moba:
                for i in range(2):
                    sel_stage1(i)

            for t in range(NT):
                bank = 2 + (t % 2)
                b = t % 2
                if moba and t == min(4, NT - 1):
                    for i in range(2):
                        sel_gate(i)
                        side.extend(sel_chain(i))
                for c in range(NCH):
                    P.add("pe", lambda e, c=c, t=t, bank=bank: e.matmul(
                        ps[bank][:, 0:256], lhsT=hT[:, c, t * 128:(t + 1) * 128], rhs=wbf[:, c, 256:512],
                        start=(c == 0), stop=(c == NCH - 1)),
                        reads=["wbf", ("hT", t // 4)], writes=[psr(bank)])
                if moba:
                    P.add("dve", lambda e, t=t, bank=bank: e.tensor_copy(
                        out=Vt[:, t, :].rearrange("p (i c) -> p i c", c=65)[:, :, 0:64],
                        in_=ps[bank][:, 0:128].rearrange("p (i c) -> p i c", c=64)),
                        writes=[psr(bank), "Vt"])
                    P.add("act", lambda e, t=t, bank=bank: e.activation(out=Gt[:, t, :], in_=ps[bank][:, 128:256], func=AF.Silu),
                          writes=[psr(bank), "Gt"])
                else:
                    P.add("dve", lambda e, t=t, bank=bank: e.tensor_copy(out=Vt[:, t, 0:128], in_=ps[bank][:, 0:128]),
                          writes=[psr(bank), "Vt"])
                    P.add("act", lambda e, b=b, bank=bank: e.activation(out=gtmp[b][:], in_=ps[bank][:, 128:256], func=AF.Silu),
                          writes=[psr(bank), ("gtmp", b)])
                    P.add("dve", lambda e, t=t, b=b: e.tensor_tensor(out=Gt[:, t, :], in0=gtmp[b][:], in1=subbc[:], op=ALU.mult),
                          reads=[("gtmp", b), "subbc"], writes=["Gt"])
                for _ in range(4):
                    if side:
                        side.pop(0)()
            while side:
                side.pop(0)()
            if moba:
                for i in range(2):
                    sel_final(i)

            slopes_all = MOBA_SLOPES + DIFF_SLOPES
            iters = []
            for g in range(NG):
                for i in range(2):
                    sl = slopes_all[heads[i]]
                    kt_first = 0
                    while kt_first < 4 * g and sl * (g * 512 - (kt_first * 128 + 127)) > ALIBI_FAR:
                        kt_first += 1
                    for kt in range(kt_first, 4 * (g + 1)):
                        iters.append(dict(g=g, i=i, kt=kt, first=(kt == kt_first), last=(kt == 4 * g + 3)))
            SB = [0, 1, 6]
            PIPE = 2

            def emit_S(n):
                it = iters[n]
                g, i, kt = it["g"], it["i"], it["kt"]
                j = kt - 4 * g
                s0 = max(0, j)
                sbk = SB[n % 3]
                P.add("pe", lambda e: e.matmul(
                    ps[sbk][:, s0 * 128:512], lhsT=KT[i][0:KR, kt * 128:(kt + 1) * 128],
                    rhs=QT[i][0:KR, g * 512 + s0 * 128:(g + 1) * 512], start=True, stop=(j < 0)),
                    reads=[("KT", i, kt // 4), ("KTaug", i), ("QT", i, g), ("QTaug", i), ("QTsel", i)],
                    writes=[psr(sbk)])
                if j >= 0:
                    P.add("pe", lambda e: e.matmul(ps[sbk][:, j * 128:(j + 1) * 128], lhsT=ident[:],
                                                   rhs=cmask[:], start=False, stop=True),
                          reads=["ident", "cmask"], writes=[psr(sbk)])

            def emit_rest(n):
                it = iters[n]
                g, i, kt = it["g"], it["i"], it["kt"]
                j = kt - 4 * g
                s0 = max(0, j)
                sbk = SB[n % 3]
                slot = n % NPT
                oset = (g * 2 + i) % 2
                ob = [2 + 2 * oset, 3 + 2 * oset]
                P.add("act", lambda e: e.activation(
                    out=PT[slot][:, s0 * 128:512], in_=ps[sbk][:, s0 * 128:512], func=AF.Exp),
                    writes=[psr(sbk), ("PT", slot)])
                for s in range(s0, 4):
                    bank = ob[s // 2]
                    col = (s % 2) * dvw
                    vlo = i * 65 if moba else 0
                    P.add("pe", lambda e, s=s, bank=bank, col=col, vlo=vlo: e.matmul(
                        ps[bank][:, col:col + dvw], lhsT=PT[slot][:, s * 128:(s + 1) * 128],
                        rhs=Vt[:, kt, vlo:vlo + dvw], start=(it["first"] and s % 2 == 0), stop=(kt == 4 * g + s),
                        skip_group_check=True),
                        reads=[("PT", slot), "Vt"], writes=[psr(bank)])
                if it["last"]:
                    pend.append((n + EPI_DELAY, (u, g, i, ob, dvw, moba)))

            NI = len(iters)
            pend = []
            EPI_DELAY = 2
            for n in range(min(PIPE, NI)):
                emit_S(n)
            for n in range(NI):
                if n + PIPE < NI:
                    emit_S(n + PIPE)
                emit_rest(n)
                while pend and pend[0][0] <= n:
                    epilogue(*pend.pop(0)[1])
            while pend:
                epilogue(*pend.pop(0)[1])

        def epilogue(u, g, i, ob, dvw, moba):
            mb = g % 2
            for s in range(4):
                t = 4 * g + s
                bank = ob[s // 2]
                col = (s % 2) * dvw
                c0 = scol()
                if moba:
                    P.add("dve", lambda e, bank=bank, col=col, c0=c0: e.reciprocal(out=small[:, c0:c0 + 1],
                                                                                   in_=ps[bank][:, col + 64:col + 65]),
                          writes=[psr(bank), ("small", c0)])
                    P.add("dve", lambda e, bank=bank, col=col, c0=c0, s=s, t=t, mb=mb, i=i: e.scalar_tensor_tensor(
                        out=mo[mb][:, s, i * 64:(i + 1) * 64], in0=ps[bank][:, col:col + 64], scalar=small[:, c0:c0 + 1],
                        in1=Gt[:, t, i * 64:(i + 1) * 64], op0=ALU.mult, op1=ALU.mult),
                        reads=[("small", c0), "Gt"], writes=[psr(bank), ("mo", mb)])
                elif i == 0:
                    P.add("dve", lambda e, bank=bank, col=col, c0=c0: e.reciprocal(out=small[:, c0:c0 + 1],
                                                                                   in_=ps[bank][:, col + 128:col + 129]),
                          writes=[psr(bank), ("small", c0)])
                    P.add("dve", lambda e, bank=bank, col=col, c0=c0, s=s: e.tensor_scalar(
                        out=abuf[:, s, :], in0=ps[bank][:, col:col + 128], scalar1=small[:, c0:c0 + 1], scalar2=None,
                        op0=ALU.mult),
                        reads=[("small", c0)], writes=[psr(bank), ("abuf", s)])
                else:
                    c1 = scol()
                    db = s % 2
                    P.add("dve", lambda e, bank=bank, col=col, c0=c0: e.reciprocal(out=small[:, c0:c0 + 1],
                                                                                   in_=ps[bank][:, col + 128:col + 129]),
                          writes=[psr(bank), ("small", c0)])
                    P.add("dve", lambda e, c0=c0: e.tensor_tensor(out=small[:, c0:c0 + 1], in0=small[:, c0:c0 + 1],
                                                                 in1=lamt[:, 4:5], op=ALU.mult),
                          reads=[("small", c0), "neglam"], writes=[("small", c0)])
                    P.add("dve", lambda e, bank=bank, col=col, c0=c0, s=s, db=db: e.scalar_tensor_tensor(
                        out=dbuf[db][:], in0=ps[bank][:, col:col + 128], scalar=small[:, c0:c0 + 1], in1=abuf[:, s, :],
                        op0=ALU.mult, op1=ALU.add),
                        reads=[("small", c0), ("abuf", s)], writes=[psr(bank), ("dbuf", db)])
                    P.add("act", lambda e, db=db, c1=c1: e.activation(out=junk[:, 0:128], in_=dbuf[db][:], func=AF.Square,
                                                                     accum_out=small[:, c1:c1 + 1]),
                          reads=[("dbuf", db)], writes=["junk", ("small", c1)])
                    P.add("act", lambda e, c1=c1: e.activation(out=small[:, c1:c1 + 1], in_=small[:, c1:c1 + 1], func=AF.Ln,
                                                              scale=1.0 / 128, bias=EPS),
                          reads=[("small", c1)], writes=[("small", c1)])
                    P.add("act", lambda e, c1=c1: e.activation(out=small[:, c1:c1 + 1], in_=small[:, c1:c1 + 1], func=AF.Exp,
                                                              scale=-0.5),
                          reads=[("small", c1)], writes=[("small", c1)])
                    P.add("dve", lambda e, db=db, c1=c1, s=s, t=t, mb=mb: e.scalar_tensor_tensor(
                        out=mo[mb][:, s, :], in0=dbuf[db][:], scalar=small[:, c1:c1 + 1], in1=Gt[:, t, :],
                        op0=ALU.mult, op1=ALU.mult),
                        reads=[("dbuf", db), ("small", c1), "Gt"], writes=[("mo", mb)])
            if i == 1:
                P.add("sp", lambda e, g=g, mb=mb, u=u: e.dma_start(
                    out=mix_d[g * 512:(g + 1) * 512, u * 128:(u + 1) * 128].rearrange("(s p) c -> p s c", p=128),
                    in_=mo[mb][:, :, :]),
                    reads=[("mo", mb)], writes=[("mixd", g)], dma=True)

        for l in range(L):
            layer(l)
        P.add("sp", None, reads=[("outd", t) for t in range(NT)])
        P.emit(nc, block, sems, dsems)
    return nc


def _host_layout(S, inputs):
    w_in = np.asarray(inputs["w_in"], dtype=np.float32)
    w_out = np.asarray(inputs["w_out"], dtype=np.float32)
    L = w_in.shape[0]
    NT = S // 128
    w_in_r = np.empty((L, 8, 128, NCH, 512), np.float32)
    for l in range(L):
        wl = w_in[l].reshape(NCH, 128, 4096)
        for u in range(8):
            base = 0 if u < 4 else 2048
            idx = u % 4
            for k in range(4):
                c0 = base + k * 512 + idx * 128
                w_in_r[l, u, :, :, k * 128:(k + 1) * 128] = wl[:, :, c0:c0 + 128].transpose(1, 0, 2)
    w_in_r = np.ascontiguousarray(w_in_r.reshape(L, 8, 128, NCH * 512))
    w_out_r = np.ascontiguousarray(w_out.reshape(L, NCH, 128, D).transpose(0, 2, 1, 3).reshape(L, 128, NCH * D))
    gcol = np.stack([np.stack([np.tile(np.asarray(inputs[k], np.float32)[l], 2) for k in
                               ("moba_q_norm", "moba_k_norm", "diff_q_norm", "diff_k_norm")], axis=1)
                     for l in range(L)], axis=0).astype(np.float32)
    lamv = np.concatenate([np.asarray(inputs[k], np.float32) for k in
                           ("lambda_q1", "lambda_k1", "lambda_q2", "lambda_k2")], axis=1)
    pos = np.arange(S)
    ident = np.eye(128, dtype=np.float32)
    kk = np.arange(128)[:, None]
    qq = np.arange(128)[None, :]
    cmask = np.where(kk <= qq, 0.0, NEGM).astype(np.float32)
    bones = np.kron(np.eye(2, dtype=np.float32), np.ones((64, 64), np.float32))
    kaug = np.zeros((20, S), np.float32)
    kaug[0:16] = (pos[None, :] // 256 == np.arange(16)[:, None]).astype(np.float32)
    kaug[16] = pos // 128
    kaug[17] = pos % 128
    kaug[18] = 1.0
    kaug[19] = 1.0
    qaug = np.zeros((12, 4, S), np.float32)
    for h, sl in enumerate(MOBA_SLOPES + DIFF_SLOPES):
        qaug[h, 0] = sl * 128.0
        qaug[h, 1] = sl
        qaug[h, 2] = -sl * 128.0 * (pos // 128)
        qaug[h, 3] = -sl * (pos % 128)
    gb = np.zeros((NT, 16), np.float32)
    for t in range(NT):
        own = t // 2
        gb[t, own] = 1e30
        gb[t, own + 1:] = -1e30
    common = {
        "w_in_r": w_in_r, "w_out_r": w_out_r,
        "norm_g": np.ascontiguousarray(np.asarray(inputs["norm_g"], np.float32)),
        "gcol": np.ascontiguousarray(gcol),
        "subln": np.ascontiguousarray(np.asarray(inputs["diff_subln"], np.float32)),
        "lamv": np.ascontiguousarray(lamv),
        "c_ident": ident, "c_cmask": cmask, "c_bones": bones, "c_kaug": kaug, "c_qaug": qaug,
        "c_gb": gb.reshape(1, NT * 16),
    }
    return L, common


def kernel(x, norm_g, w_in, moba_q_norm, moba_k_norm, diff_q_norm, diff_k_norm,
           lambda_q1, lambda_k1, lambda_q2, lambda_k2, diff_subln, w_out):
    inputs = dict(x=x, norm_g=norm_g, w_in=w_in, moba_q_norm=moba_q_norm, moba_k_norm=moba_k_norm,
                  diff_q_norm=diff_q_norm, diff_k_norm=diff_k_norm, lambda_q1=lambda_q1, lambda_k1=lambda_k1,
                  lambda_q2=lambda_q2, lambda_k2=lambda_k2, diff_subln=diff_subln, w_out=w_out)
    x = np.asarray(x, dtype=np.float32)
    B, S, _ = x.shape
    L, common = _host_layout(S, inputs)
    lambda_inits = [0.8 - 0.6 * math.exp(-0.3 * l) for l in range(L)]
    nc = build(S, L, lambda_inits)
    in_maps = [dict(common, x=np.ascontiguousarray(x[b])) for b in range(B)]
    res = run_bass_kernel_spmd(nc, in_maps, core_ids=list(range(B)))
    return np.stack([np.asarray(r["out"], dtype=np.float32) for r in res.results], axis=0)
```

```python
import math
import numpy as np
import concourse.bass as bass
import concourse.mybir as mybir
from concourse.bass_utils import run_bass_kernel_spmd

F32 = mybir.dt.float32
BF16 = mybir.dt.bfloat16
AF = mybir.ActivationFunctionType
ALU = mybir.AluOpType
AX = mybir.AxisListType

D = 1024
NCH = 8
EPS = 1e-6
NEGM = -30000.0
KR = 84
MOBA_SLOPES = [2.0 ** (-8.0 * i / 8) for i in range(1, 9)]
DIFF_SLOPES = [2.0 ** (-8.0 * i / 4) for i in range(1, 5)]
NDSEM = 8
ALIBI_FAR = 60.0


class Prog:
    def __init__(self):
        self.ops = []
        self.last_w = {}
        self.readers = {}
        self.dma_hist = {"sp": [], "pool": []}

    def add(self, eng, fn, reads=(), writes=(), dma=False):
        idx = len(self.ops)
        raw = set()
        deps = set()
        for r in reads:
            if r in self.last_w:
                raw.add(self.last_w[r])
        for w in writes:
            if w in self.last_w:
                deps.add(self.last_w[w])
            for rd in self.readers.get(w, ()):
                deps.add(rd)
        deps |= raw
        keep = set()
        for j in deps:
            oj = self.ops[j]
            if oj["dma"]:
                keep.add(j)
            elif oj["eng"] != eng:
                keep.add(j)
            elif (j in raw) and eng != "pe" and not dma:
                keep.add(j)
            elif dma:
                keep.add(j)
        op = dict(eng=eng, fn=fn, dma=dma, deps=keep, sig=dma, sem=None, val=None)
        if dma:
            h = self.dma_hist[eng]
            n = len(h)
            if n >= NDSEM:
                op["deps"].add(h[n - NDSEM])
            op["slot"] = n % NDSEM
            op["val"] = 16 * (n // NDSEM + 1)
            h.append(idx)
        for w in writes:
            self.last_w[w] = idx
            self.readers[w] = []
        for r in reads:
            self.readers.setdefault(r, []).append(idx)
        self.ops.append(op)
        return idx

    def emit(self, nc, block, sems, dsems):
        ops = self.ops
        for op in ops:
            for j in op["deps"]:
                ops[j]["sig"] = True
        cnt = {e: 0 for e in sems}
        for op in ops:
            if op["dma"]:
                op["sem"] = dsems[op["eng"]][op["slot"]]
            elif op["sig"]:
                cnt[op["eng"]] += 1
                op["sem"] = sems[op["eng"]]
                op["val"] = cnt[op["eng"]]

        def run(engname, eng):
            waited = {}
            for op in ops:
                if op["eng"] != engname:
                    continue
                need = {}
                for j in op["deps"]:
                    oj = ops[j]
                    key = id(oj["sem"])
                    if waited.get(key, 0) >= oj["val"]:
                        continue
                    if key not in need or need[key][1] < oj["val"]:
                        need[key] = (oj["sem"], oj["val"])
                for key, (sem, val) in need.items():
                    eng.wait_ge(sem, val)
                    waited[key] = val
                if op["fn"] is None:
                    continue
                ins = op["fn"](eng)
                if op["dma"]:
                    ins.then_inc(op["sem"], 16)
                elif op["sig"]:
                    ins.then_inc(op["sem"], 1)

        @block.tensor
        def _(e):
            run("pe", e)

        @block.scalar
        def _(e):
            run("act", e)

        @block.vector
        def _(e):
            run("dve", e)

        @block.gpsimd
        def _(e):
            run("pool", e)

        @block.sync
        def _(e):
            run("sp", e)


def build(S, L, lambda_inits):
    NT = S // 128
    NG = S // 512
    NB = S // 256
    assert S % 512 == 0 and NT <= 32
    nc = bass.Bass("TRN2", target_bir_lowering=False)

    def dram(name, shape, dt, kind):
        return nc.dram_tensor(name, list(shape), dt, kind=kind).ap()

    x_d = dram("x", [S, D], F32, "ExternalInput")
    w_in_d = dram("w_in_r", [L, 8, 128, NCH * 512], F32, "ExternalInput")
    w_out_d = dram("w_out_r", [L, 128, NCH * D], F32, "ExternalInput")
    ng_d = dram("norm_g", [L, D], F32, "ExternalInput")
    gcol_d = dram("gcol", [L, 128, 4], F32, "ExternalInput")
    sub_d = dram("subln", [L, 128], F32, "ExternalInput")
    lamv_d = dram("lamv", [L, 256], F32, "ExternalInput")
    ident_d = dram("c_ident", [128, 128], F32, "ExternalInput")
    cm_d = dram("c_cmask", [128, 128], F32, "ExternalInput")
    bones_d = dram("c_bones", [128, 128], F32, "ExternalInput")
    kaug_d = dram("c_kaug", [20, S], F32, "ExternalInput")
    qaug_d = dram("c_qaug", [12, 4, S], F32, "ExternalInput")
    gb_d = dram("c_gb", [1, NT * 16], F32, "ExternalInput")
    out_d = dram("out", [S, D], F32, "ExternalOutput")
    x1_d = dram("x1_scratch", [S, D], F32, "Internal")
    mix_d = dram("mix_scratch", [S, D], BF16, "Internal")

    from contextlib import ExitStack
    es = ExitStack()

    def sb(name, shape, dt):
        return es.enter_context(nc.sbuf_tensor(name, list(shape), dt))

    def pst(name, shape, dt):
        return es.enter_context(nc.psum_tensor(name, list(shape), dt))

    with es:
        hT = sb("hT", [128, NCH, S], BF16)
        xt = [sb(f"xt{i}", [128, D], F32) for i in range(3)]
        hb = [sb(f"hb{i}", [128, D], BF16) for i in range(2)]
        junk = sb("junk", [128, D], BF16)
        junk2 = sb("junk2", [128, D], BF16)
        gbc = sb("gbc", [128, D], F32)
        wbf = sb("wbf", [128, NCH, 512], BF16)
        wout = sb("wout", [128, NCH, D], BF16)
        QT = [sb(f"QT{i}", [128, S], BF16) for i in range(2)]
        KT = [sb(f"KT{i}", [128, S], BF16) for i in range(2)]
        Vt = sb("Vt", [128, NT, 130], BF16)
        Gt = sb("Gt", [128, NT, 128], BF16)
        gtmp = [sb(f"gtmp{i}", [128, 128], F32) for i in range(2)]
        sq = [sb(f"sq{i}", [128, 512], BF16) for i in range(2)]
        rs = [sb(f"rs{i}", [128, 512], F32) for i in range(2)]
        NPT = 4
        PT = [sb(f"PT{i}", [128, 512], BF16) for i in range(NPT)]
        gm = [sb(f"gm{i}", [128, NT, 16], F32) for i in range(2)]
        top8 = sb("top8", [128, NT, 8], F32)
        thr = sb("thr", [128, NT], F32)
        sel = sb("sel", [128, NT, 16], F32)
        selb = [sb(f"selb{i}", [128, NT, 16], BF16) for i in range(2)]
        km = [sb(f"km{i}", [64, 16], F32) for i in range(2)]
        kmb = [sb(f"kmb{i}", [64, 16], BF16) for i in range(2)]
        mo = [sb(f"mo{i}", [128, 4, 128], BF16) for i in range(2)]
        abuf = sb("abuf", [128, 4, 128], F32)
        dbuf = sb("dbuf", [128, 4, 128], F32)
        junkf = sb("junkf", [128, 128], F32)
        small4 = sb("small4", [128, 64], F32)
        small = sb("small", [128, 64], F32)
        mt = [sb(f"mt{i}", [128, D], BF16) for i in range(2)]
        mT = [sb(f"mT{i}", [128, NCH, 128], BF16) for i in range(2)]
        ident = sb("ident", [128, 128], BF16)
        cmask = sb("cmask", [128, 128], BF16)
        bones = sb("bones", [128, 128], BF16)
        gbt = sb("gbt", [128, NT * 16], F32)
        gcol = sb("gcol_s", [128, 4], F32)
        qcol = sb("qcol_s", [128, 4], F32)
        subbc = sb("subbc", [128, 128], F32)
        lamv = sb("lamv_s", [128, 256], F32)
        lamt = sb("lamt", [128, 8], F32)

        ps = [pst(f"ps{i}", [128, 512], F32) for i in range(7)]
        tp = pst("tp", [128, 1024], BF16)

        sems = {e: es.enter_context(nc.semaphore(f"sem_{e}")) for e in ["pe", "act", "dve", "pool", "sp"]}
        dsems = {q: [es.enter_context(nc.semaphore(f"dsem_{q}{i}")) for i in range(NDSEM)] for q in ["sp", "pool"]}
        block = es.enter_context(nc.Block())

        P = Prog()
        cnt = {"s": 0, "pt": 0, "small": 0, "pj": 0, "s4": 0, "tpb": 0}

        def psr(k):
            return ("ps", k)

        def scol():
            c = cnt["small"] % 64
            cnt["small"] += 1
            return c

        P.add("pool", lambda e: e.dma_start(out=ident[:], in_=ident_d[:, :]), writes=["ident"], dma=True)
        P.add("pool", lambda e: e.dma_start(out=cmask[:], in_=cm_d[:, :]), writes=["cmask"], dma=True)
        P.add("pool", lambda e: e.dma_start(out=bones[:], in_=bones_d[:, :]), writes=["bones"], dma=True)
        P.add("sp", lambda e: e.dma_start(out=gbt[:], in_=gb_d[:, :].to_broadcast([128, NT * 16])), writes=["gbt"], dma=True)
        for i in range(2):
            P.add("pool", lambda e, i=i: e.dma_start(out=KT[i][64:84, :], in_=kaug_d[:, :]),
                  writes=[("KTaug", i)], dma=True)
        for i in range(2):
            P.add("dve", lambda e, i=i: e.memset(kmb[i][:], 0.0), writes=[("kmb", i)])

        def layer(l):
            src = x_d if l == 0 else x1_d
            dst = out_d if l == L - 1 else x1_d
            srcn = "xd" if l == 0 else "x1d"
            dstn = "outd" if l == L - 1 else "x1d"
            li = lambda_inits[l]

            P.add("sp", lambda e: e.dma_start(out=gbc[:], in_=ng_d[l:l + 1, :].to_broadcast([128, D])),
                  writes=["gbc"], dma=True)
            P.add("sp", lambda e: e.dma_start(out=gcol[:], in_=gcol_d[l, :, :]), writes=["gcol"], dma=True)
            P.add("sp", lambda e: e.dma_start(out=subbc[:], in_=sub_d[l:l + 1, :].to_broadcast([128, 128])),
                  writes=["subbc"], dma=True)
            P.add("sp", lambda e: e.dma_start(out=lamv[:], in_=lamv_d[l:l + 1, :].to_broadcast([128, 256])),
                  writes=["lamv"], dma=True)
            P.add("pool", lambda e: e.dma_start(out=wout[:].rearrange("p c n -> p (c n)"), in_=w_out_d[l, :, :]),
                  writes=["wout"], dma=True)
            P.add("dve", lambda e: e.tensor_scalar(out=qcol[:], in0=gcol[:], scalar1=0.125, scalar2=None, op0=ALU.mult),
                  reads=["gcol"], writes=["qcol"])
            P.add("dve", lambda e: e.tensor_scalar(out=subbc[:], in0=subbc[:], scalar1=float(1.0 - li), scalar2=None,
                                                   op0=ALU.mult), reads=["subbc"], writes=["subbc"])
            P.add("dve", lambda e: e.tensor_tensor(out=lamv[:, 0:64], in0=lamv[:, 0:64], in1=lamv[:, 64:128], op=ALU.mult),
                  reads=["lamv"], writes=["lamv"])
            P.add("dve", lambda e: e.tensor_tensor(out=lamv[:, 128:192], in0=lamv[:, 128:192], in1=lamv[:, 192:256],
                                                   op=ALU.mult), reads=["lamv"], writes=["lamv"])
            P.add("dve", lambda e: e.tensor_reduce(out=lamt[:, 0:1], in_=lamv[:, 0:64], axis=AX.X, op=ALU.add),
                  reads=["lamv"], writes=["lamt"])
            P.add("dve", lambda e: e.tensor_reduce(out=lamt[:, 1:2], in_=lamv[:, 128:192], axis=AX.X, op=ALU.add),
                  reads=["lamv", "lamt"], writes=["lamt"])
            P.add("act", lambda e: e.activation(out=lamt[:, 2:4], in_=lamt[:, 0:2], func=AF.Exp),
                  reads=["lamt"], writes=["lamt"])
            P.add("dve", lambda e: e.scalar_tensor_tensor(out=lamt[:, 4:5], in0=lamt[:, 3:4], scalar=float(-li),
                                                          in1=lamt[:, 2:3], op0=ALU.add, op1=ALU.subtract),
                  reads=["lamt"], writes=["neglam"])

            cols0 = {}

            def p0_A(t):
                b3 = t % 3
                P.add("sp", lambda e: e.dma_start(out=xt[b3][:], in_=src[t * 128:(t + 1) * 128, :]),
                      reads=[(srcn, t)], writes=[("xt", b3)], dma=True)
                c0 = scol()
                cols0[t] = c0
                jk = junk if t % 2 == 0 else junk2
                P.add("act", lambda e: e.activation(out=jk[:], in_=xt[b3][:], func=AF.Square,
                                                    accum_out=small[:, c0:c0 + 1]),
                      reads=[("xt", b3)], writes=[("junk", t % 2), ("small", c0)])

            def p0_B(t):
                b3 = t % 3
                b = t % 2
                c0 = cols0[t]
                P.add("act", lambda e: e.activation(out=small[:, c0:c0 + 1], in_=small[:, c0:c0 + 1], func=AF.Ln,
                                                    scale=1.0 / D, bias=EPS),
                      reads=[("small", c0)], writes=[("small", c0)])
                P.add("act", lambda e: e.activation(out=small[:, c0:c0 + 1], in_=small[:, c0:c0 + 1], func=AF.Exp,
                                                    scale=-0.5),
                      reads=[("small", c0)], writes=[("small", c0)])
                P.add("dve", lambda e: e.scalar_tensor_tensor(out=hb[b][:], in0=xt[b3][:], scalar=small[:, c0:c0 + 1],
                                                              in1=gbc[:], op0=ALU.mult, op1=ALU.mult),
                      reads=[("xt", b3), ("small", c0), "gbc"], writes=[("hb", b)])

            def p0_B2(t):
                b = t % 2
                for c in range(NCH):
                    P.add("pe", lambda e, c=c: e.transpose(tp[:, c * 128:(c + 1) * 128], hb[b][:, c * 128:(c + 1) * 128],
                                                           ident[:]),
                          reads=[("hb", b), "ident"], writes=["tp"])

            def p0_C(t):
                P.add("dve", lambda e: e.tensor_copy(out=hT[:, :, t * 128:(t + 1) * 128],
                                                     in_=tp[:, :].rearrange("p (c n) -> p c n", n=128)),
                      writes=["tp", ("hT", t // 4)])

            p0_A(0)
            for t in range(NT):
                if t + 1 < NT:
                    p0_A(t + 1)
                p0_B(t)
                if t >= 1:
                    p0_C(t - 1)
                p0_B2(t)
            p0_C(NT - 1)

            for u in range(8):
                unit(l, u, li)

            def p2_load(t):
                b = t % 2
                b3 = t % 3
                P.add("sp", lambda e: e.dma_start(out=mt[b][:], in_=mix_d[t * 128:(t + 1) * 128, :]),
                      reads=[("mixd", t // 4)], writes=[("mt", b)], dma=True)
                P.add("sp", lambda e: e.dma_start(out=xt[b3][:], in_=src[t * 128:(t + 1) * 128, :]),
                      reads=[(srcn, t)], writes=[("xt", b3)], dma=True)

            p2_load(0)
            for t in range(NT):
                b = t % 2
                b3 = t % 3
                if t + 1 < NT:
                    p2_load(t + 1)
                for c in range(NCH):
                    P.add("pe", lambda e, b=b, c=c: e.transpose(tp[:, c * 128:(c + 1) * 128], mt[b][:, c * 128:(c + 1) * 128],
                                                                ident[:]),
                          reads=[("mt", b), "ident"], writes=["tp"])
                P.add("act", lambda e, b=b: e.copy(out=mT[b][:, :, :], in_=tp[:, :].rearrange("p (c n) -> p c n", n=128)),
                      writes=["tp", ("mT", b)])
                for half in range(2):
                    bank = ([6, 0] if t % 2 == 0 else [1, 2])[half]
                    for c in range(NCH):
                        P.add("pe", lambda e, b=b, c=c, half=half, bank=bank: e.matmul(
                            ps[bank][:, :], lhsT=mT[b][:, c, :], rhs=wout[:, c, half * 512:(half + 1) * 512],
                            start=(c == 0), stop=(c == NCH - 1)),
                            reads=[("mT", b), "wout"], writes=[psr(bank)])
                    P.add("dve", lambda e, b3=b3, half=half, bank=bank: e.tensor_tensor(
                        out=xt[b3][:, half * 512:(half + 1) * 512], in0=xt[b3][:, half * 512:(half + 1) * 512],
                        in1=ps[bank][:, :], op=ALU.add),
                        reads=[("xt", b3)], writes=[psr(bank), ("xt", b3)])
                P.add("pool", lambda e, t=t, b3=b3: e.dma_start(out=dst[t * 128:(t + 1) * 128, :], in_=xt[b3][:]),
                      reads=[("xt", b3)], writes=[(dstn, t)], dma=True)

        def unit(l, u, li):
            moba = u < 4
            dvw = 65 if moba else 129
            heads = [2 * u, 2 * u + 1] if moba else [8 + (u - 4), 8 + (u - 4)]
            P.add("pool", lambda e: e.dma_start(out=wbf[:].rearrange("p c n -> p (c n)"), in_=w_in_d[l, u, :, :]),
                  writes=["wbf"], dma=True)
            for i in range(2):
                P.add("pool", lambda e, i=i: e.dma_start(out=QT[i][80:84, :], in_=qaug_d[heads[i], :, :]),
                      writes=[("QTaug", i)], dma=True)
            if moba:
                P.add("dve", lambda e: e.memset(Vt[:, :, 64:65], 1.0), writes=["Vt"])
                P.add("dve", lambda e: e.memset(Vt[:, :, 129:130], 1.0), writes=["Vt"])
            else:
                P.add("dve", lambda e: e.memset(Vt[:, :, 128:129], 1.0), writes=["Vt"])
                if u == 4:
                    for i in range(2):
                        P.add("dve", lambda e, i=i: e.memset(QT[i][64:80, :], 0.0), writes=[("QTsel", i)])

            groups = [(which, g) for which in range(2) for g in range(NG)]
            PJB = [6, 1, 2, 3]
            SSB = [0, 4]

            def emit_proj(n):
                which, g = groups[n]
                pj = PJB[n % 4]
                for c in range(NCH):
                    P.add("pe", lambda e, c=c: e.matmul(
                        ps[pj][:, :], lhsT=wbf[:, c, which * 128:(which + 1) * 128],
                        rhs=hT[:, c, g * 512:(g + 1) * 512], start=(c == 0), stop=(c == NCH - 1)),
                        reads=["wbf", ("hT", g)], writes=[psr(pj)])

            def emit_chainA(n):
                pj = PJB[n % 4]
                b = n % 2
                ssb = SSB[n % 2]
                P.add("act", lambda e: e.activation(out=sq[b][:], in_=ps[pj][:, :], func=AF.Square),
                      writes=[psr(pj), ("sq", b)])
                P.add("pe", lambda e: e.matmul(ps[ssb][:, :], lhsT=bones[:], rhs=sq[b][:], start=True, stop=True),
                      reads=["bones", ("sq", b)], writes=[psr(ssb)])

            def emit_chainB(n):
                which, g = groups[n]
                pj = PJB[n % 4]
                b = n % 2
                ssb = SSB[n % 2]
                T = QT if which == 0 else KT
                tname = "QT" if which == 0 else "KT"
                colt = qcol if which == 0 else gcol
                cidx = (0 if moba else 2) + which
                P.add("act", lambda e: e.activation(out=rs[b][:], in_=ps[ssb][:, :], func=AF.Ln, scale=1.0 / 64, bias=EPS),
                      writes=[psr(ssb), ("rs", b)])
                P.add("act", lambda e: e.activation(out=rs[b][:], in_=rs[b][:], func=AF.Exp, scale=-0.5),
                      reads=[("rs", b)], writes=[("rs", b)])
                for i in range(2):
                    if moba and which == 1:
                        for hh in range(2):
                            P.add("dve", lambda e, i=i, hh=hh: e.scalar_tensor_tensor(
                                out=T[i][0:64, g * 512 + hh * 256:g * 512 + (hh + 1) * 256],
                                in0=ps[pj][i * 64:(i + 1) * 64, hh * 256:(hh + 1) * 256],
                                scalar=colt[i * 64:(i + 1) * 64, cidx:cidx + 1],
                                in1=rs[b][i * 64:(i + 1) * 64, hh * 256:(hh + 1) * 256],
                                op0=ALU.mult, op1=ALU.mult, accum_out=km[i][:, 2 * g + hh:2 * g + hh + 1]),
                                reads=[("rs", b), "qcol", "gcol"], writes=[psr(pj), (tname, i, g), ("km", i)])
                    else:
                        P.add("dve", lambda e, i=i: e.scalar_tensor_tensor(
                            out=T[i][0:64, g * 512:(g + 1) * 512], in0=ps[pj][i * 64:(i + 1) * 64, :],
                            scalar=colt[i * 64:(i + 1) * 64, cidx:cidx + 1], in1=rs[b][i * 64:(i + 1) * 64, :],
                            op0=ALU.mult, op1=ALU.mult),
                            reads=[("rs", b), "qcol", "gcol"], writes=[psr(pj), (tname, i, g)])

            NGR = len(groups)
            emit_proj(0)
            if NGR > 1:
                emit_proj(1)
            emit_chainA(0)
            for n in range(NGR):
                if n + 2 < NGR:
                    emit_proj(n + 2)
                if n + 1 < NGR:
                    emit_chainA(n + 1)
                emit_chainB(n)

            side = []

            def sel_stage1(i):
                P.add("dve", lambda e: e.tensor_scalar(out=kmb[i][:, 0:NB], in0=km[i][:, 0:NB], scalar1=1.0 / 256,
                                                       scalar2=None, op0=ALU.mult),
                      reads=[("km", i)], writes=[("kmb", i)])

            def sel_gate(i):
                gbank = 6 if i == 0 else 0
                for t in range(NT):
                    P.add("pe", lambda e, t=t: e.matmul(ps[gbank][:, t * 16:(t + 1) * 16],
                                                        lhsT=QT[i][0:64, t * 128:(t + 1) * 128], rhs=kmb[i][:, :],
                                                        start=True, stop=True),
                          reads=[("QT", i, t // 4), ("kmb", i)], writes=[psr(gbank)])
                P.add("dve", lambda e: e.tensor_tensor(out=gm[i][:].rearrange("p t n -> p (t n)"),
                                                       in0=ps[gbank][:, 0:NT * 16], in1=gbt[:], op=ALU.add),
                      reads=["gbt"], writes=[psr(gbank), ("gm", i)])

            def sel_chain(i):
                th = []
                for t in range(NT):
                    th.append(lambda t=t: P.add("dve", lambda e: e.max(out=top8[:, t, :], in_=gm[i][:, t, :]),
                                                reads=[("gm", i)], writes=["top8"]))
                th.append(lambda: P.add("dve", lambda e: e.tensor_scalar(out=thr[:], in0=top8[:, :, 3], scalar1=-1e29,
                                                                         scalar2=None, op0=ALU.max),
                                        reads=["top8"], writes=["thr"]))
                th.append(lambda: P.add("dve", lambda e: e.tensor_tensor(
                    out=sel[:], in0=gm[i][:], in1=thr[:].unsqueeze(2).to_broadcast([128, NT, 16]), op=ALU.is_ge),
                    reads=[("gm", i), "thr"], writes=["sel"]))
                th.append(lambda: P.add("dve", lambda e: e.tensor_scalar(out=selb[i][:], in0=sel[:], scalar1=-NEGM,
                                                                         scalar2=NEGM, op0=ALU.mult, op1=ALU.add),
                                        reads=["sel"], writes=[("selb", i)]))
                return th

            def sel_final_thunks(i):
                th = []
                for k, t0 in enumerate(range(0, NT, 8)):
                    def f(k=k, t0=t0):
                        w = cnt["tpb"] % 3
                        cnt["tpb"] += 1
                        if w == 0:
                            buf = tp[0:16, :]
                            res = "tp"
                        else:
                            buf = ps[3 + w][0:16, :].bitcast(BF16)
                            res = psr(3 + w)
                        for t in range(t0, t0 + 8):
                            P.add("pe", lambda e, t=t: e.transpose(buf[:, (t - t0) * 128:(t - t0 + 1) * 128],
                                                                   selb[i][:, t, :], ident[:]),
                                  reads=[("selb", i), "ident"], writes=[res])
                        if k % 2 == 0:
                            P.add("act", lambda e: e.copy(out=QT[i][64:80, t0 * 128:(t0 + 8) * 128], in_=buf),
                                  writes=[res, ("QTsel", i)])
                        else:
                            P.add("dve", lambda e: e.tensor_copy(out=QT[i][64:80, t0 * 128:(t0 + 8) * 128], in_=buf),
                                  writes=[res, ("QTsel", i)])
                    th.append(f)
                return th

            if moba:
                for i in range(2):
                    sel_stage1(i)

            for t in range(NT):
                bank = 2 + (t % 2)
                b = t % 2
                if moba and t == min(4, NT - 1):
                    for i in range(2):
                        sel_gate(i)
                        side.extend(sel_chain(i))
                        side.extend(sel_final_thunks(i))
                for c in range(NCH):
                    P.add("pe", lambda e, c=c, t=t, bank=bank: e.matmul(
                        ps[bank][:, 0:256], lhsT=hT[:, c, t * 128:(t + 1) * 128], rhs=wbf[:, c, 256:512],
                        start=(c == 0), stop=(c == NCH - 1)),
                        reads=["wbf", ("hT", t // 4)], writes=[psr(bank)])
                if moba:
                    P.add("dve", lambda e, t=t, bank=bank: e.tensor_copy(
                        out=Vt[:, t, :].rearrange("p (i c) -> p i c", c=65)[:, :, 0:64],
                        in_=ps[bank][:, 0:128].rearrange("p (i c) -> p i c", c=64)),
                        writes=[psr(bank), "Vt"])
                    P.add("act", lambda e, t=t, bank=bank: e.activation(out=Gt[:, t, :], in_=ps[bank][:, 128:256], func=AF.Silu),
                          writes=[psr(bank), "Gt"])
                else:
                    P.add("dve", lambda e, t=t, bank=bank: e.tensor_copy(out=Vt[:, t, 0:128], in_=ps[bank][:, 0:128]),
                          writes=[psr(bank), "Vt"])
                    P.add("act", lambda e, b=b, bank=bank: e.activation(out=gtmp[b][:], in_=ps[bank][:, 128:256], func=AF.Silu),
                          writes=[psr(bank), ("gtmp", b)])
                    P.add("dve", lambda e, t=t, b=b: e.tensor_tensor(out=Gt[:, t, :], in0=gtmp[b][:], in1=subbc[:], op=ALU.mult),
                          reads=[("gtmp", b), "subbc"], writes=["Gt"])
                for _ in range(4):
                    if side:
                        side.pop(0)()
            while side:
                side.pop(0)()

            slopes_all = MOBA_SLOPES + DIFF_SLOPES
            iters = []
            for g in range(NG):
                for i in range(2):
                    sl = slopes_all[heads[i]]
                    kt_first = 0
                    while kt_first < 4 * g and sl * (g * 512 - (kt_first * 128 + 127)) > ALIBI_FAR:
                        kt_first += 1
                    for kt in range(kt_first, 4 * (g + 1)):
                        iters.append(dict(g=g, i=i, kt=kt, first=(kt == kt_first), last=(kt == 4 * g + 3)))
            SB = [0, 1, 6]
            PIPE = 2

            def emit_S(n):
                it = iters[n]
                g, i, kt = it["g"], it["i"], it["kt"]
                j = kt - 4 * g
                s0 = max(0, j)
                sbk = SB[n % 3]
                P.add("pe", lambda e: e.matmul(
                    ps[sbk][:, s0 * 128:512], lhsT=KT[i][0:KR, kt * 128:(kt + 1) * 128],
                    rhs=QT[i][0:KR, g * 512 + s0 * 128:(g + 1) * 512], start=True, stop=(j < 0)),
                    reads=[("KT", i, kt // 4), ("KTaug", i), ("QT", i, g), ("QTaug", i), ("QTsel", i)],
                    writes=[psr(sbk)])
                if j >= 0:
                    P.add("pe", lambda e: e.matmul(ps[sbk][:, j * 128:(j + 1) * 128], lhsT=ident[:],
                                                   rhs=cmask[:], start=False, stop=True),
                          reads=["ident", "cmask"], writes=[psr(sbk)])

            def emit_rest(n):
                it = iters[n]
                g, i, kt = it["g"], it["i"], it["kt"]
                j = kt - 4 * g
                s0 = max(0, j)
                sbk = SB[n % 3]
                slot = n % NPT
                oset = (g * 2 + i) % 2
                ob = [2 + 2 * oset, 3 + 2 * oset]
                P.add("act", lambda e: e.activation(
                    out=PT[slot][:, s0 * 128:512], in_=ps[sbk][:, s0 * 128:512], func=AF.Exp),
                    writes=[psr(sbk), ("PT", slot)])
                for s in range(s0, 4):
                    bank = ob[s // 2]
                    col = (s % 2) * dvw
                    vlo = i * 65 if moba else 0
                    P.add("pe", lambda e, s=s, bank=bank, col=col, vlo=vlo: e.matmul(
                        ps[bank][:, col:col + dvw], lhsT=PT[slot][:, s * 128:(s + 1) * 128],
                        rhs=Vt[:, kt, vlo:vlo + dvw], start=(it["first"] and s % 2 == 0), stop=(kt == 4 * g + s),
                        skip_group_check=True),
                        reads=[("PT", slot), "Vt"], writes=[psr(bank)])
                if it["last"]:
                    pend.append((n + EPI_DELAY, (u, g, i, ob, dvw, moba)))

            NI = len(iters)
            pend = []
            EPI_DELAY = 2
            for n in range(min(PIPE, NI)):
                emit_S(n)
            for n in range(NI):
                if n + PIPE < NI:
                    emit_S(n + PIPE)
                emit_rest(n)
                while pend and pend[0][0] <= n:
                    epilogue(*pend.pop(0)[1])
            while pend:
                epilogue(*pend.pop(0)[1])

        def epilogue(u, g, i, ob, dvw, moba):
            mb = g % 2
            if (not moba) and i == 1:
                k4 = cnt["s4"] % 16
                cnt["s4"] += 1
                c4 = 4 * k4
            for s in range(4):
                t = 4 * g + s
                bank = ob[s // 2]
                col = (s % 2) * dvw
                c0 = scol()
                if moba:
                    P.add("dve", lambda e, bank=bank, col=col, c0=c0: e.reciprocal(out=small[:, c0:c0 + 1],
                                                                                   in_=ps[bank][:, col + 64:col + 65]),
                          writes=[psr(bank), ("small", c0)])
                    P.add("dve", lambda e, bank=bank, col=col, c0=c0, s=s, t=t, mb=mb, i=i: e.scalar_tensor_tensor(
                        out=mo[mb][:, s, i * 64:(i + 1) * 64], in0=ps[bank][:, col:col + 64], scalar=small[:, c0:c0 + 1],
                        in1=Gt[:, t, i * 64:(i + 1) * 64], op0=ALU.mult, op1=ALU.mult),
                        reads=[("small", c0), "Gt"], writes=[psr(bank), ("mo", mb)])
                elif i == 0:
                    P.add("dve", lambda e, bank=bank, col=col, c0=c0: e.reciprocal(out=small[:, c0:c0 + 1],
                                                                                   in_=ps[bank][:, col + 128:col + 129]),
                          writes=[psr(bank), ("small", c0)])
                    P.add("dve", lambda e, bank=bank, col=col, c0=c0, s=s: e.tensor_scalar(
                        out=abuf[:, s, :], in0=ps[bank][:, col:col + 128], scalar1=small[:, c0:c0 + 1], scalar2=None,
                        op0=ALU.mult),
                        reads=[("small", c0)], writes=[psr(bank), ("abuf", s)])
                else:
                    P.add("dve", lambda e, bank=bank, col=col, c0=c0: e.reciprocal(out=small[:, c0:c0 + 1],
                                                                                   in_=ps[bank][:, col + 128:col + 129]),
                          writes=[psr(bank), ("small", c0)])
                    P.add("dve", lambda e, c0=c0: e.tensor_tensor(out=small[:, c0:c0 + 1], in0=small[:, c0:c0 + 1],
                                                                 in1=lamt[:, 4:5], op=ALU.mult),
                          reads=[("small", c0), "neglam"], writes=[("small", c0)])
                    P.add("dve", lambda e, bank=bank, col=col, c0=c0, s=s: e.scalar_tensor_tensor(
                        out=dbuf[:, s, :], in0=ps[bank][:, col:col + 128], scalar=small[:, c0:c0 + 1], in1=abuf[:, s, :],
                        op0=ALU.mult, op1=ALU.add),
                        reads=[("small", c0), ("abuf", s)], writes=[psr(bank), ("dbuf", s)])
                    P.add("dve", lambda e, s=s, c4=c4: e.scalar_tensor_tensor(
                        out=junkf[:], in0=dbuf[:, s, :], scalar=1.0, in1=dbuf[:, s, :], op0=ALU.mult, op1=ALU.mult,
                        accum_out=small4[:, c4 + s:c4 + s + 1]),
                        reads=[("dbuf", s)], writes=["junkf", ("small4", c4)])
            if (not moba) and i == 1:
                P.add("act", lambda e, c4=c4: e.activation(out=small4[:, c4:c4 + 4], in_=small4[:, c4:c4 + 4], func=AF.Ln,
                                                          scale=1.0 / 128, bias=EPS),
                      reads=[("small4", c4)], writes=[("small4", c4)])
                P.add("act", lambda e, c4=c4: e.activation(out=small4[:, c4:c4 + 4], in_=small4[:, c4:c4 + 4], func=AF.Exp,
                                                          scale=-0.5),
                      reads=[("small4", c4)], writes=[("small4", c4)])
                for s in range(4):
                    t = 4 * g + s
                    P.add("dve", lambda e, s=s, t=t, mb=mb, c4=c4: e.scalar_tensor_tensor(
                        out=mo[mb][:, s, :], in0=dbuf[:, s, :], scalar=small4[:, c4 + s:c4 + s + 1], in1=Gt[:, t, :],
                        op0=ALU.mult, op1=ALU.mult),
                        reads=[("dbuf", s), ("small4", c4), "Gt"], writes=[("mo", mb)])
            if i == 1:
                P.add("sp", lambda e, g=g, mb=mb, u=u: e.dma_start(
                    out=mix_d[g * 512:(g + 1) * 512, u * 128:(u + 1) * 128].rearrange("(s p) c -> p s c", p=128),
                    in_=mo[mb][:, :, :]),
                    reads=[("mo", mb)], writes=[("mixd", g)], dma=True)

        for l in range(L):
            layer(l)
        P.add("sp", None, reads=[("outd", t) for t in range(NT)])
        print('sbuf bytes remaining', nc.sbuf_bytes_remaining)
        P.emit(nc, block, sems, dsems)
    return nc


def _host_layout(S, inputs):
    w_in = np.asarray(inputs["w_in"], dtype=np.float32)
    w_out = np.asarray(inputs["w_out"], dtype=np.float32)
    L = w_in.shape[0]
    NT = S // 128
    w_in_r = np.empty((L, 8, 128, NCH, 512), np.float32)
    for l in range(L):
        wl = w_in[l].reshape(NCH, 128, 4096)
        for u in range(8):
            base = 0 if u < 4 else 2048
            idx = u % 4
            for k in range(4):
                c0 = base + k * 512 + idx * 128
                w_in_r[l, u, :, :, k * 128:(k + 1) * 128] = wl[:, :, c0:c0 + 128].transpose(1, 0, 2)
    w_in_r = np.ascontiguousarray(w_in_r.reshape(L, 8, 128, NCH * 512))
    w_out_r = np.ascontiguousarray(w_out.reshape(L, NCH, 128, D).transpose(0, 2, 1, 3).reshape(L, 128, NCH * D))
    gcol = np.stack([np.stack([np.tile(np.asarray(inputs[k], np.float32)[l], 2) for k in
                               ("moba_q_norm", "moba_k_norm", "diff_q_norm", "diff_k_norm")], axis=1)
                     for l in range(L)], axis=0).astype(np.float32)
    lamv = np.concatenate([np.asarray(inputs[k], np.float32) for k in
                           ("lambda_q1", "lambda_k1", "lambda_q2", "lambda_k2")], axis=1)
    pos = np.arange(S)
    ident = np.eye(128, dtype=np.float32)
    kk = np.arange(128)[:, None]
    qq = np.arange(128)[None, :]
    cmask = np.where(kk <= qq, 0.0, NEGM).astype(np.float32)
    bones = np.kron(np.eye(2, dtype=np.float32), np.ones((64, 64), np.float32))
    kaug = np.zeros((20, S), np.float32)
    kaug[0:16] = (pos[None, :] // 256 == np.arange(16)[:, None]).astype(np.float32)
    kaug[16] = pos // 128
    kaug[17] = pos % 128
    kaug[18] = 1.0
    kaug[19] = 1.0
    qaug = np.zeros((12, 4, S), np.float32)
    for h, sl in enumerate(MOBA_SLOPES + DIFF_SLOPES):
        qaug[h, 0] = sl * 128.0
        qaug[h, 1] = sl
        qaug[h, 2] = -sl * 128.0 * (pos // 128)
        qaug[h, 3] = -sl * (pos % 128)
    gb = np.zeros((NT, 16), np.float32)
    for t in range(NT):
        own = t // 2
        gb[t, own] = 1e30
        gb[t, own + 1:] = -1e30
    common = {
        "w_in_r": w_in_r, "w_out_r": w_out_r,
        "norm_g": np.ascontiguousarray(np.asarray(inputs["norm_g"], np.float32)),
        "gcol": np.ascontiguousarray(gcol),
        "subln": np.ascontiguousarray(np.asarray(inputs["diff_subln"], np.float32)),
        "lamv": np.ascontiguousarray(lamv),
        "c_ident": ident, "c_cmask": cmask, "c_bones": bones, "c_kaug": kaug, "c_qaug": qaug,
        "c_gb": gb.reshape(1, NT * 16),
    }
    return L, common


def kernel(x, norm_g, w_in, moba_q_norm, moba_k_norm, diff_q_norm, diff_k_norm,
           lambda_q1, lambda_k1, lambda_q2, lambda_k2, diff_subln, w_out):
    inputs = dict(x=x, norm_g=norm_g, w_in=w_in, moba_q_norm=moba_q_norm, moba_k_norm=moba_k_norm,
                  diff_q_norm=diff_q_norm, diff_k_norm=diff_k_norm, lambda_q1=lambda_q1, lambda_k1=lambda_k1,
                  lambda_q2=lambda_q2, lambda_k2=lambda_k2, diff_subln=diff_subln, w_out=w_out)
    x = np.asarray(x, dtype=np.float32)
    B, S, _ = x.shape
    L, common = _host_layout(S, inputs)
    lambda_inits = [0.8 - 0.6 * math.exp(-0.3 * l) for l in range(L)]
    nc = build(S, L, lambda_inits)
    in_maps = [dict(common, x=np.ascontiguousarray(x[b])) for b in range(B)]
    res = run_bass_kernel_spmd(nc, in_maps, core_ids=list(range(B)))
    return np.stack([np.asarray(r["out"], dtype=np.float32) for r in res.results], axis=0)
```

```python
import math
import numpy as np
import concourse.bass as bass
import concourse.mybir as mybir
from concourse.bass_utils import run_bass_kernel_spmd

F32 = mybir.dt.float32
BF16 = mybir.dt.bfloat16
AF = mybir.ActivationFunctionType
ALU = mybir.AluOpType
AX = mybir.AxisListType

D = 1024
NCH = 8
EPS = 1e-6
NEGM = -30000.0
KR = 84
MOBA_SLOPES = [2.0 ** (-8.0 * i / 8) for i in range(1, 9)]
DIFF_SLOPES = [2.0 ** (-8.0 * i / 4) for i in range(1, 5)]
NDSEM = 8
ALIBI_FAR = 60.0


class Prog:
    def __init__(self):
        self.ops = []
        self.last_w = {}
        self.readers = {}
        self.dma_hist = {"sp": [], "pool": []}

    def add(self, eng, fn, reads=(), writes=(), dma=False):
        idx = len(self.ops)
        raw = set()
        deps = set()
        for r in reads:
            if r in self.last_w:
                raw.add(self.last_w[r])
        for w in writes:
            if w in self.last_w:
                deps.add(self.last_w[w])
            for rd in self.readers.get(w, ()):
                deps.add(rd)
        deps |= raw
        keep = set()
        for j in deps:
            oj = self.ops[j]
            if oj["dma"]:
                keep.add(j)
            elif oj["eng"] != eng:
                keep.add(j)
            elif (j in raw) and eng != "pe" and not dma:
                keep.add(j)
            elif dma:
                keep.add(j)
        op = dict(eng=eng, fn=fn, dma=dma, deps=keep, sig=dma, sem=None, val=None)
        if dma:
            h = self.dma_hist[eng]
            n = len(h)
            if n >= NDSEM:
                op["deps"].add(h[n - NDSEM])
            op["slot"] = n % NDSEM
            op["val"] = 16 * (n // NDSEM + 1)
            h.append(idx)
        for w in writes:
            self.last_w[w] = idx
            self.readers[w] = []
        for r in reads:
            self.readers.setdefault(r, []).append(idx)
        self.ops.append(op)
        return idx

    def emit(self, nc, block, sems, dsems):
        ops = self.ops
        for op in ops:
            for j in op["deps"]:
                ops[j]["sig"] = True
        cnt = {e: 0 for e in sems}
        for op in ops:
            if op["dma"]:
                op["sem"] = dsems[op["eng"]][op["slot"]]
            elif op["sig"]:
                cnt[op["eng"]] += 1
                op["sem"] = sems[op["eng"]]
                op["val"] = cnt[op["eng"]]

        def run(engname, eng):
            waited = {}
            for op in ops:
                if op["eng"] != engname:
                    continue
                need = {}
                for j in op["deps"]:
                    oj = ops[j]
                    key = id(oj["sem"])
                    if waited.get(key, 0) >= oj["val"]:
                        continue
                    if key not in need or need[key][1] < oj["val"]:
                        need[key] = (oj["sem"], oj["val"])
                for key, (sem, val) in need.items():
                    eng.wait_ge(sem, val)
                    waited[key] = val
                if op["fn"] is None:
                    continue
                ins = op["fn"](eng)
                if op["dma"]:
                    ins.then_inc(op["sem"], 16)
                elif op["sig"]:
                    ins.then_inc(op["sem"], 1)

        @block.tensor
        def _(e):
            run("pe", e)

        @block.scalar
        def _(e):
            run("act", e)

        @block.vector
        def _(e):
            run("dve", e)

        @block.gpsimd
        def _(e):
            run("pool", e)

        @block.sync
        def _(e):
            run("sp", e)


def build(S, L, lambda_inits):
    NT = S // 128
    NG = S // 512
    NB = S // 256
    assert S % 512 == 0 and NT <= 32
    nc = bass.Bass("TRN2", target_bir_lowering=False)

    def dram(name, shape, dt, kind):
        return nc.dram_tensor(name, list(shape), dt, kind=kind).ap()

    x_d = dram("x", [S, D], F32, "ExternalInput")
    w_in_d = dram("w_in_r", [L, 8, 128, NCH * 512], F32, "ExternalInput")
    w_out_d = dram("w_out_r", [L, 128, NCH * D], F32, "ExternalInput")
    ng_d = dram("norm_g", [L, D], F32, "ExternalInput")
    gcol_d = dram("gcol", [L, 128, 4], F32, "ExternalInput")
    sub_d = dram("subln", [L, 128], F32, "ExternalInput")
    lamv_d = dram("lamv", [L, 256], F32, "ExternalInput")
    ident_d = dram("c_ident", [128, 128], F32, "ExternalInput")
    cm_d = dram("c_cmask", [128, 128], F32, "ExternalInput")
    bones_d = dram("c_bones", [128, 128], F32, "ExternalInput")
    kaug_d = dram("c_kaug", [20, S], F32, "ExternalInput")
    qaug_d = dram("c_qaug", [12, 4, S], F32, "ExternalInput")
    gb_d = dram("c_gb", [1, NT * 16], F32, "ExternalInput")
    out_d = dram("out", [S, D], F32, "ExternalOutput")
    x1_d = dram("x1_scratch", [S, D], F32, "Internal")
    mix_d = dram("mix_scratch", [S, D], BF16, "Internal")

    from contextlib import ExitStack
    es = ExitStack()

    def sb(name, shape, dt):
        return es.enter_context(nc.sbuf_tensor(name, list(shape), dt))

    def pst(name, shape, dt):
        return es.enter_context(nc.psum_tensor(name, list(shape), dt))

    with es:
        hT = sb("hT", [128, NCH, S], BF16)
        xt = [sb(f"xt{i}", [128, D], F32) for i in range(3)]
        hb = [sb(f"hb{i}", [128, D], BF16) for i in range(2)]
        junk = sb("junk", [128, D], BF16)
        junk2 = sb("junk2", [128, D], BF16)
        gbc = sb("gbc", [128, D], F32)
        wbf = sb("wbf", [128, NCH, 512], BF16)
        wout = sb("wout", [128, NCH, D], BF16)
        QT = [sb(f"QT{i}", [128, S], BF16) for i in range(2)]
        KT = [sb(f"KT{i}", [128, S], BF16) for i in range(2)]
        Vt = sb("Vt", [128, NT, 130], BF16)
        Gt = sb("Gt", [128, NT, 128], BF16)
        gtmp = [sb(f"gtmp{i}", [128, 128], F32) for i in range(2)]
        sq = [sb(f"sq{i}", [128, 512], BF16) for i in range(2)]
        rs = [sb(f"rs{i}", [128, 512], F32) for i in range(2)]
        NPT = 4
        PT = [sb(f"PT{i}", [128, 512], BF16) for i in range(NPT)]
        gm = [sb(f"gm{i}", [128, NT, 16], F32) for i in range(2)]
        top8 = sb("top8", [128, NT, 8], F32)
        thr = sb("thr", [128, NT], F32)
        sel = sb("sel", [128, NT, 16], F32)
        selb = [sb(f"selb{i}", [128, NT, 16], BF16) for i in range(2)]
        km = [sb(f"km{i}", [64, 16], F32) for i in range(2)]
        kmb = [sb(f"kmb{i}", [64, 16], BF16) for i in range(2)]
        mo = [sb(f"mo{i}", [128, 4, 128], BF16) for i in range(2)]
        abuf = sb("abuf", [128, 4, 128], F32)
        dbuf = sb("dbuf", [128, 4, 128], F32)
        junkf = sb("junkf", [128, 128], F32)
        small4 = sb("small4", [128, 64], F32)
        small = sb("small", [128, 64], F32)
        mt = [sb(f"mt{i}", [128, D], BF16) for i in range(2)]
        mT = [sb(f"mT{i}", [128, NCH, 128], BF16) for i in range(2)]
        ident = sb("ident", [128, 128], BF16)
        cmask = sb("cmask", [128, 128], BF16)
        bones = sb("bones", [128, 128], BF16)
        gbt = sb("gbt", [128, NT * 16], F32)
        gcol = sb("gcol_s", [128, 4], F32)
        qcol = sb("qcol_s", [128, 4], F32)
        subbc = sb("subbc", [128, 128], F32)
        lamv = sb("lamv_s", [128, 256], F32)
        lamt = sb("lamt", [128, 8], F32)

        ps = [pst(f"ps{i}", [128, 512], F32) for i in range(7)]
        tp = pst("tp", [128, 1024], BF16)

        sems = {e: es.enter_context(nc.semaphore(f"sem_{e}")) for e in ["pe", "act", "dve", "pool", "sp"]}
        dsems = {q: [es.enter_context(nc.semaphore(f"dsem_{q}{i}")) for i in range(NDSEM)] for q in ["sp", "pool"]}
        block = es.enter_context(nc.Block())

        P = Prog()
        cnt = {"s": 0, "pt": 0, "small": 0, "pj": 0, "s4": 0, "tpb": 0}

        def psr(k):
            return ("ps", k)

        def scol():
            c = cnt["small"] % 64
            cnt["small"] += 1
            return c

        P.add("pool", lambda e: e.dma_start(out=ident[:], in_=ident_d[:, :]), writes=["ident"], dma=True)
        P.add("pool", lambda e: e.dma_start(out=cmask[:], in_=cm_d[:, :]), writes=["cmask"], dma=True)
        P.add("pool", lambda e: e.dma_start(out=bones[:], in_=bones_d[:, :]), writes=["bones"], dma=True)
        P.add("sp", lambda e: e.dma_start(out=gbt[:], in_=gb_d[:, :].to_broadcast([128, NT * 16])), writes=["gbt"], dma=True)
        for i in range(2):
            P.add("pool", lambda e, i=i: e.dma_start(out=KT[i][64:84, :], in_=kaug_d[:, :]),
                  writes=[("KTaug", i)], dma=True)
        for i in range(2):
            P.add("dve", lambda e, i=i: e.memset(kmb[i][:], 0.0), writes=[("kmb", i)])

        def layer(l):
            src = x_d if l == 0 else x1_d
            dst = out_d if l == L - 1 else x1_d
            srcn = "xd" if l == 0 else "x1d"
            dstn = "outd" if l == L - 1 else "x1d"
            li = lambda_inits[l]

            P.add("sp", lambda e: e.dma_start(out=gbc[:], in_=ng_d[l:l + 1, :].to_broadcast([128, D])),
                  writes=["gbc"], dma=True)
            P.add("sp", lambda e: e.dma_start(out=gcol[:], in_=gcol_d[l, :, :]), writes=["gcol"], dma=True)
            P.add("sp", lambda e: e.dma_start(out=subbc[:], in_=sub_d[l:l + 1, :].to_broadcast([128, 128])),
                  writes=["subbc"], dma=True)
            P.add("sp", lambda e: e.dma_start(out=lamv[:], in_=lamv_d[l:l + 1, :].to_broadcast([128, 256])),
                  writes=["lamv"], dma=True)
            P.add("pool", lambda e: e.dma_start(out=wout[:].rearrange("p c n -> p (c n)"), in_=w_out_d[l, :, :]),
                  writes=["wout"], dma=True)
            P.add("dve", lambda e: e.tensor_scalar(out=qcol[:], in0=gcol[:], scalar1=0.125, scalar2=None, op0=ALU.mult),
                  reads=["gcol"], writes=["qcol"])
            P.add("dve", lambda e: e.tensor_scalar(out=subbc[:], in0=subbc[:], scalar1=float(1.0 - li), scalar2=None,
                                                   op0=ALU.mult), reads=["subbc"], writes=["subbc"])
            P.add("dve", lambda e: e.tensor_tensor(out=lamv[:, 0:64], in0=lamv[:, 0:64], in1=lamv[:, 64:128], op=ALU.mult),
                  reads=["lamv"], writes=["lamv"])
            P.add("dve", lambda e: e.tensor_tensor(out=lamv[:, 128:192], in0=lamv[:, 128:192], in1=lamv[:, 192:256],
                                                   op=ALU.mult), reads=["lamv"], writes=["lamv"])
            P.add("dve", lambda e: e.tensor_reduce(out=lamt[:, 0:1], in_=lamv[:, 0:64], axis=AX.X, op=ALU.add),
                  reads=["lamv"], writes=["lamt"])
            P.add("dve", lambda e: e.tensor_reduce(out=lamt[:, 1:2], in_=lamv[:, 128:192], axis=AX.X, op=ALU.add),
                  reads=["lamv", "lamt"], writes=["lamt"])
            P.add("act", lambda e: e.activation(out=lamt[:, 2:4], in_=lamt[:, 0:2], func=AF.Exp),
                  reads=["lamt"], writes=["lamt"])
            P.add("dve", lambda e: e.scalar_tensor_tensor(out=lamt[:, 4:5], in0=lamt[:, 3:4], scalar=float(-li),
                                                          in1=lamt[:, 2:3], op0=ALU.add, op1=ALU.subtract),
                  reads=["lamt"], writes=["neglam"])

            cols0 = {}

            def p0_A(t):
                b3 = t % 3
                P.add("sp", lambda e: e.dma_start(out=xt[b3][:], in_=src[t * 128:(t + 1) * 128, :]),
                      reads=[(srcn, t)], writes=[("xt", b3)], dma=True)
                c0 = scol()
                cols0[t] = c0
                jk = junk if t % 2 == 0 else junk2
                P.add("act", lambda e: e.activation(out=jk[:], in_=xt[b3][:], func=AF.Square,
                                                    accum_out=small[:, c0:c0 + 1]),
                      reads=[("xt", b3)], writes=[("junk", t % 2), ("small", c0)])

            def p0_B(t):
                b3 = t % 3
                b = t % 2
                c0 = cols0[t]
                P.add("act", lambda e: e.activation(out=small[:, c0:c0 + 1], in_=small[:, c0:c0 + 1], func=AF.Ln,
                                                    scale=1.0 / D, bias=EPS),
                      reads=[("small", c0)], writes=[("small", c0)])
                P.add("act", lambda e: e.activation(out=small[:, c0:c0 + 1], in_=small[:, c0:c0 + 1], func=AF.Exp,
                                                    scale=-0.5),
                      reads=[("small", c0)], writes=[("small", c0)])
                P.add("dve", lambda e: e.scalar_tensor_tensor(out=hb[b][:], in0=xt[b3][:], scalar=small[:, c0:c0 + 1],
                                                              in1=gbc[:], op0=ALU.mult, op1=ALU.mult),
                      reads=[("xt", b3), ("small", c0), "gbc"], writes=[("hb", b)])

            def p0_B2(t):
                b = t % 2
                for c in range(NCH):
                    P.add("pe", lambda e, c=c: e.transpose(tp[:, c * 128:(c + 1) * 128], hb[b][:, c * 128:(c + 1) * 128],
                                                           ident[:]),
                          reads=[("hb", b), "ident"], writes=["tp"])

            def p0_C(t):
                P.add("dve", lambda e: e.tensor_copy(out=hT[:, :, t * 128:(t + 1) * 128],
                                                     in_=tp[:, :].rearrange("p (c n) -> p c n", n=128)),
                      writes=["tp", ("hT", t // 4)])

            p0_A(0)
            for t in range(NT):
                if t + 1 < NT:
                    p0_A(t + 1)
                p0_B(t)
                if t >= 1:
                    p0_C(t - 1)
                p0_B2(t)
            p0_C(NT - 1)

            for u in range(8):
                unit(l, u, li)

            def p2_load(t):
                b = t % 2
                b3 = t % 3
                P.add("sp", lambda e: e.dma_start(out=mt[b][:], in_=mix_d[t * 128:(t + 1) * 128, :]),
                      reads=[("mixd", t // 4)], writes=[("mt", b)], dma=True)
                P.add("sp", lambda e: e.dma_start(out=xt[b3][:], in_=src[t * 128:(t + 1) * 128, :]),
                      reads=[(srcn, t)], writes=[("xt", b3)], dma=True)

            p2_load(0)
            for t in range(NT):
                b = t % 2
                b3 = t % 3
                if t + 1 < NT:
                    p2_load(t + 1)
                for c in range(NCH):
                    P.add("pe", lambda e, b=b, c=c: e.transpose(tp[:, c * 128:(c + 1) * 128], mt[b][:, c * 128:(c + 1) * 128],
                                                                ident[:]),
                          reads=[("mt", b), "ident"], writes=["tp"])
                P.add("act", lambda e, b=b: e.copy(out=mT[b][:, :, :], in_=tp[:, :].rearrange("p (c n) -> p c n", n=128)),
                      writes=["tp", ("mT", b)])
                for half in range(2):
                    bank = ([6, 0] if t % 2 == 0 else [1, 2])[half]
                    for c in range(NCH):
                        P.add("pe", lambda e, b=b, c=c, half=half, bank=bank: e.matmul(
                            ps[bank][:, :], lhsT=mT[b][:, c, :], rhs=wout[:, c, half * 512:(half + 1) * 512],
                            start=(c == 0), stop=(c == NCH - 1)),
                            reads=[("mT", b), "wout"], writes=[psr(bank)])
                    P.add("dve", lambda e, b3=b3, half=half, bank=bank: e.tensor_tensor(
                        out=xt[b3][:, half * 512:(half + 1) * 512], in0=xt[b3][:, half * 512:(half + 1) * 512],
                        in1=ps[bank][:, :], op=ALU.add),
                        reads=[("xt", b3)], writes=[psr(bank), ("xt", b3)])
                P.add("pool", lambda e, t=t, b3=b3: e.dma_start(out=dst[t * 128:(t + 1) * 128, :], in_=xt[b3][:]),
                      reads=[("xt", b3)], writes=[(dstn, t)], dma=True)

        def unit(l, u, li):
            moba = u < 4
            dvw = 65 if moba else 129
            heads = [2 * u, 2 * u + 1] if moba else [8 + (u - 4), 8 + (u - 4)]
            P.add("pool", lambda e: e.dma_start(out=wbf[:].rearrange("p c n -> p (c n)"), in_=w_in_d[l, u, :, :]),
                  writes=["wbf"], dma=True)
            for i in range(2):
                P.add("pool", lambda e, i=i: e.dma_start(out=QT[i][80:84, :], in_=qaug_d[heads[i], :, :]),
                      writes=[("QTaug", i)], dma=True)
            if moba:
                P.add("dve", lambda e: e.memset(Vt[:, :, 64:65], 1.0), writes=["Vt"])
                P.add("dve", lambda e: e.memset(Vt[:, :, 129:130], 1.0), writes=["Vt"])
            else:
                P.add("dve", lambda e: e.memset(Vt[:, :, 128:129], 1.0), writes=["Vt"])
                if u == 4:
                    for i in range(2):
                        P.add("dve", lambda e, i=i: e.memset(QT[i][64:80, :], 0.0), writes=[("QTsel", i)])

            groups = [(which, g) for which in range(2) for g in range(NG)]
            PJB = [6, 1, 2, 3]
            SSB = [0, 4]

            def emit_proj(n):
                which, g = groups[n]
                pj = PJB[n % 4]
                for c in range(NCH):
                    P.add("pe", lambda e, c=c: e.matmul(
                        ps[pj][:, :], lhsT=wbf[:, c, which * 128:(which + 1) * 128],
                        rhs=hT[:, c, g * 512:(g + 1) * 512], start=(c == 0), stop=(c == NCH - 1)),
                        reads=["wbf", ("hT", g)], writes=[psr(pj)])

            def emit_chainA(n):
                pj = PJB[n % 4]
                b = n % 2
                ssb = SSB[n % 2]
                P.add("act", lambda e: e.activation(out=sq[b][:], in_=ps[pj][:, :], func=AF.Square),
                      writes=[psr(pj), ("sq", b)])
                P.add("pe", lambda e: e.matmul(ps[ssb][:, :], lhsT=bones[:], rhs=sq[b][:], start=True, stop=True),
                      reads=["bones", ("sq", b)], writes=[psr(ssb)])

            def emit_chainB(n):
                which, g = groups[n]
                pj = PJB[n % 4]
                b = n % 2
                ssb = SSB[n % 2]
                T = QT if which == 0 else KT
                tname = "QT" if which == 0 else "KT"
                colt = qcol if which == 0 else gcol
                cidx = (0 if moba else 2) + which
                P.add("act", lambda e: e.activation(out=rs[b][:], in_=ps[ssb][:, :], func=AF.Ln, scale=1.0 / 64, bias=EPS),
                      writes=[psr(ssb), ("rs", b)])
                P.add("act", lambda e: e.activation(out=rs[b][:], in_=rs[b][:], func=AF.Exp, scale=-0.5),
                      reads=[("rs", b)], writes=[("rs", b)])
                for i in range(2):
                    if moba and which == 1:
                        for hh in range(2):
                            P.add("dve", lambda e, i=i, hh=hh: e.scalar_tensor_tensor(
                                out=T[i][0:64, g * 512 + hh * 256:g * 512 + (hh + 1) * 256],
                                in0=ps[pj][i * 64:(i + 1) * 64, hh * 256:(hh + 1) * 256],
                                scalar=colt[i * 64:(i + 1) * 64, cidx:cidx + 1],
                                in1=rs[b][i * 64:(i + 1) * 64, hh * 256:(hh + 1) * 256],
                                op0=ALU.mult, op1=ALU.mult, accum_out=km[i][:, 2 * g + hh:2 * g + hh + 1]),
                                reads=[("rs", b), "qcol", "gcol"], writes=[psr(pj), (tname, i, g), ("km", i)])
                    else:
                        P.add("dve", lambda e, i=i: e.scalar_tensor_tensor(
                            out=T[i][0:64, g * 512:(g + 1) * 512], in0=ps[pj][i * 64:(i + 1) * 64, :],
                            scalar=colt[i * 64:(i + 1) * 64, cidx:cidx + 1], in1=rs[b][i * 64:(i + 1) * 64, :],
                            op0=ALU.mult, op1=ALU.mult),
                            reads=[("rs", b), "qcol", "gcol"], writes=[psr(pj), (tname, i, g)])

            NGR = len(groups)
            emit_proj(0)
            if NGR > 1:
                emit_proj(1)
            emit_chainA(0)
            for n in range(NGR):
                if n + 2 < NGR:
                    emit_proj(n + 2)
                if n + 1 < NGR:
                    emit_chainA(n + 1)
                emit_chainB(n)

            side = []

            def sel_stage1(i):
                P.add("dve", lambda e: e.tensor_scalar(out=kmb[i][:, 0:NB], in0=km[i][:, 0:NB], scalar1=1.0 / 256,
                                                       scalar2=None, op0=ALU.mult),
                      reads=[("km", i)], writes=[("kmb", i)])

            def sel_gate(i):
                gbank = 6 if i == 0 else 0
                for t in range(NT):
                    P.add("pe", lambda e, t=t: e.matmul(ps[gbank][:, t * 16:(t + 1) * 16],
                                                        lhsT=QT[i][0:64, t * 128:(t + 1) * 128], rhs=kmb[i][:, :],
                                                        start=True, stop=True),
                          reads=[("QT", i, t // 4), ("kmb", i)], writes=[psr(gbank)])
                P.add("dve", lambda e: e.tensor_tensor(out=gm[i][:].rearrange("p t n -> p (t n)"),
                                                       in0=ps[gbank][:, 0:NT * 16], in1=gbt[:], op=ALU.add),
                      reads=["gbt"], writes=[psr(gbank), ("gm", i)])

            def sel_chain(i):
                th = []
                for t in range(NT):
                    th.append(lambda t=t: P.add("dve", lambda e: e.max(out=top8[:, t, :], in_=gm[i][:, t, :]),
                                                reads=[("gm", i)], writes=["top8"]))
                th.append(lambda: P.add("dve", lambda e: e.tensor_scalar(out=thr[:], in0=top8[:, :, 3], scalar1=-1e29,
                                                                         scalar2=None, op0=ALU.max),
                                        reads=["top8"], writes=["thr"]))
                th.append(lambda: P.add("dve", lambda e: e.tensor_tensor(
                    out=sel[:], in0=gm[i][:], in1=thr[:].unsqueeze(2).to_broadcast([128, NT, 16]), op=ALU.is_ge),
                    reads=[("gm", i), "thr"], writes=["sel"]))
                th.append(lambda: P.add("dve", lambda e: e.tensor_scalar(out=selb[i][:], in0=sel[:], scalar1=-NEGM,
                                                                         scalar2=NEGM, op0=ALU.mult, op1=ALU.add),
                                        reads=["sel"], writes=[("selb", i)]))
                return th

            def sel_final_thunks(i):
                th = []
                for k, t0 in enumerate(range(0, NT, 8)):
                    def f(k=k, t0=t0):
                        w = cnt["tpb"] % 3
                        cnt["tpb"] += 1
                        if w == 0:
                            buf = tp[0:16, :]
                            res = "tp"
                        else:
                            buf = ps[3 + w][0:16, :].bitcast(BF16)
                            res = psr(3 + w)
                        for t in range(t0, t0 + 8):
                            P.add("pe", lambda e, t=t: e.transpose(buf[:, (t - t0) * 128:(t - t0 + 1) * 128],
                                                                   selb[i][:, t, :], ident[:]),
                                  reads=[("selb", i), "ident"], writes=[res])
                        if k % 2 == 0:
                            P.add("act", lambda e: e.copy(out=QT[i][64:80, t0 * 128:(t0 + 8) * 128], in_=buf),
                                  writes=[res, ("QTsel", i)])
                        else:
                            P.add("dve", lambda e: e.tensor_copy(out=QT[i][64:80, t0 * 128:(t0 + 8) * 128], in_=buf),
                                  writes=[res, ("QTsel", i)])
                    th.append(f)
                return th

            if moba:
                for i in range(2):
                    sel_stage1(i)

            for t in range(NT):
                bank = 2 + (t % 2)
                b = t % 2
                if moba and t == min(4, NT - 1):
                    for i in range(2):
                        sel_gate(i)
                        side.extend(sel_chain(i))
                        side.extend(sel_final_thunks(i))
                for c in range(NCH):
                    P.add("pe", lambda e, c=c, t=t, bank=bank: e.matmul(
                        ps[bank][:, 0:256], lhsT=hT[:, c, t * 128:(t + 1) * 128], rhs=wbf[:, c, 256:512],
                        start=(c == 0), stop=(c == NCH - 1)),
                        reads=["wbf", ("hT", t // 4)], writes=[psr(bank)])
                if moba:
                    P.add("dve", lambda e, t=t, bank=bank: e.tensor_copy(
                        out=Vt[:, t, :].rearrange("p (i c) -> p i c", c=65)[:, :, 0:64],
                        in_=ps[bank][:, 0:128].rearrange("p (i c) -> p i c", c=64)),
                        writes=[psr(bank), "Vt"])
                    P.add("act", lambda e, t=t, bank=bank: e.activation(out=Gt[:, t, :], in_=ps[bank][:, 128:256], func=AF.Silu),
                          writes=[psr(bank), "Gt"])
                else:
                    P.add("dve", lambda e, t=t, bank=bank: e.tensor_copy(out=Vt[:, t, 0:128], in_=ps[bank][:, 0:128]),
                          writes=[psr(bank), "Vt"])
                    P.add("act", lambda e, b=b, bank=bank: e.activation(out=gtmp[b][:], in_=ps[bank][:, 128:256], func=AF.Silu),
                          writes=[psr(bank), ("gtmp", b)])
                    P.add("dve", lambda e, t=t, b=b: e.tensor_tensor(out=Gt[:, t, :], in0=gtmp[b][:], in1=subbc[:], op=ALU.mult),
                          reads=[("gtmp", b), "subbc"], writes=["Gt"])
                for _ in range(4):
                    if side:
                        side.pop(0)()
            while side:
                side.pop(0)()

            slopes_all = MOBA_SLOPES + DIFF_SLOPES
            iters = []
            for g in range(NG):
                for i in range(2):
                    sl = slopes_all[heads[i]]
                    glist = []
                    for kt in range(0, 4 * (g + 1)):
                        j = kt - 4 * g
                        s0 = max(0, j)
                        s1 = s0
                        for sq_ in range(s0, 4):
                            tq = 4 * g + sq_
                            dmin = 0 if tq == kt else (tq * 128 - (kt * 128 + 127))
                            if sl * dmin <= ALIBI_FAR:
                                s1 = sq_ + 1
                        if s1 > s0:
                            glist.append(dict(g=g, i=i, kt=kt, s0=s0, s1=s1))
                    started = set()
                    for it in glist:
                        it["startbank"] = {}
                        for sq_ in range(it["s0"], it["s1"]):
                            bk = sq_ // 2
                            if bk not in started:
                                started.add(bk)
                                it["startbank"][sq_] = True
                    glist[-1]["last"] = True
                    assert glist[-1]["kt"] == 4 * g + 3 and started == {0, 1}
                    iters.extend(glist)
            SB = [0, 1, 6]
            PIPE = 2

            def emit_S(n):
                it = iters[n]
                g, i, kt, s0, s1 = it["g"], it["i"], it["kt"], it["s0"], it["s1"]
                j = kt - 4 * g
                sbk = SB[n % 3]
                P.add("pe", lambda e: e.matmul(
                    ps[sbk][:, s0 * 128:s1 * 128], lhsT=KT[i][0:KR, kt * 128:(kt + 1) * 128],
                    rhs=QT[i][0:KR, g * 512 + s0 * 128:g * 512 + s1 * 128], start=True, stop=(j < 0)),
                    reads=[("KT", i, kt // 4), ("KTaug", i), ("QT", i, g), ("QTaug", i), ("QTsel", i)],
                    writes=[psr(sbk)])
                if j >= 0:
                    P.add("pe", lambda e: e.matmul(ps[sbk][:, j * 128:(j + 1) * 128], lhsT=ident[:],
                                                   rhs=cmask[:], start=False, stop=True),
                          reads=["ident", "cmask"], writes=[psr(sbk)])

            def emit_rest(n):
                it = iters[n]
                g, i, kt, s0, s1 = it["g"], it["i"], it["kt"], it["s0"], it["s1"]
                sbk = SB[n % 3]
                slot = n % NPT
                oset = (g * 2 + i) % 2
                ob = [2 + 2 * oset, 3 + 2 * oset]
                P.add("act", lambda e: e.activation(
                    out=PT[slot][:, s0 * 128:s1 * 128], in_=ps[sbk][:, s0 * 128:s1 * 128], func=AF.Exp),
                    writes=[psr(sbk), ("PT", slot)])
                for s in range(s0, s1):
                    bank = ob[s // 2]
                    col = (s % 2) * dvw
                    vlo = i * 65 if moba else 0
                    P.add("pe", lambda e, s=s, bank=bank, col=col, vlo=vlo: e.matmul(
                        ps[bank][:, col:col + dvw], lhsT=PT[slot][:, s * 128:(s + 1) * 128],
                        rhs=Vt[:, kt, vlo:vlo + dvw], start=bool(it["startbank"].get(s, False)), stop=(kt == 4 * g + s),
                        skip_group_check=True),
                        reads=[("PT", slot), "Vt"], writes=[psr(bank)])
                if it.get("last"):
                    pend.append((n + EPI_DELAY, (u, g, i, ob, dvw, moba)))

            NI = len(iters)
            pend = []
            EPI_DELAY = 2
            for n in range(min(PIPE, NI)):
                emit_S(n)
            for n in range(NI):
                if n + PIPE < NI:
                    emit_S(n + PIPE)
                emit_rest(n)
                while pend and pend[0][0] <= n:
                    epilogue(*pend.pop(0)[1])
            while pend:
                epilogue(*pend.pop(0)[1])

        def epilogue(u, g, i, ob, dvw, moba):
            mb = g % 2
            if (not moba) and i == 1:
                k4 = cnt["s4"] % 16
                cnt["s4"] += 1
                c4 = 4 * k4
            for s in range(4):
                t = 4 * g + s
                bank = ob[s // 2]
                col = (s % 2) * dvw
                c0 = scol()
                if moba:
                    P.add("dve", lambda e, bank=bank, col=col, c0=c0: e.reciprocal(out=small[:, c0:c0 + 1],
                                                                                   in_=ps[bank][:, col + 64:col + 65]),
                          writes=[psr(bank), ("small", c0)])
                    P.add("dve", lambda e, bank=bank, col=col, c0=c0, s=s, t=t, mb=mb, i=i: e.scalar_tensor_tensor(
                        out=mo[mb][:, s, i * 64:(i + 1) * 64], in0=ps[bank][:, col:col + 64], scalar=small[:, c0:c0 + 1],
                        in1=Gt[:, t, i * 64:(i + 1) * 64], op0=ALU.mult, op1=ALU.mult),
                        reads=[("small", c0), "Gt"], writes=[psr(bank), ("mo", mb)])
                elif i == 0:
                    P.add("dve", lambda e, bank=bank, col=col, c0=c0: e.reciprocal(out=small[:, c0:c0 + 1],
                                                                                   in_=ps[bank][:, col + 128:col + 129]),
                          writes=[psr(bank), ("small", c0)])
                    P.add("dve", lambda e, bank=bank, col=col, c0=c0, s=s: e.tensor_scalar(
                        out=abuf[:, s, :], in0=ps[bank][:, col:col + 128], scalar1=small[:, c0:c0 + 1], scalar2=None,
                        op0=ALU.mult),
                        reads=[("small", c0)], writes=[psr(bank), ("abuf", s)])
                else:
                    P.add("dve", lambda e, bank=bank, col=col, c0=c0: e.reciprocal(out=small[:, c0:c0 + 1],
                                                                                   in_=ps[bank][:, col + 128:col + 129]),
                          writes=[psr(bank), ("small", c0)])
                    P.add("dve", lambda e, c0=c0: e.tensor_tensor(out=small[:, c0:c0 + 1], in0=small[:, c0:c0 + 1],
                                                                 in1=lamt[:, 4:5], op=ALU.mult),
                          reads=[("small", c0), "neglam"], writes=[("small", c0)])
                    P.add("dve", lambda e, bank=bank, col=col, c0=c0, s=s: e.scalar_tensor_tensor(
                        out=dbuf[:, s, :], in0=ps[bank][:, col:col + 128], scalar=small[:, c0:c0 + 1], in1=abuf[:, s, :],
                        op0=ALU.mult, op1=ALU.add),
                        reads=[("small", c0), ("abuf", s)], writes=[psr(bank), ("dbuf", s)])
                    P.add("dve", lambda e, s=s, c4=c4: e.scalar_tensor_tensor(
                        out=junkf[:], in0=dbuf[:, s, :], scalar=1.0, in1=dbuf[:, s, :], op0=ALU.mult, op1=ALU.mult,
                        accum_out=small4[:, c4 + s:c4 + s + 1]),
                        reads=[("dbuf", s)], writes=["junkf", ("small4", c4)])
            if (not moba) and i == 1:
                P.add("act", lambda e, c4=c4: e.activation(out=small4[:, c4:c4 + 4], in_=small4[:, c4:c4 + 4], func=AF.Ln,
                                                          scale=1.0 / 128, bias=EPS),
                      reads=[("small4", c4)], writes=[("small4", c4)])
                P.add("act", lambda e, c4=c4: e.activation(out=small4[:, c4:c4 + 4], in_=small4[:, c4:c4 + 4], func=AF.Exp,
                                                          scale=-0.5),
                      reads=[("small4", c4)], writes=[("small4", c4)])
                for s in range(4):
                    t = 4 * g + s
                    P.add("dve", lambda e, s=s, t=t, mb=mb, c4=c4: e.scalar_tensor_tensor(
                        out=mo[mb][:, s, :], in0=dbuf[:, s, :], scalar=small4[:, c4 + s:c4 + s + 1], in1=Gt[:, t, :],
                        op0=ALU.mult, op1=ALU.mult),
                        reads=[("dbuf", s), ("small4", c4), "Gt"], writes=[("mo", mb)])
            if i == 1:
                P.add("sp", lambda e, g=g, mb=mb, u=u: e.dma_start(
                    out=mix_d[g * 512:(g + 1) * 512, u * 128:(u + 1) * 128].rearrange("(s p) c -> p s c", p=128),
                    in_=mo[mb][:, :, :]),
                    reads=[("mo", mb)], writes=[("mixd", g)], dma=True)

        for l in range(L):
            layer(l)
        P.add("sp", None, reads=[("outd", t) for t in range(NT)])
        print('sbuf bytes remaining', nc.sbuf_bytes_remaining)
        P.emit(nc, block, sems, dsems)
    return nc


def _host_layout(S, inputs):
    w_in = np.asarray(inputs["w_in"], dtype=np.float32)
    w_out = np.asarray(inputs["w_out"], dtype=np.float32)
    L = w_in.shape[0]
    NT = S // 128
    w_in_r = np.empty((L, 8, 128, NCH, 512), np.float32)
    for l in range(L):
        wl = w_in[l].reshape(NCH, 128, 4096)
        for u in range(8):
            base = 0 if u < 4 else 2048
            idx = u % 4
            for k in range(4):
                c0 = base + k * 512 + idx * 128
                w_in_r[l, u, :, :, k * 128:(k + 1) * 128] = wl[:, :, c0:c0 + 128].transpose(1, 0, 2)
    w_in_r = np.ascontiguousarray(w_in_r.reshape(L, 8, 128, NCH * 512))
    w_out_r = np.ascontiguousarray(w_out.reshape(L, NCH, 128, D).transpose(0, 2, 1, 3).reshape(L, 128, NCH * D))
    gcol = np.stack([np.stack([np.tile(np.asarray(inputs[k], np.float32)[l], 2) for k in
                               ("moba_q_norm", "moba_k_norm", "diff_q_norm", "diff_k_norm")], axis=1)
                     for l in range(L)], axis=0).astype(np.float32)
    lamv = np.concatenate([np.asarray(inputs[k], np.float32) for k in
                           ("lambda_q1", "lambda_k1", "lambda_q2", "lambda_k2")], axis=1)
    pos = np.arange(S)
    ident = np.eye(128, dtype=np.float32)
    kk = np.arange(128)[:, None]
    qq = np.arange(128)[None, :]
    cmask = np.where(kk <= qq, 0.0, NEGM).astype(np.float32)
    bones = np.kron(np.eye(2, dtype=np.float32), np.ones((64, 64), np.float32))
    kaug = np.zeros((20, S), np.float32)
    kaug[0:16] = (pos[None, :] // 256 == np.arange(16)[:, None]).astype(np.float32)
    kaug[16] = pos // 128
    kaug[17] = pos % 128
    kaug[18] = 1.0
    kaug[19] = 1.0
    qaug = np.zeros((12, 4, S), np.float32)
    for h, sl in enumerate(MOBA_SLOPES + DIFF_SLOPES):
        qaug[h, 0] = sl * 128.0
        qaug[h, 1] = sl
        qaug[h, 2] = -sl * 128.0 * (pos // 128)
        qaug[h, 3] = -sl * (pos % 128)
    gb = np.zeros((NT, 16), np.float32)
    for t in range(NT):
        own = t // 2
        gb[t, own] = 1e30
        gb[t, own + 1:] = -1e30
    common = {
        "w_in_r": w_in_r, "w_out_r": w_out_r,
        "norm_g": np.ascontiguousarray(np.asarray(inputs["norm_g"], np.float32)),
        "gcol": np.ascontiguousarray(gcol),
        "subln": np.ascontiguousarray(np.asarray(inputs["diff_subln"], np.float32)),
        "lamv": np.ascontiguousarray(lamv),
        "c_ident": ident, "c_cmask": cmask, "c_bones": bones, "c_kaug": kaug, "c_qaug": qaug,
        "c_gb": gb.reshape(1, NT * 16),
    }
    return L, common


def kernel(x, norm_g, w_in, moba_q_norm, moba_k_norm, diff_q_norm, diff_k_norm,
           lambda_q1, lambda_k1, lambda_q2, lambda_k2, diff_subln, w_out):
    inputs = dict(x=x, norm_g=norm_g, w_in=w_in, moba_q_norm=moba_q_norm, moba_k_norm=moba_k_norm,
                  diff_q_norm=diff_q_norm, diff_k_norm=diff_k_norm, lambda_q1=lambda_q1, lambda_k1=lambda_k1,
                  lambda_q2=lambda_q2, lambda_k2=lambda_k2, diff_subln=diff_subln, w_out=w_out)
    x = np.asarray(x, dtype=np.float32)
    B, S, _ = x.shape
    L, common = _host_layout(S, inputs)
    lambda_inits = [0.8 - 0.6 * math.exp(-0.3 * l) for l in range(L)]
    nc = build(S, L, lambda_inits)
    in_maps = [dict(common, x=np.ascontiguousarray(x[b])) for b in range(B)]
    res = run_bass_kernel_spmd(nc, in_maps, core_ids=list(range(B)))
    return np.stack([np.asarray(r["out"], dtype=np.float32) for r in res.results], axis=0)
```

```python
import math
import numpy as np
import concourse.bass as bass
import concourse.mybir as mybir
from concourse.bass_utils import run_bass_kernel_spmd

F32 = mybir.dt.float32
BF16 = mybir.dt.bfloat16
AF = mybir.ActivationFunctionType
ALU = mybir.AluOpType
AX = mybir.AxisListType

D = 1024
NCH = 8
EPS = 1e-6
NEGM = -30000.0
KR = 84
MOBA_SLOPES = [2.0 ** (-8.0 * i / 8) for i in range(1, 9)]
DIFF_SLOPES = [2.0 ** (-8.0 * i / 4) for i in range(1, 5)]
NDSEM = 8
ALIBI_FAR = 60.0


class Prog:
    def __init__(self):
        self.ops = []
        self.last_w = {}
        self.readers = {}
        self.dma_hist = {"sp": [], "pool": []}

    def add(self, eng, fn, reads=(), writes=(), dma=False):
        idx = len(self.ops)
        raw = set()
        deps = set()
        for r in reads:
            if r in self.last_w:
                raw.add(self.last_w[r])
        for w in writes:
            if w in self.last_w:
                deps.add(self.last_w[w])
            for rd in self.readers.get(w, ()):
                deps.add(rd)
        deps |= raw
        keep = set()
        for j in deps:
            oj = self.ops[j]
            if oj["dma"]:
                keep.add(j)
            elif oj["eng"] != eng:
                keep.add(j)
            elif (j in raw) and eng != "pe" and not dma:
                keep.add(j)
            elif dma:
                keep.add(j)
        op = dict(eng=eng, fn=fn, dma=dma, deps=keep, sig=dma, sem=None, val=None)
        if dma:
            h = self.dma_hist[eng]
            n = len(h)
            if n >= NDSEM:
                op["deps"].add(h[n - NDSEM])
            op["slot"] = n % NDSEM
            op["val"] = 16 * (n // NDSEM + 1)
            h.append(idx)
        for w in writes:
            self.last_w[w] = idx
            self.readers[w] = []
        for r in reads:
            self.readers.setdefault(r, []).append(idx)
        self.ops.append(op)
        return idx

    def emit(self, nc, block, sems, dsems):
        ops = self.ops
        for op in ops:
            for j in op["deps"]:
                ops[j]["sig"] = True
        cnt = {e: 0 for e in sems}
        for op in ops:
            if op["dma"]:
                op["sem"] = dsems[op["eng"]][op["slot"]]
            elif op["sig"]:
                cnt[op["eng"]] += 1
                op["sem"] = sems[op["eng"]]
                op["val"] = cnt[op["eng"]]

        def run(engname, eng):
            waited = {}
            for op in ops:
                if op["eng"] != engname:
                    continue
                need = {}
                for j in op["deps"]:
                    oj = ops[j]
                    key = id(oj["sem"])
                    if waited.get(key, 0) >= oj["val"]:
                        continue
                    if key not in need or need[key][1] < oj["val"]:
                        need[key] = (oj["sem"], oj["val"])
                for key, (sem, val) in need.items():
                    eng.wait_ge(sem, val)
                    waited[key] = val
                if op["fn"] is None:
                    continue
                ins = op["fn"](eng)
                if op["dma"]:
                    ins.then_inc(op["sem"], 16)
                elif op["sig"]:
                    ins.then_inc(op["sem"], 1)

        @block.tensor
        def _(e):
            run("pe", e)

        @block.scalar
        def _(e):
            run("act", e)

        @block.vector
        def _(e):
            run("dve", e)

        @block.gpsimd
        def _(e):
            run("pool", e)

        @block.sync
        def _(e):
            run("sp", e)


def build(S, L, lambda_inits):
    NT = S // 128
    NG = S // 512
    NB = S // 256
    assert S % 512 == 0 and NT <= 32
    nc = bass.Bass("TRN2", target_bir_lowering=False)

    def dram(name, shape, dt, kind):
        return nc.dram_tensor(name, list(shape), dt, kind=kind).ap()

    x_d = dram("x", [S, D], F32, "ExternalInput")
    w_in_d = dram("w_in_r", [L, 8, 128, NCH * 512], F32, "ExternalInput")
    w_out_d = dram("w_out_r", [L, 128, NCH * D], F32, "ExternalInput")
    ng_d = dram("norm_g", [L, D], F32, "ExternalInput")
    gcol_d = dram("gcol", [L, 128, 4], F32, "ExternalInput")
    sub_d = dram("subln", [L, 128], F32, "ExternalInput")
    lamv_d = dram("lamv", [L, 256], F32, "ExternalInput")
    ident_d = dram("c_ident", [128, 128], F32, "ExternalInput")
    cm_d = dram("c_cmask", [128, 128], F32, "ExternalInput")
    bones_d = dram("c_bones", [128, 128], F32, "ExternalInput")
    kaug_d = dram("c_kaug", [20, S], F32, "ExternalInput")
    qaug_d = dram("c_qaug", [12, 4, S], F32, "ExternalInput")
    gb_d = dram("c_gb", [1, NT * 16], F32, "ExternalInput")
    out_d = dram("out", [S, D], F32, "ExternalOutput")
    x1_d = dram("x1_scratch", [S, D], F32, "Internal")
    mix_d = dram("mix_scratch", [S, D], BF16, "Internal")

    from contextlib import ExitStack
    es = ExitStack()

    def sb(name, shape, dt):
        return es.enter_context(nc.sbuf_tensor(name, list(shape), dt))

    def pst(name, shape, dt):
        return es.enter_context(nc.psum_tensor(name, list(shape), dt))

    with es:
        hT = sb("hT", [128, NCH, S], BF16)
        xt = [sb(f"xt{i}", [128, D], F32) for i in range(3)]
        hb = [sb(f"hb{i}", [128, D], BF16) for i in range(2)]
        junk = sb("junk", [128, D], BF16)
        junk2 = sb("junk2", [128, D], BF16)
        gbc = sb("gbc", [128, D], F32)
        wbf = sb("wbf", [128, NCH, 512], BF16)
        wout = sb("wout", [128, NCH, D], BF16)
        QT = [sb(f"QT{i}", [128, S], BF16) for i in range(2)]
        KT = [sb(f"KT{i}", [128, S], BF16) for i in range(2)]
        Vt = sb("Vt", [128, NT, 130], BF16)
        Gt = sb("Gt", [128, NT, 128], BF16)
        gtmp = [sb(f"gtmp{i}", [128, 128], F32) for i in range(2)]
        sq = [sb(f"sq{i}", [128, 512], BF16) for i in range(2)]
        rs = [sb(f"rs{i}", [128, 512], F32) for i in range(2)]
        NPT = 4
        PT = [sb(f"PT{i}", [128, 512], BF16) for i in range(NPT)]
        gm = [sb(f"gm{i}", [128, NT, 16], F32) for i in range(2)]
        top8 = sb("top8", [128, NT, 8], F32)
        thr = sb("thr", [128, NT], F32)
        sel = sb("sel", [128, NT, 16], F32)
        selb = [sb(f"selb{i}", [128, NT, 16], BF16) for i in range(2)]
        km = [sb(f"km{i}", [64, 16], F32) for i in range(2)]
        kmb = [sb(f"kmb{i}", [64, 16], BF16) for i in range(2)]
        mo = [sb(f"mo{i}", [128, 4, 128], BF16) for i in range(2)]
        abuf = sb("abuf", [128, 4, 128], F32)
        dbuf = sb("dbuf", [128, 4, 128], F32)
        junkf = sb("junkf", [128, 128], F32)
        small4 = sb("small4", [128, 64], F32)
        small = sb("small", [128, 64], F32)
        mt = [sb(f"mt{i}", [128, D], BF16) for i in range(2)]
        mT = [sb(f"mT{i}", [128, NCH, 128], BF16) for i in range(2)]
        ident = sb("ident", [128, 128], BF16)
        cmask = sb("cmask", [128, 128], BF16)
        bones = sb("bones", [128, 128], BF16)
        gbt = sb("gbt", [128, NT * 16], F32)
        gcol = sb("gcol_s", [128, 4], F32)
        qcol = sb("qcol_s", [128, 4], F32)
        subbc = sb("subbc", [128, 128], F32)
        lamv = sb("lamv_s", [128, 256], F32)
        lamt = sb("lamt", [128, 8], F32)

        ps = [pst(f"ps{i}", [128, 512], F32) for i in range(7)]
        tp = pst("tp", [128, 1024], BF16)

        sems = {e: es.enter_context(nc.semaphore(f"sem_{e}")) for e in ["pe", "act", "dve", "pool", "sp"]}
        dsems = {q: [es.enter_context(nc.semaphore(f"dsem_{q}{i}")) for i in range(NDSEM)] for q in ["sp", "pool"]}
        block = es.enter_context(nc.Block())

        P = Prog()
        cnt = {"s": 0, "pt": 0, "small": 0, "pj": 0, "s4": 0, "tpb": 0}

        def psr(k):
            return ("ps", k)

        def scol():
            c = cnt["small"] % 64
            cnt["small"] += 1
            return c

        P.add("pool", lambda e: e.dma_start(out=ident[:], in_=ident_d[:, :]), writes=["ident"], dma=True)
        P.add("pool", lambda e: e.dma_start(out=cmask[:], in_=cm_d[:, :]), writes=["cmask"], dma=True)
        P.add("pool", lambda e: e.dma_start(out=bones[:], in_=bones_d[:, :]), writes=["bones"], dma=True)
        P.add("sp", lambda e: e.dma_start(out=gbt[:], in_=gb_d[:, :].to_broadcast([128, NT * 16])), writes=["gbt"], dma=True)
        for i in range(2):
            P.add("pool", lambda e, i=i: e.dma_start(out=KT[i][64:84, :], in_=kaug_d[:, :]),
                  writes=[("KTaug", i)], dma=True)
        for i in range(2):
            P.add("dve", lambda e, i=i: e.memset(kmb[i][:], 0.0), writes=[("kmb", i)])

        def layer(l):
            src = x_d if l == 0 else x1_d
            dst = out_d if l == L - 1 else x1_d
            srcn = "xd" if l == 0 else "x1d"
            dstn = "outd" if l == L - 1 else "x1d"
            li = lambda_inits[l]

            if l == 0:
                P.add("sp", lambda e: e.dma_start(out=gbc[:], in_=ng_d[l:l + 1, :].to_broadcast([128, D])),
                      writes=["gbc"], dma=True)
            P.add("sp", lambda e: e.dma_start(out=gcol[:], in_=gcol_d[l, :, :]), writes=["gcol"], dma=True)
            P.add("sp", lambda e: e.dma_start(out=subbc[:], in_=sub_d[l:l + 1, :].to_broadcast([128, 128])),
                  writes=["subbc"], dma=True)
            P.add("sp", lambda e: e.dma_start(out=lamv[:], in_=lamv_d[l:l + 1, :].to_broadcast([128, 256])),
                  writes=["lamv"], dma=True)
            P.add("pool", lambda e: e.dma_start(out=wout[:].rearrange("p c n -> p (c n)"), in_=w_out_d[l, :, :]),
                  writes=["wout"], dma=True)
            P.add("dve", lambda e: e.tensor_scalar(out=qcol[:], in0=gcol[:], scalar1=0.125, scalar2=None, op0=ALU.mult),
                  reads=["gcol"], writes=["qcol"])
            P.add("dve", lambda e: e.tensor_scalar(out=subbc[:], in0=subbc[:], scalar1=float(1.0 - li), scalar2=None,
                                                   op0=ALU.mult), reads=["subbc"], writes=["subbc"])
            P.add("dve", lambda e: e.tensor_tensor(out=lamv[:, 0:64], in0=lamv[:, 0:64], in1=lamv[:, 64:128], op=ALU.mult),
                  reads=["lamv"], writes=["lamv"])
            P.add("dve", lambda e: e.tensor_tensor(out=lamv[:, 128:192], in0=lamv[:, 128:192], in1=lamv[:, 192:256],
                                                   op=ALU.mult), reads=["lamv"], writes=["lamv"])
            P.add("dve", lambda e: e.tensor_reduce(out=lamt[:, 0:1], in_=lamv[:, 0:64], axis=AX.X, op=ALU.add),
                  reads=["lamv"], writes=["lamt"])
            P.add("dve", lambda e: e.tensor_reduce(out=lamt[:, 1:2], in_=lamv[:, 128:192], axis=AX.X, op=ALU.add),
                  reads=["lamv", "lamt"], writes=["lamt"])
            P.add("act", lambda e: e.activation(out=lamt[:, 2:4], in_=lamt[:, 0:2], func=AF.Exp),
                  reads=["lamt"], writes=["lamt"])
            P.add("dve", lambda e: e.scalar_tensor_tensor(out=lamt[:, 4:5], in0=lamt[:, 3:4], scalar=float(-li),
                                                          in1=lamt[:, 2:3], op0=ALU.add, op1=ALU.subtract),
                  reads=["lamt"], writes=["neglam"])

            cols0 = {}

            def p0_A(t):
                b3 = t % 3
                P.add("sp", lambda e: e.dma_start(out=xt[b3][:], in_=src[t * 128:(t + 1) * 128, :]),
                      reads=[(srcn, t)], writes=[("xt", b3)], dma=True)
                c0 = scol()
                cols0[t] = c0
                jk = junk if t % 2 == 0 else junk2
                P.add("act", lambda e: e.activation(out=jk[:], in_=xt[b3][:], func=AF.Square,
                                                    accum_out=small[:, c0:c0 + 1]),
                      reads=[("xt", b3)], writes=[("junk", t % 2), ("small", c0)])

            def p0_B(t):
                b3 = t % 3
                b = t % 2
                c0 = cols0[t]
                P.add("act", lambda e: e.activation(out=small[:, c0:c0 + 1], in_=small[:, c0:c0 + 1], func=AF.Ln,
                                                    scale=1.0 / D, bias=EPS),
                      reads=[("small", c0)], writes=[("small", c0)])
                P.add("act", lambda e: e.activation(out=small[:, c0:c0 + 1], in_=small[:, c0:c0 + 1], func=AF.Exp,
                                                    scale=-0.5),
                      reads=[("small", c0)], writes=[("small", c0)])
                P.add("dve", lambda e: e.scalar_tensor_tensor(out=hb[b][:], in0=xt[b3][:], scalar=small[:, c0:c0 + 1],
                                                              in1=gbc[:], op0=ALU.mult, op1=ALU.mult),
                      reads=[("xt", b3), ("small", c0), "gbc"], writes=[("hb", b)])

            def p0_B2(t):
                b = t % 2
                for c in range(NCH):
                    P.add("pe", lambda e, c=c: e.transpose(tp[:, c * 128:(c + 1) * 128], hb[b][:, c * 128:(c + 1) * 128],
                                                           ident[:]),
                          reads=[("hb", b), "ident"], writes=["tp"])

            def p0_C(t):
                P.add("dve", lambda e: e.tensor_copy(out=hT[:, :, t * 128:(t + 1) * 128],
                                                     in_=tp[:, :].rearrange("p (c n) -> p c n", n=128)),
                      writes=["tp", ("hT", t // 4)])

            if l == 0:
                p0_A(0)
                for t in range(NT):
                    if t + 1 < NT:
                        p0_A(t + 1)
                    p0_B(t)
                    if t >= 1:
                        p0_C(t - 1)
                    p0_B2(t)
                p0_C(NT - 1)

            for u in range(8):
                unit(l, u, li)

            def p2_load(t):
                b = t % 2
                b3 = t % 3
                P.add("sp", lambda e: e.dma_start(out=mt[b][:], in_=mix_d[t * 128:(t + 1) * 128, :]),
                      reads=[("mixd", t // 4)], writes=[("mt", b)], dma=True)
                P.add("sp", lambda e: e.dma_start(out=xt[b3][:], in_=src[t * 128:(t + 1) * 128, :]),
                      reads=[(srcn, t)], writes=[("xt", b3)], dma=True)

            fuse = l < L - 1
            if fuse:
                P.add("sp", lambda e: e.dma_start(out=gbc[:], in_=ng_d[l + 1:l + 2, :].to_broadcast([128, D])),
                      writes=["gbc"], dma=True)
            tpn = ps[5][:, :].bitcast(BF16)
            fcols = {}

            def f_act(t):
                b3 = t % 3
                c0 = scol()
                fcols[t] = c0
                jk = junk if t % 2 == 0 else junk2
                P.add("act", lambda e: e.activation(out=jk[:], in_=xt[b3][:], func=AF.Square,
                                                    accum_out=small[:, c0:c0 + 1]),
                      reads=[("xt", b3)], writes=[("junk", t % 2), ("small", c0)])
                P.add("act", lambda e: e.activation(out=small[:, c0:c0 + 1], in_=small[:, c0:c0 + 1], func=AF.Ln,
                                                    scale=1.0 / D, bias=EPS),
                      reads=[("small", c0)], writes=[("small", c0)])
                P.add("act", lambda e: e.activation(out=small[:, c0:c0 + 1], in_=small[:, c0:c0 + 1], func=AF.Exp,
                                                    scale=-0.5),
                      reads=[("small", c0)], writes=[("small", c0)])

            def f_stt(t):
                b3 = t % 3
                b = t % 2
                c0 = fcols[t]
                P.add("dve", lambda e: e.scalar_tensor_tensor(out=hb[b][:], in0=xt[b3][:], scalar=small[:, c0:c0 + 1],
                                                              in1=gbc[:], op0=ALU.mult, op1=ALU.mult),
                      reads=[("xt", b3), ("small", c0), "gbc"], writes=[("hb", b)])

            def f_tr(t):
                b = t % 2
                for c in range(NCH):
                    P.add("pe", lambda e, c=c: e.transpose(tpn[:, c * 128:(c + 1) * 128], hb[b][:, c * 128:(c + 1) * 128],
                                                           ident[:]),
                          reads=[("hb", b), "ident"], writes=[psr(5)])
                P.add("dve", lambda e: e.tensor_copy(out=hT[:, :, t * 128:(t + 1) * 128],
                                                     in_=tpn.rearrange("p (c n) -> p c n", n=128)),
                      writes=[psr(5), ("hT", t // 4)])

            p2_load(0)
            for t in range(NT):
                b = t % 2
                b3 = t % 3
                if t + 1 < NT:
                    p2_load(t + 1)
                for c in range(NCH):
                    P.add("pe", lambda e, b=b, c=c: e.transpose(tp[:, c * 128:(c + 1) * 128], mt[b][:, c * 128:(c + 1) * 128],
                                                                ident[:]),
                          reads=[("mt", b), "ident"], writes=["tp"])
                P.add("act", lambda e, b=b: e.copy(out=mT[b][:, :, :], in_=tp[:, :].rearrange("p (c n) -> p c n", n=128)),
                      writes=["tp", ("mT", b)])
                if fuse and t >= 1:
                    f_act(t - 1)
                    f_stt(t - 1)
                banks = [6, 0] if t % 2 == 0 else [1, 2]
                for half in range(2):
                    bank = banks[half]
                    for c in range(NCH):
                        P.add("pe", lambda e, b=b, c=c, half=half, bank=bank: e.matmul(
                            ps[bank][:, :], lhsT=mT[b][:, c, :], rhs=wout[:, c, half * 512:(half + 1) * 512],
                            start=(c == 0), stop=(c == NCH - 1)),
                            reads=[("mT", b), "wout"], writes=[psr(bank)])
                if fuse and t >= 1:
                    f_tr(t - 1)
                for half in range(2):
                    bank = banks[half]
                    P.add("dve", lambda e, b3=b3, half=half, bank=bank: e.tensor_tensor(
                        out=xt[b3][:, half * 512:(half + 1) * 512], in0=xt[b3][:, half * 512:(half + 1) * 512],
                        in1=ps[bank][:, :], op=ALU.add),
                        reads=[("xt", b3)], writes=[psr(bank), ("xt", b3)])
                P.add("pool", lambda e, t=t, b3=b3: e.dma_start(out=dst[t * 128:(t + 1) * 128, :], in_=xt[b3][:]),
                      reads=[("xt", b3)], writes=[(dstn, t)], dma=True)
            if fuse:
                f_act(NT - 1)
                f_stt(NT - 1)
                f_tr(NT - 1)

        def unit(l, u, li):
            moba = u < 4
            dvw = 65 if moba else 129
            heads = [2 * u, 2 * u + 1] if moba else [8 + (u - 4), 8 + (u - 4)]
            P.add("pool", lambda e: e.dma_start(out=wbf[:].rearrange("p c n -> p (c n)"), in_=w_in_d[l, u, :, :]),
                  writes=["wbf"], dma=True)
            for i in range(2):
                P.add("pool", lambda e, i=i: e.dma_start(out=QT[i][80:84, :], in_=qaug_d[heads[i], :, :]),
                      writes=[("QTaug", i)], dma=True)
            if moba:
                P.add("dve", lambda e: e.memset(Vt[:, :, 64:65], 1.0), writes=["Vt"])
                P.add("dve", lambda e: e.memset(Vt[:, :, 129:130], 1.0), writes=["Vt"])
            else:
                P.add("dve", lambda e: e.memset(Vt[:, :, 128:129], 1.0), writes=["Vt"])
                if u == 4:
                    for i in range(2):
                        P.add("dve", lambda e, i=i: e.memset(QT[i][64:80, :], 0.0), writes=[("QTsel", i)])

            groups = [(which, g) for which in range(2) for g in range(NG)]
            PJB = [6, 1, 2, 3]
            SSB = [0, 4]

            def emit_proj(n):
                which, g = groups[n]
                pj = PJB[n % 4]
                for c in range(NCH):
                    P.add("pe", lambda e, c=c: e.matmul(
                        ps[pj][:, :], lhsT=wbf[:, c, which * 128:(which + 1) * 128],
                        rhs=hT[:, c, g * 512:(g + 1) * 512], start=(c == 0), stop=(c == NCH - 1)),
                        reads=["wbf", ("hT", g)], writes=[psr(pj)])

            def emit_chainA(n):
                pj = PJB[n % 4]
                b = n % 2
                ssb = SSB[n % 2]
                P.add("act", lambda e: e.activation(out=sq[b][:], in_=ps[pj][:, :], func=AF.Square),
                      writes=[psr(pj), ("sq", b)])
                P.add("pe", lambda e: e.matmul(ps[ssb][:, :], lhsT=bones[:], rhs=sq[b][:], start=True, stop=True),
                      reads=["bones", ("sq", b)], writes=[psr(ssb)])

            def emit_chainB(n):
                which, g = groups[n]
                pj = PJB[n % 4]
                b = n % 2
                ssb = SSB[n % 2]
                T = QT if which == 0 else KT
                tname = "QT" if which == 0 else "KT"
                colt = qcol if which == 0 else gcol
                cidx = (0 if moba else 2) + which
                P.add("act", lambda e: e.activation(out=rs[b][:], in_=ps[ssb][:, :], func=AF.Ln, scale=1.0 / 64, bias=EPS),
                      writes=[psr(ssb), ("rs", b)])
                P.add("act", lambda e: e.activation(out=rs[b][:], in_=rs[b][:], func=AF.Exp, scale=-0.5),
                      reads=[("rs", b)], writes=[("rs", b)])
                for i in range(2):
                    if moba and which == 1:
                        for hh in range(2):
                            P.add("dve", lambda e, i=i, hh=hh: e.scalar_tensor_tensor(
                                out=T[i][0:64, g * 512 + hh * 256:g * 512 + (hh + 1) * 256],
                                in0=ps[pj][i * 64:(i + 1) * 64, hh * 256:(hh + 1) * 256],
                                scalar=colt[i * 64:(i + 1) * 64, cidx:cidx + 1],
                                in1=rs[b][i * 64:(i + 1) * 64, hh * 256:(hh + 1) * 256],
                                op0=ALU.mult, op1=ALU.mult, accum_out=km[i][:, 2 * g + hh:2 * g + hh + 1]),
                                reads=[("rs", b), "qcol", "gcol"], writes=[psr(pj), (tname, i, g), ("km", i)])
                    else:
                        P.add("dve", lambda e, i=i: e.scalar_tensor_tensor(
                            out=T[i][0:64, g * 512:(g + 1) * 512], in0=ps[pj][i * 64:(i + 1) * 64, :],
                            scalar=colt[i * 64:(i + 1) * 64, cidx:cidx + 1], in1=rs[b][i * 64:(i + 1) * 64, :],
                            op0=ALU.mult, op1=ALU.mult),
                            reads=[("rs", b), "qcol", "gcol"], writes=[psr(pj), (tname, i, g)])

            NGR = len(groups)
            emit_proj(0)
            if NGR > 1:
                emit_proj(1)
            emit_chainA(0)
            for n in range(NGR):
                if n + 2 < NGR:
                    emit_proj(n + 2)
                if n + 1 < NGR:
                    emit_chainA(n + 1)
                emit_chainB(n)

            side = []

            def sel_stage1(i):
                P.add("dve", lambda e: e.tensor_scalar(out=kmb[i][:, 0:NB], in0=km[i][:, 0:NB], scalar1=1.0 / 256,
                                                       scalar2=None, op0=ALU.mult),
                      reads=[("km", i)], writes=[("kmb", i)])

            def sel_gate(i):
                gbank = 6 if i == 0 else 0
                for t in range(NT):
                    P.add("pe", lambda e, t=t: e.matmul(ps[gbank][:, t * 16:(t + 1) * 16],
                                                        lhsT=QT[i][0:64, t * 128:(t + 1) * 128], rhs=kmb[i][:, :],
                                                        start=True, stop=True),
                          reads=[("QT", i, t // 4), ("kmb", i)], writes=[psr(gbank)])
                P.add("dve", lambda e: e.tensor_tensor(out=gm[i][:].rearrange("p t n -> p (t n)"),
                                                       in0=ps[gbank][:, 0:NT * 16], in1=gbt[:], op=ALU.add),
                      reads=["gbt"], writes=[psr(gbank), ("gm", i)])

            def sel_chain(i):
                th = []
                for t in range(NT):
                    th.append(lambda t=t: P.add("dve", lambda e: e.max(out=top8[:, t, :], in_=gm[i][:, t, :]),
                                                reads=[("gm", i)], writes=["top8"]))
                th.append(lambda: P.add("dve", lambda e: e.tensor_scalar(out=thr[:], in0=top8[:, :, 3], scalar1=-1e29,
                                                                         scalar2=None, op0=ALU.max),
                                        reads=["top8"], writes=["thr"]))
                th.append(lambda: P.add("dve", lambda e: e.tensor_tensor(
                    out=sel[:], in0=gm[i][:], in1=thr[:].unsqueeze(2).to_broadcast([128, NT, 16]), op=ALU.is_ge),
                    reads=[("gm", i), "thr"], writes=["sel"]))
                th.append(lambda: P.add("dve", lambda e: e.tensor_scalar(out=selb[i][:], in0=sel[:], scalar1=-NEGM,
                                                                         scalar2=NEGM, op0=ALU.mult, op1=ALU.add),
                                        reads=["sel"], writes=[("selb", i)]))
                return th

            def sel_final_thunks(i):
                th = []
                for k, t0 in enumerate(range(0, NT, 8)):
                    def f(k=k, t0=t0):
                        w = cnt["tpb"] % 3
                        cnt["tpb"] += 1
                        if w == 0:
                            buf = tp[0:16, :]
                            res = "tp"
                        else:
                            buf = ps[3 + w][0:16, :].bitcast(BF16)
                            res = psr(3 + w)
                        for t in range(t0, t0 + 8):
                            P.add("pe", lambda e, t=t: e.transpose(buf[:, (t - t0) * 128:(t - t0 + 1) * 128],
                                                                   selb[i][:, t, :], ident[:]),
                                  reads=[("selb", i), "ident"], writes=[res])
                        if k % 2 == 0:
                            P.add("act", lambda e: e.copy(out=QT[i][64:80, t0 * 128:(t0 + 8) * 128], in_=buf),
                                  writes=[res, ("QTsel", i)])
                        else:
                            P.add("dve", lambda e: e.tensor_copy(out=QT[i][64:80, t0 * 128:(t0 + 8) * 128], in_=buf),
                                  writes=[res, ("QTsel", i)])
                    th.append(f)
                return th

            if moba:
                for i in range(2):
                    sel_stage1(i)

            for t in range(NT):
                bank = [2, 3, 1][t % 3]
                b = t % 2
                if moba and t == min(4, NT - 1):
                    for i in range(2):
                        sel_gate(i)
                        side.extend(sel_chain(i))
                        side.extend(sel_final_thunks(i))
                for c in range(NCH):
                    P.add("pe", lambda e, c=c, t=t, bank=bank: e.matmul(
                        ps[bank][:, 0:256], lhsT=hT[:, c, t * 128:(t + 1) * 128], rhs=wbf[:, c, 256:512],
                        start=(c == 0), stop=(c == NCH - 1)),
                        reads=["wbf", ("hT", t // 4)], writes=[psr(bank)])
                if moba:
                    P.add("dve", lambda e, t=t, bank=bank: e.tensor_copy(
                        out=Vt[:, t, :].rearrange("p (i c) -> p i c", c=65)[:, :, 0:64],
                        in_=ps[bank][:, 0:128].rearrange("p (i c) -> p i c", c=64)),
                        writes=[psr(bank), "Vt"])
                    P.add("act", lambda e, t=t, bank=bank: e.activation(out=Gt[:, t, :], in_=ps[bank][:, 128:256], func=AF.Silu),
                          writes=[psr(bank), "Gt"])
                else:
                    P.add("dve", lambda e, t=t, bank=bank: e.tensor_copy(out=Vt[:, t, 0:128], in_=ps[bank][:, 0:128]),
                          writes=[psr(bank), "Vt"])
                    P.add("act", lambda e, b=b, bank=bank: e.activation(out=gtmp[b][:], in_=ps[bank][:, 128:256], func=AF.Silu),
                          writes=[psr(bank), ("gtmp", b)])
                    P.add("dve", lambda e, t=t, b=b: e.tensor_tensor(out=Gt[:, t, :], in0=gtmp[b][:], in1=subbc[:], op=ALU.mult),
                          reads=[("gtmp", b), "subbc"], writes=["Gt"])
                for _ in range(4):
                    if side:
                        side.pop(0)()
            while side:
                side.pop(0)()

            slopes_all = MOBA_SLOPES + DIFF_SLOPES
            iters = []
            for g in range(NG):
                for i in range(2):
                    sl = slopes_all[heads[i]]
                    glist = []
                    for kt in range(0, 4 * (g + 1)):
                        j = kt - 4 * g
                        s0 = max(0, j)
                        s1 = s0
                        for sq_ in range(s0, 4):
                            tq = 4 * g + sq_
                            dmin = 0 if tq == kt else (tq * 128 - (kt * 128 + 127))
                            if sl * dmin <= ALIBI_FAR:
                                s1 = sq_ + 1
                        if s1 > s0:
                            glist.append(dict(g=g, i=i, kt=kt, s0=s0, s1=s1))
                    started = set()
                    for it in glist:
                        it["startbank"] = {}
                        for sq_ in range(it["s0"], it["s1"]):
                            bk = sq_ // 2
                            if bk not in started:
                                started.add(bk)
                                it["startbank"][sq_] = True
                    glist[-1]["last"] = True
                    assert glist[-1]["kt"] == 4 * g + 3 and started == {0, 1}
                    iters.extend(glist)
            SB = [0, 1, 6]
            PIPE = 2

            def emit_S(n):
                it = iters[n]
                g, i, kt, s0, s1 = it["g"], it["i"], it["kt"], it["s0"], it["s1"]
                j = kt - 4 * g
                sbk = SB[n % 3]
                P.add("pe", lambda e: e.matmul(
                    ps[sbk][:, s0 * 128:s1 * 128], lhsT=KT[i][0:KR, kt * 128:(kt + 1) * 128],
                    rhs=QT[i][0:KR, g * 512 + s0 * 128:g * 512 + s1 * 128], start=True, stop=(j < 0)),
                    reads=[("KT", i, kt // 4), ("KTaug", i), ("QT", i, g), ("QTaug", i), ("QTsel", i)],
                    writes=[psr(sbk)])
                if j >= 0:
                    P.add("pe", lambda e: e.matmul(ps[sbk][:, j * 128:(j + 1) * 128], lhsT=ident[:],
                                                   rhs=cmask[:], start=False, stop=True),
                          reads=["ident", "cmask"], writes=[psr(sbk)])

            def emit_rest(n):
                it = iters[n]
                g, i, kt, s0, s1 = it["g"], it["i"], it["kt"], it["s0"], it["s1"]
                sbk = SB[n % 3]
                slot = n % NPT
                oset = (g * 2 + i) % 2
                ob = [2 + 2 * oset, 3 + 2 * oset]
                P.add("act", lambda e: e.activation(
                    out=PT[slot][:, s0 * 128:s1 * 128], in_=ps[sbk][:, s0 * 128:s1 * 128], func=AF.Exp),
                    writes=[psr(sbk), ("PT", slot)])
                for s in range(s0, s1):
                    bank = ob[s // 2]
                    col = (s % 2) * dvw
                    vlo = i * 65 if moba else 0
                    P.add("pe", lambda e, s=s, bank=bank, col=col, vlo=vlo: e.matmul(
                        ps[bank][:, col:col + dvw], lhsT=PT[slot][:, s * 128:(s + 1) * 128],
                        rhs=Vt[:, kt, vlo:vlo + dvw], start=bool(it["startbank"].get(s, False)), stop=(kt == 4 * g + s),
                        skip_group_check=True),
                        reads=[("PT", slot), "Vt"], writes=[psr(bank)])
                if it.get("last"):
                    pend.append((n + EPI_DELAY, (u, g, i, ob, dvw, moba)))

            NI = len(iters)
            pend = []
            EPI_DELAY = 2
            for n in range(min(PIPE, NI)):
                emit_S(n)
            for n in range(NI):
                if n + PIPE < NI:
                    emit_S(n + PIPE)
                emit_rest(n)
                while pend and pend[0][0] <= n:
                    epilogue(*pend.pop(0)[1])
            while pend:
                epilogue(*pend.pop(0)[1])

        def epilogue(u, g, i, ob, dvw, moba):
            mb = g % 2
            if (not moba) and i == 1:
                k4 = cnt["s4"] % 16
                cnt["s4"] += 1
                c4 = 4 * k4
            for s in range(4):
                t = 4 * g + s
                bank = ob[s // 2]
                col = (s % 2) * dvw
                c0 = scol()
                if moba:
                    P.add("dve", lambda e, bank=bank, col=col, c0=c0: e.reciprocal(out=small[:, c0:c0 + 1],
                                                                                   in_=ps[bank][:, col + 64:col + 65]),
                          writes=[psr(bank), ("small", c0)])
                    P.add("dve", lambda e, bank=bank, col=col, c0=c0, s=s, t=t, mb=mb, i=i: e.scalar_tensor_tensor(
                        out=mo[mb][:, s, i * 64:(i + 1) * 64], in0=ps[bank][:, col:col + 64], scalar=small[:, c0:c0 + 1],
                        in1=Gt[:, t, i * 64:(i + 1) * 64], op0=ALU.mult, op1=ALU.mult),
                        reads=[("small", c0), "Gt"], writes=[psr(bank), ("mo", mb)])
                elif i == 0:
                    P.add("dve", lambda e, bank=bank, col=col, c0=c0: e.reciprocal(out=small[:, c0:c0 + 1],
                                                                                   in_=ps[bank][:, col + 128:col + 129]),
                          writes=[psr(bank), ("small", c0)])
                    P.add("dve", lambda e, bank=bank, col=col, c0=c0, s=s: e.tensor_scalar(
                        out=abuf[:, s, :], in0=ps[bank][:, col:col + 128], scalar1=small[:, c0:c0 + 1], scalar2=None,
                        op0=ALU.mult),
                        reads=[("small", c0)], writes=[psr(bank), ("abuf", s)])
                else:
                    P.add("dve", lambda e, bank=bank, col=col, c0=c0: e.reciprocal(out=small[:, c0:c0 + 1],
                                                                                   in_=ps[bank][:, col + 128:col + 129]),
                          writes=[psr(bank), ("small", c0)])
                    P.add("dve", lambda e, c0=c0: e.tensor_tensor(out=small[:, c0:c0 + 1], in0=small[:, c0:c0 + 1],
                                                                 in1=lamt[:, 4:5], op=ALU.mult),
                          reads=[("small", c0), "neglam"], writes=[("small", c0)])
                    P.add("dve", lambda e, bank=bank, col=col, c0=c0, s=s: e.scalar_tensor_tensor(
                        out=dbuf[:, s, :], in0=ps[bank][:, col:col + 128], scalar=small[:, c0:c0 + 1], in1=abuf[:, s, :],
                        op0=ALU.mult, op1=ALU.add),
                        reads=[("small", c0), ("abuf", s)], writes=[psr(bank), ("dbuf", s)])
                    P.add("dve", lambda e, s=s, c4=c4: e.scalar_tensor_tensor(
                        out=junkf[:], in0=dbuf[:, s, :], scalar=1.0, in1=dbuf[:, s, :], op0=ALU.mult, op1=ALU.mult,
                        accum_out=small4[:, c4 + s:c4 + s + 1]),
                        reads=[("dbuf", s)], writes=["junkf", ("small4", c4)])
            if (not moba) and i == 1:
                P.add("act", lambda e, c4=c4: e.activation(out=small4[:, c4:c4 + 4], in_=small4[:, c4:c4 + 4], func=AF.Ln,
                                                          scale=1.0 / 128, bias=EPS),
                      reads=[("small4", c4)], writes=[("small4", c4)])
                P.add("act", lambda e, c4=c4: e.activation(out=small4[:, c4:c4 + 4], in_=small4[:, c4:c4 + 4], func=AF.Exp,
                                                          scale=-0.5),
                      reads=[("small4", c4)], writes=[("small4", c4)])
                for s in range(4):
                    t = 4 * g + s
                    P.add("dve", lambda e, s=s, t=t, mb=mb, c4=c4: e.scalar_tensor_tensor(
                        out=mo[mb][:, s, :], in0=dbuf[:, s, :], scalar=small4[:, c4 + s:c4 + s + 1], in1=Gt[:, t, :],
                        op0=ALU.mult, op1=ALU.mult),
                        reads=[("dbuf", s), ("small4", c4), "Gt"], writes=[("mo", mb)])
            if i == 1:
                P.add("sp", lambda e, g=g, mb=mb, u=u: e.dma_start(
                    out=mix_d[g * 512:(g + 1) * 512, u * 128:(u + 1) * 128].rearrange("(s p) c -> p s c", p=128),
                    in_=mo[mb][:, :, :]),
                    reads=[("mo", mb)], writes=[("mixd", g)], dma=True)

        for l in range(L):
            layer(l)
        P.add("sp", None, reads=[("outd", t) for t in range(NT)])
        print('sbuf bytes remaining', nc.sbuf_bytes_remaining)
        P.emit(nc, block, sems, dsems)
    return nc


def _host_layout(S, inputs):
    w_in = np.asarray(inputs["w_in"], dtype=np.float32)
    w_out = np.asarray(inputs["w_out"], dtype=np.float32)
    L = w_in.shape[0]
    NT = S // 128
    w_in_r = np.empty((L, 8, 128, NCH, 512), np.float32)
    for l in range(L):
        wl = w_in[l].reshape(NCH, 128, 4096)
        for u in range(8):
            base = 0 if u < 4 else 2048
            idx = u % 4
            for k in range(4):
                c0 = base + k * 512 + idx * 128
                w_in_r[l, u, :, :, k * 128:(k + 1) * 128] = wl[:, :, c0:c0 + 128].transpose(1, 0, 2)
    w_in_r = np.ascontiguousarray(w_in_r.reshape(L, 8, 128, NCH * 512))
    w_out_r = np.ascontiguousarray(w_out.reshape(L, NCH, 128, D).transpose(0, 2, 1, 3).reshape(L, 128, NCH * D))
    gcol = np.stack([np.stack([np.tile(np.asarray(inputs[k], np.float32)[l], 2) for k in
                               ("moba_q_norm", "moba_k_norm", "diff_q_norm", "diff_k_norm")], axis=1)
                     for l in range(L)], axis=0).astype(np.float32)
    lamv = np.concatenate([np.asarray(inputs[k], np.float32) for k in
                           ("lambda_q1", "lambda_k1", "lambda_q2", "lambda_k2")], axis=1)
    pos = np.arange(S)
    ident = np.eye(128, dtype=np.float32)
    kk = np.arange(128)[:, None]
    qq = np.arange(128)[None, :]
    cmask = np.where(kk <= qq, 0.0, NEGM).astype(np.float32)
    bones = np.kron(np.eye(2, dtype=np.float32), np.ones((64, 64), np.float32))
    kaug = np.zeros((20, S), np.float32)
    kaug[0:16] = (pos[None, :] // 256 == np.arange(16)[:, None]).astype(np.float32)
    kaug[16] = pos // 128
    kaug[17] = pos % 128
    kaug[18] = 1.0
    kaug[19] = 1.0
    qaug = np.zeros((12, 4, S), np.float32)
    for h, sl in enumerate(MOBA_SLOPES + DIFF_SLOPES):
        qaug[h, 0] = sl * 128.0
        qaug[h, 1] = sl
        qaug[h, 2] = -sl * 128.0 * (pos // 128)
        qaug[h, 3] = -sl * (pos % 128)
    gb = np.zeros((NT, 16), np.float32)
    for t in range(NT):
        own = t // 2
        gb[t, own] = 1e30
        gb[t, own + 1:] = -1e30
    common = {
        "w_in_r": w_in_r, "w_out_r": w_out_r,
        "norm_g": np.ascontiguousarray(np.asarray(inputs["norm_g"], np.float32)),
        "gcol": np.ascontiguousarray(gcol),
        "subln": np.ascontiguousarray(np.asarray(inputs["diff_subln"], np.float32)),
        "lamv": np.ascontiguousarray(lamv),
        "c_ident": ident, "c_cmask": cmask, "c_bones": bones, "c_kaug": kaug, "c_qaug": qaug,
        "c_gb": gb.reshape(1, NT * 16),
    }
    return L, common


def kernel(x, norm_g, w_in, moba_q_norm, moba_k_norm, diff_q_norm, diff_k_norm,
           lambda_q1, lambda_k1, lambda_q2, lambda_k2, diff_subln, w_out):
    inputs = dict(x=x, norm_g=norm_g, w_in=w_in, moba_q_norm=moba_q_norm, moba_k_norm=moba_k_norm,
                  diff_q_norm=diff_q_norm, diff_k_norm=diff_k_norm, lambda_q1=lambda_q1, lambda_k1=lambda_k1,
                  lambda_q2=lambda_q2, lambda_k2=lambda_k2, diff_subln=diff_subln, w_out=w_out)
    x = np.asarray(x, dtype=np.float32)
    B, S, _ = x.shape
    L, common = _host_layout(S, inputs)
    lambda_inits = [0.8 - 0.6 * math.exp(-0.3 * l) for l in range(L)]
    nc = build(S, L, lambda_inits)
    in_maps = [dict(common, x=np.ascontiguousarray(x[b])) for b in range(B)]
    res = run_bass_kernel_spmd(nc, in_maps, core_ids=list(range(B)))
    return np.stack([np.asarray(r["out"], dtype=np.float32) for r in res.results], axis=0)
```

```python
import math
import numpy as np
import concourse.bass as bass
import concourse.mybir as mybir
from concourse.bass_utils import run_bass_kernel_spmd

F32 = mybir.dt.float32
BF16 = mybir.dt.bfloat16
AF = mybir.ActivationFunctionType
ALU = mybir.AluOpType
AX = mybir.AxisListType

D = 1024
NCH = 8
EPS = 1e-6
NEGM = -30000.0
KR = 84
MOBA_SLOPES = [2.0 ** (-8.0 * i / 8) for i in range(1, 9)]
DIFF_SLOPES = [2.0 ** (-8.0 * i / 4) for i in range(1, 5)]
NDSEM = 8
ALIBI_FAR = 45.0


class Prog:
    def __init__(self):
        self.ops = []
        self.last_w = {}
        self.readers = {}
        self.dma_hist = {"sp": [], "pool": []}

    def add(self, eng, fn, reads=(), writes=(), dma=False):
        idx = len(self.ops)
        raw = set()
        deps = set()
        for r in reads:
            if r in self.last_w:
                raw.add(self.last_w[r])
        for w in writes:
            if w in self.last_w:
                deps.add(self.last_w[w])
            for rd in self.readers.get(w, ()):
                deps.add(rd)
        deps |= raw
        keep = set()
        for j in deps:
            oj = self.ops[j]
            if oj["dma"]:
                keep.add(j)
            elif oj["eng"] != eng:
                keep.add(j)
            elif (j in raw) and eng != "pe" and not dma:
                keep.add(j)
            elif dma:
                keep.add(j)
        op = dict(eng=eng, fn=fn, dma=dma, deps=keep, sig=dma, sem=None, val=None)
        if dma:
            h = self.dma_hist[eng]
            n = len(h)
            if n >= NDSEM:
                op["deps"].add(h[n - NDSEM])
            op["slot"] = n % NDSEM
            op["val"] = 16 * (n // NDSEM + 1)
            h.append(idx)
        for w in writes:
            self.last_w[w] = idx
            self.readers[w] = []
        for r in reads:
            self.readers.setdefault(r, []).append(idx)
        self.ops.append(op)
        return idx

    def emit(self, nc, block, sems, dsems):
        ops = self.ops
        for op in ops:
            for j in op["deps"]:
                ops[j]["sig"] = True
        cnt = {e: 0 for e in sems}
        for op in ops:
            if op["dma"]:
                op["sem"] = dsems[op["eng"]][op["slot"]]
            elif op["sig"]:
                cnt[op["eng"]] += 1
                op["sem"] = sems[op["eng"]]
                op["val"] = cnt[op["eng"]]

        def run(engname, eng):
            waited = {}
            for op in ops:
                if op["eng"] != engname:
                    continue
                need = {}
                for j in op["deps"]:
                    oj = ops[j]
                    key = id(oj["sem"])
                    if waited.get(key, 0) >= oj["val"]:
                        continue
                    if key not in need or need[key][1] < oj["val"]:
                        need[key] = (oj["sem"], oj["val"])
                for key, (sem, val) in need.items():
                    eng.wait_ge(sem, val)
                    waited[key] = val
                if op["fn"] is None:
                    continue
                ins = op["fn"](eng)
                if op["dma"]:
                    ins.then_inc(op["sem"], 16)
                elif op["sig"]:
                    ins.then_inc(op["sem"], 1)

        @block.tensor
        def _(e):
            run("pe", e)

        @block.scalar
        def _(e):
            run("act", e)

        @block.vector
        def _(e):
            run("dve", e)

        @block.gpsimd
        def _(e):
            run("pool", e)

        @block.sync
        def _(e):
            run("sp", e)


def build(S, L, lambda_inits):
    NT = S // 128
    NG = S // 512
    NB = S // 256
    assert S % 512 == 0 and NT <= 32
    nc = bass.Bass("TRN2", target_bir_lowering=False)

    def dram(name, shape, dt, kind):
        return nc.dram_tensor(name, list(shape), dt, kind=kind).ap()

    x_d = dram("x", [S, D], F32, "ExternalInput")
    w_in_d = dram("w_in_r", [L, 8, 128, NCH * 512], F32, "ExternalInput")
    w_out_d = dram("w_out_r", [L, 128, NCH * D], F32, "ExternalInput")
    ng_d = dram("norm_g", [L, D], F32, "ExternalInput")
    gcol_d = dram("gcol", [L, 128, 4], F32, "ExternalInput")
    sub_d = dram("subln", [L, 128], F32, "ExternalInput")
    lamv_d = dram("lamv", [L, 256], F32, "ExternalInput")
    ident_d = dram("c_ident", [128, 128], F32, "ExternalInput")
    cm_d = dram("c_cmask", [128, 128], F32, "ExternalInput")
    bones_d = dram("c_bones", [128, 128], F32, "ExternalInput")
    kaug_d = dram("c_kaug", [20, S], F32, "ExternalInput")
    qaug_d = dram("c_qaug", [12, 4, S], F32, "ExternalInput")
    gb_d = dram("c_gb", [1, NT * 16], F32, "ExternalInput")
    out_d = dram("out", [S, D], F32, "ExternalOutput")
    x1_d = dram("x1_scratch", [S, D], F32, "Internal")
    mix_d = dram("mix_scratch", [S, D], BF16, "Internal")

    from contextlib import ExitStack
    es = ExitStack()

    def sb(name, shape, dt):
        return es.enter_context(nc.sbuf_tensor(name, list(shape), dt))

    def pst(name, shape, dt):
        return es.enter_context(nc.psum_tensor(name, list(shape), dt))

    with es:
        hT = sb("hT", [128, NCH, S], BF16)
        xt = [sb(f"xt{i}", [128, D], F32) for i in range(3)]
        hb = [sb(f"hb{i}", [128, D], BF16) for i in range(2)]
        junk = sb("junk", [128, D], BF16)
        junk2 = sb("junk2", [128, D], BF16)
        gbc = sb("gbc", [128, D], F32)
        wbf = sb("wbf", [128, NCH, 512], BF16)
        wout = sb("wout", [128, NCH, D], BF16)
        QT = [sb(f"QT{i}", [128, S], BF16) for i in range(2)]
        KT = [sb(f"KT{i}", [128, S], BF16) for i in range(2)]
        Vt = sb("Vt", [128, NT, 130], BF16)
        Gt = sb("Gt", [128, NT, 128], BF16)
        gtmp = [sb(f"gtmp{i}", [128, 128], F32) for i in range(2)]
        sq = [sb(f"sq{i}", [128, 512], BF16) for i in range(2)]
        rs = [sb(f"rs{i}", [128, 512], F32) for i in range(2)]
        NPT = 4
        PT = [sb(f"PT{i}", [128, 512], BF16) for i in range(NPT)]
        gm = [sb(f"gm{i}", [128, NT, 16], F32) for i in range(2)]
        top8 = sb("top8", [128, NT, 8], F32)
        thr = sb("thr", [128, NT], F32)
        sel = sb("sel", [128, NT, 16], F32)
        selb = [sb(f"selb{i}", [128, NT, 16], BF16) for i in range(2)]
        km = [sb(f"km{i}", [64, 16], F32) for i in range(2)]
        kmb = [sb(f"kmb{i}", [64, 16], BF16) for i in range(2)]
        mo = [sb(f"mo{i}", [128, 4, 128], BF16) for i in range(2)]
        abuf = sb("abuf", [128, 4, 128], F32)
        dbuf = sb("dbuf", [128, 4, 128], F32)
        junkf = sb("junkf", [128, 128], F32)
        small4 = sb("small4", [128, 64], F32)
        small = sb("small", [128, 64], F32)
        mt = [sb(f"mt{i}", [128, D], BF16) for i in range(2)]
        mT = [sb(f"mT{i}", [128, NCH, 128], BF16) for i in range(2)]
        ident = sb("ident", [128, 128], BF16)
        cmask = sb("cmask", [128, 128], BF16)
        bones = sb("bones", [128, 128], BF16)
        gbt = sb("gbt", [128, NT * 16], F32)
        gcol = sb("gcol_s", [128, 4], F32)
        qcol = sb("qcol_s", [128, 4], F32)
        subbc = sb("subbc", [128, 128], F32)
        lamv = sb("lamv_s", [128, 256], F32)
        lamt = sb("lamt", [128, 8], F32)

        ps = [pst(f"ps{i}", [128, 512], F32) for i in range(7)]
        tp = pst("tp", [128, 1024], BF16)

        sems = {e: es.enter_context(nc.semaphore(f"sem_{e}")) for e in ["pe", "act", "dve", "pool", "sp"]}
        dsems = {q: [es.enter_context(nc.semaphore(f"dsem_{q}{i}")) for i in range(NDSEM)] for q in ["sp", "pool"]}
        block = es.enter_context(nc.Block())

        P = Prog()
        cnt = {"s": 0, "pt": 0, "small": 0, "pj": 0, "s4": 0, "tpb": 0}

        def psr(k):
            return ("ps", k)

        def scol():
            c = cnt["small"] % 64
            cnt["small"] += 1
            return c

        P.add("pool", lambda e: e.dma_start(out=ident[:], in_=ident_d[:, :]), writes=["ident"], dma=True)
        P.add("pool", lambda e: e.dma_start(out=cmask[:], in_=cm_d[:, :]), writes=["cmask"], dma=True)
        P.add("pool", lambda e: e.dma_start(out=bones[:], in_=bones_d[:, :]), writes=["bones"], dma=True)
        P.add("sp", lambda e: e.dma_start(out=gbt[:], in_=gb_d[:, :].to_broadcast([128, NT * 16])), writes=["gbt"], dma=True)
        for i in range(2):
            P.add("pool", lambda e, i=i: e.dma_start(out=KT[i][64:84, :], in_=kaug_d[:, :]),
                  writes=[("KTaug", i)], dma=True)
        for i in range(2):
            P.add("dve", lambda e, i=i: e.memset(kmb[i][:], 0.0), writes=[("kmb", i)])

        def layer(l):
            src = x_d if l == 0 else x1_d
            dst = out_d if l == L - 1 else x1_d
            srcn = "xd" if l == 0 else "x1d"
            dstn = "outd" if l == L - 1 else "x1d"
            li = lambda_inits[l]

            if l == 0:
                P.add("sp", lambda e: e.dma_start(out=gbc[:], in_=ng_d[l:l + 1, :].to_broadcast([128, D])),
                      writes=["gbc"], dma=True)
            P.add("sp", lambda e: e.dma_start(out=gcol[:], in_=gcol_d[l, :, :]), writes=["gcol"], dma=True)
            P.add("sp", lambda e: e.dma_start(out=subbc[:], in_=sub_d[l:l + 1, :].to_broadcast([128, 128])),
                  writes=["subbc"], dma=True)
            P.add("sp", lambda e: e.dma_start(out=lamv[:], in_=lamv_d[l:l + 1, :].to_broadcast([128, 256])),
                  writes=["lamv"], dma=True)
            P.add("pool", lambda e: e.dma_start(out=wout[:].rearrange("p c n -> p (c n)"), in_=w_out_d[l, :, :]),
                  writes=["wout"], dma=True)
            P.add("dve", lambda e: e.tensor_scalar(out=qcol[:], in0=gcol[:], scalar1=0.125, scalar2=None, op0=ALU.mult),
                  reads=["gcol"], writes=["qcol"])
            P.add("dve", lambda e: e.tensor_scalar(out=subbc[:], in0=subbc[:], scalar1=float(1.0 - li), scalar2=None,
                                                   op0=ALU.mult), reads=["subbc"], writes=["subbc"])
            P.add("dve", lambda e: e.tensor_tensor(out=lamv[:, 0:64], in0=lamv[:, 0:64], in1=lamv[:, 64:128], op=ALU.mult),
                  reads=["lamv"], writes=["lamv"])
            P.add("dve", lambda e: e.tensor_tensor(out=lamv[:, 128:192], in0=lamv[:, 128:192], in1=lamv[:, 192:256],
                                                   op=ALU.mult), reads=["lamv"], writes=["lamv"])
            P.add("dve", lambda e: e.tensor_reduce(out=lamt[:, 0:1], in_=lamv[:, 0:64], axis=AX.X, op=ALU.add),
                  reads=["lamv"], writes=["lamt"])
            P.add("dve", lambda e: e.tensor_reduce(out=lamt[:, 1:2], in_=lamv[:, 128:192], axis=AX.X, op=ALU.add),
                  reads=["lamv", "lamt"], writes=["lamt"])
            P.add("act", lambda e: e.activation(out=lamt[:, 2:4], in_=lamt[:, 0:2], func=AF.Exp),
                  reads=["lamt"], writes=["lamt"])
            P.add("dve", lambda e: e.scalar_tensor_tensor(out=lamt[:, 4:5], in0=lamt[:, 3:4], scalar=float(-li),
                                                          in1=lamt[:, 2:3], op0=ALU.add, op1=ALU.subtract),
                  reads=["lamt"], writes=["neglam"])

            cols0 = {}

            def p0_A(t):
                b3 = t % 3
                P.add("sp", lambda e: e.dma_start(out=xt[b3][:], in_=src[t * 128:(t + 1) * 128, :]),
                      reads=[(srcn, t)], writes=[("xt", b3)], dma=True)
                c0 = scol()
                cols0[t] = c0
                jk = junk if t % 2 == 0 else junk2
                P.add("act", lambda e: e.activation(out=jk[:], in_=xt[b3][:], func=AF.Square,
                                                    accum_out=small[:, c0:c0 + 1]),
                      reads=[("xt", b3)], writes=[("junk", t % 2), ("small", c0)])

            def p0_B(t):
                b3 = t % 3
                b = t % 2
                c0 = cols0[t]
                P.add("act", lambda e: e.activation(out=small[:, c0:c0 + 1], in_=small[:, c0:c0 + 1], func=AF.Ln,
                                                    scale=1.0 / D, bias=EPS),
                      reads=[("small", c0)], writes=[("small", c0)])
                P.add("act", lambda e: e.activation(out=small[:, c0:c0 + 1], in_=small[:, c0:c0 + 1], func=AF.Exp,
                                                    scale=-0.5),
                      reads=[("small", c0)], writes=[("small", c0)])
                P.add("dve", lambda e: e.scalar_tensor_tensor(out=hb[b][:], in0=xt[b3][:], scalar=small[:, c0:c0 + 1],
                                                              in1=gbc[:], op0=ALU.mult, op1=ALU.mult),
                      reads=[("xt", b3), ("small", c0), "gbc"], writes=[("hb", b)])

            def p0_B2(t):
                b = t % 2
                for c in range(NCH):
                    P.add("pe", lambda e, c=c: e.transpose(tp[:, c * 128:(c + 1) * 128], hb[b][:, c * 128:(c + 1) * 128],
                                                           ident[:]),
                          reads=[("hb", b), "ident"], writes=["tp"])

            def p0_C(t):
                P.add("dve", lambda e: e.tensor_copy(out=hT[:, :, t * 128:(t + 1) * 128],
                                                     in_=tp[:, :].rearrange("p (c n) -> p c n", n=128)),
                      writes=["tp", ("hT", t // 4)])

            if l == 0:
                p0_A(0)
                for t in range(NT):
                    if t + 1 < NT:
                        p0_A(t + 1)
                    p0_B(t)
                    if t >= 1:
                        p0_C(t - 1)
                    p0_B2(t)
                p0_C(NT - 1)

            for u in range(8):
                unit(l, u, li)

            def p2_load(t):
                b = t % 2
                b3 = t % 3
                P.add("sp", lambda e: e.dma_start(out=mt[b][:], in_=mix_d[t * 128:(t + 1) * 128, :]),
                      reads=[("mixd", t // 4)], writes=[("mt", b)], dma=True)
                P.add("sp", lambda e: e.dma_start(out=xt[b3][:], in_=src[t * 128:(t + 1) * 128, :]),
                      reads=[(srcn, t)], writes=[("xt", b3)], dma=True)

            fuse = l < L - 1
            if fuse:
                P.add("sp", lambda e: e.dma_start(out=gbc[:], in_=ng_d[l + 1:l + 2, :].to_broadcast([128, D])),
                      writes=["gbc"], dma=True)
            tpn = ps[5][:, :].bitcast(BF16)
            fcols = {}

            def f_act(t):
                b3 = t % 3
                c0 = scol()
                fcols[t] = c0
                jk = junk if t % 2 == 0 else junk2
                P.add("act", lambda e: e.activation(out=jk[:], in_=xt[b3][:], func=AF.Square,
                                                    accum_out=small[:, c0:c0 + 1]),
                      reads=[("xt", b3)], writes=[("junk", t % 2), ("small", c0)])
                P.add("act", lambda e: e.activation(out=small[:, c0:c0 + 1], in_=small[:, c0:c0 + 1], func=AF.Ln,
                                                    scale=1.0 / D, bias=EPS),
                      reads=[("small", c0)], writes=[("small", c0)])
                P.add("act", lambda e: e.activation(out=small[:, c0:c0 + 1], in_=small[:, c0:c0 + 1], func=AF.Exp,
                                                    scale=-0.5),
                      reads=[("small", c0)], writes=[("small", c0)])

            def f_stt(t):
                b3 = t % 3
                b = t % 2
                c0 = fcols[t]
                P.add("dve", lambda e: e.scalar_tensor_tensor(out=hb[b][:], in0=xt[b3][:], scalar=small[:, c0:c0 + 1],
                                                              in1=gbc[:], op0=ALU.mult, op1=ALU.mult),
                      reads=[("xt", b3), ("small", c0), "gbc"], writes=[("hb", b)])

            def f_tr(t):
                b = t % 2
                for c in range(NCH):
                    P.add("pe", lambda e, c=c: e.transpose(tpn[:, c * 128:(c + 1) * 128], hb[b][:, c * 128:(c + 1) * 128],
                                                           ident[:]),
                          reads=[("hb", b), "ident"], writes=[psr(5)])
                P.add("dve", lambda e: e.tensor_copy(out=hT[:, :, t * 128:(t + 1) * 128],
                                                     in_=tpn.rearrange("p (c n) -> p c n", n=128)),
                      writes=[psr(5), ("hT", t // 4)])

            p2_load(0)
            for t in range(NT):
                b = t % 2
                b3 = t % 3
                if t + 1 < NT:
                    p2_load(t + 1)
                for c in range(NCH):
                    P.add("pe", lambda e, b=b, c=c: e.transpose(tp[:, c * 128:(c + 1) * 128], mt[b][:, c * 128:(c + 1) * 128],
                                                                ident[:]),
                          reads=[("mt", b), "ident"], writes=["tp"])
                P.add("act", lambda e, b=b: e.copy(out=mT[b][:, :, :], in_=tp[:, :].rearrange("p (c n) -> p c n", n=128)),
                      writes=["tp", ("mT", b)])
                if fuse and t >= 1:
                    f_act(t - 1)
                    f_stt(t - 1)
                banks = [6, 0] if t % 2 == 0 else [1, 2]
                for half in range(2):
                    bank = banks[half]
                    for c in range(NCH):
                        P.add("pe", lambda e, b=b, c=c, half=half, bank=bank: e.matmul(
                            ps[bank][:, :], lhsT=mT[b][:, c, :], rhs=wout[:, c, half * 512:(half + 1) * 512],
                            start=(c == 0), stop=(c == NCH - 1)),
                            reads=[("mT", b), "wout"], writes=[psr(bank)])
                if fuse and t >= 1:
                    f_tr(t - 1)
                for half in range(2):
                    bank = banks[half]
                    P.add("dve", lambda e, b3=b3, half=half, bank=bank: e.tensor_tensor(
                        out=xt[b3][:, half * 512:(half + 1) * 512], in0=xt[b3][:, half * 512:(half + 1) * 512],
                        in1=ps[bank][:, :], op=ALU.add),
                        reads=[("xt", b3)], writes=[psr(bank), ("xt", b3)])
                P.add("pool", lambda e, t=t, b3=b3: e.dma_start(out=dst[t * 128:(t + 1) * 128, :], in_=xt[b3][:]),
                      reads=[("xt", b3)], writes=[(dstn, t)], dma=True)
            if fuse:
                f_act(NT - 1)
                f_stt(NT - 1)
                f_tr(NT - 1)

        def unit(l, u, li):
            moba = u < 4
            dvw = 65 if moba else 129
            heads = [2 * u, 2 * u + 1] if moba else [8 + (u - 4), 8 + (u - 4)]
            P.add("pool", lambda e: e.dma_start(out=wbf[:].rearrange("p c n -> p (c n)"), in_=w_in_d[l, u, :, :]),
                  writes=["wbf"], dma=True)
            for i in range(2):
                P.add("pool", lambda e, i=i: e.dma_start(out=QT[i][80:84, :], in_=qaug_d[heads[i], :, :]),
                      writes=[("QTaug", i)], dma=True)
            if moba:
                P.add("dve", lambda e: e.memset(Vt[:, :, 64:65], 1.0), writes=["Vt"])
                P.add("dve", lambda e: e.memset(Vt[:, :, 129:130], 1.0), writes=["Vt"])
            else:
                P.add("dve", lambda e: e.memset(Vt[:, :, 128:129], 1.0), writes=["Vt"])
                if u == 4:
                    for i in range(2):
                        P.add("dve", lambda e, i=i: e.memset(QT[i][64:80, :], 0.0), writes=[("QTsel", i)])

            groups = [(which, g) for which in range(2) for g in range(NG)]
            PJB = [6, 1, 2, 3]
            SSB = [0, 4]

            def emit_proj(n):
                which, g = groups[n]
                pj = PJB[n % 4]
                for c in range(NCH):
                    P.add("pe", lambda e, c=c: e.matmul(
                        ps[pj][:, :], lhsT=wbf[:, c, which * 128:(which + 1) * 128],
                        rhs=hT[:, c, g * 512:(g + 1) * 512], start=(c == 0), stop=(c == NCH - 1)),
                        reads=["wbf", ("hT", g)], writes=[psr(pj)])

            def emit_chainA(n):
                pj = PJB[n % 4]
                b = n % 2
                ssb = SSB[n % 2]
                P.add("act", lambda e: e.activation(out=sq[b][:], in_=ps[pj][:, :], func=AF.Square),
                      writes=[psr(pj), ("sq", b)])
                P.add("pe", lambda e: e.matmul(ps[ssb][:, :], lhsT=bones[:], rhs=sq[b][:], start=True, stop=True),
                      reads=["bones", ("sq", b)], writes=[psr(ssb)])

            def emit_chainB(n):
                which, g = groups[n]
                pj = PJB[n % 4]
                b = n % 2
                ssb = SSB[n % 2]
                T = QT if which == 0 else KT
                tname = "QT" if which == 0 else "KT"
                colt = qcol if which == 0 else gcol
                cidx = (0 if moba else 2) + which
                P.add("act", lambda e: e.activation(out=rs[b][:], in_=ps[ssb][:, :], func=AF.Ln, scale=1.0 / 64, bias=EPS),
                      writes=[psr(ssb), ("rs", b)])
                P.add("act", lambda e: e.activation(out=rs[b][:], in_=rs[b][:], func=AF.Exp, scale=-0.5),
                      reads=[("rs", b)], writes=[("rs", b)])
                for i in range(2):
                    if moba and which == 1:
                        for hh in range(2):
                            P.add("dve", lambda e, i=i, hh=hh: e.scalar_tensor_tensor(
                                out=T[i][0:64, g * 512 + hh * 256:g * 512 + (hh + 1) * 256],
                                in0=ps[pj][i * 64:(i + 1) * 64, hh * 256:(hh + 1) * 256],
                                scalar=colt[i * 64:(i + 1) * 64, cidx:cidx + 1],
                                in1=rs[b][i * 64:(i + 1) * 64, hh * 256:(hh + 1) * 256],
                                op0=ALU.mult, op1=ALU.mult, accum_out=km[i][:, 2 * g + hh:2 * g + hh + 1]),
                                reads=[("rs", b), "qcol", "gcol"], writes=[psr(pj), (tname, i, g), ("km", i)])
                    else:
                        P.add("dve", lambda e, i=i: e.scalar_tensor_tensor(
                            out=T[i][0:64, g * 512:(g + 1) * 512], in0=ps[pj][i * 64:(i + 1) * 64, :],
                            scalar=colt[i * 64:(i + 1) * 64, cidx:cidx + 1], in1=rs[b][i * 64:(i + 1) * 64, :],
                            op0=ALU.mult, op1=ALU.mult),
                            reads=[("rs", b), "qcol", "gcol"], writes=[psr(pj), (tname, i, g)])

            NGR = len(groups)
            emit_proj(0)
            if NGR > 1:
                emit_proj(1)
            emit_chainA(0)
            for n in range(NGR):
                if n + 2 < NGR:
                    emit_proj(n + 2)
                if n + 1 < NGR:
                    emit_chainA(n + 1)
                emit_chainB(n)

            side = []

            def sel_stage1(i):
                P.add("dve", lambda e: e.tensor_scalar(out=kmb[i][:, 0:NB], in0=km[i][:, 0:NB], scalar1=1.0 / 256,
                                                       scalar2=None, op0=ALU.mult),
                      reads=[("km", i)], writes=[("kmb", i)])

            def sel_gate(i):
                gbank = 6 if i == 0 else 0
                for t in range(NT):
                    P.add("pe", lambda e, t=t: e.matmul(ps[gbank][:, t * 16:(t + 1) * 16],
                                                        lhsT=QT[i][0:64, t * 128:(t + 1) * 128], rhs=kmb[i][:, :],
                                                        start=True, stop=True),
                          reads=[("QT", i, t // 4), ("kmb", i)], writes=[psr(gbank)])
                P.add("dve", lambda e: e.tensor_tensor(out=gm[i][:].rearrange("p t n -> p (t n)"),
                                                       in0=ps[gbank][:, 0:NT * 16], in1=gbt[:], op=ALU.add),
                      reads=["gbt"], writes=[psr(gbank), ("gm", i)])

            def sel_chain(i):
                th = []
                for t in range(NT):
                    th.append(lambda t=t: P.add("dve", lambda e: e.max(out=top8[:, t, :], in_=gm[i][:, t, :]),
                                                reads=[("gm", i)], writes=["top8"]))
                th.append(lambda: P.add("dve", lambda e: e.tensor_scalar(out=thr[:], in0=top8[:, :, 3], scalar1=-1e29,
                                                                         scalar2=None, op0=ALU.max),
                                        reads=["top8"], writes=["thr"]))
                th.append(lambda: P.add("dve", lambda e: e.tensor_tensor(
                    out=sel[:], in0=gm[i][:], in1=thr[:].unsqueeze(2).to_broadcast([128, NT, 16]), op=ALU.is_ge),
                    reads=[("gm", i), "thr"], writes=["sel"]))
                th.append(lambda: P.add("dve", lambda e: e.tensor_scalar(out=selb[i][:], in0=sel[:], scalar1=-NEGM,
                                                                         scalar2=NEGM, op0=ALU.mult, op1=ALU.add),
                                        reads=["sel"], writes=[("selb", i)]))
                return th

            def sel_final_thunks(i):
                th = []
                for k, t0 in enumerate(range(0, NT, 8)):
                    def f(k=k, t0=t0):
                        w = cnt["tpb"] % 3
                        cnt["tpb"] += 1
                        if w == 0:
                            buf = tp[0:16, :]
                            res = "tp"
                        else:
                            buf = ps[3 + w][0:16, :].bitcast(BF16)
                            res = psr(3 + w)
                        for t in range(t0, t0 + 8):
                            P.add("pe", lambda e, t=t: e.transpose(buf[:, (t - t0) * 128:(t - t0 + 1) * 128],
                                                                   selb[i][:, t, :], ident[:]),
                                  reads=[("selb", i), "ident"], writes=[res])
                        if k % 2 == 0:
                            P.add("act", lambda e: e.copy(out=QT[i][64:80, t0 * 128:(t0 + 8) * 128], in_=buf),
                                  writes=[res, ("QTsel", i)])
                        else:
                            P.add("dve", lambda e: e.tensor_copy(out=QT[i][64:80, t0 * 128:(t0 + 8) * 128], in_=buf),
                                  writes=[res, ("QTsel", i)])
                    th.append(f)
                return th

            if moba:
                for i in range(2):
                    sel_stage1(i)

            for t in range(NT):
                bank = [2, 3, 1][t % 3]
                b = t % 2
                if moba and t == min(4, NT - 1):
                    for i in range(2):
                        sel_gate(i)
                        side.extend(sel_chain(i))
                        side.extend(sel_final_thunks(i))
                for c in range(NCH):
                    P.add("pe", lambda e, c=c, t=t, bank=bank: e.matmul(
                        ps[bank][:, 0:256], lhsT=hT[:, c, t * 128:(t + 1) * 128], rhs=wbf[:, c, 256:512],
                        start=(c == 0), stop=(c == NCH - 1)),
                        reads=["wbf", ("hT", t // 4)], writes=[psr(bank)])
                if moba:
                    P.add("dve", lambda e, t=t, bank=bank: e.tensor_copy(
                        out=Vt[:, t, :].rearrange("p (i c) -> p i c", c=65)[:, :, 0:64],
                        in_=ps[bank][:, 0:128].rearrange("p (i c) -> p i c", c=64)),
                        writes=[psr(bank), "Vt"])
                    P.add("act", lambda e, t=t, bank=bank: e.activation(out=Gt[:, t, :], in_=ps[bank][:, 128:256], func=AF.Silu),
                          writes=[psr(bank), "Gt"])
                else:
                    P.add("dve", lambda e, t=t, bank=bank: e.tensor_copy(out=Vt[:, t, 0:128], in_=ps[bank][:, 0:128]),
                          writes=[psr(bank), "Vt"])
                    P.add("act", lambda e, b=b, bank=bank: e.activation(out=gtmp[b][:], in_=ps[bank][:, 128:256], func=AF.Silu),
                          writes=[psr(bank), ("gtmp", b)])
                    P.add("dve", lambda e, t=t, b=b: e.tensor_tensor(out=Gt[:, t, :], in0=gtmp[b][:], in1=subbc[:], op=ALU.mult),
                          reads=[("gtmp", b), "subbc"], writes=["Gt"])
                for _ in range(4):
                    if side:
                        side.pop(0)()
            while side:
                side.pop(0)()

            slopes_all = MOBA_SLOPES + DIFF_SLOPES
            iters = []
            for g in range(NG):
                for i in range(2):
                    sl = slopes_all[heads[i]]
                    glist = []
                    for kt in range(0, 4 * (g + 1)):
                        j = kt - 4 * g
                        s0 = max(0, j)
                        s1 = s0
                        for sq_ in range(s0, 4):
                            tq = 4 * g + sq_
                            dmin = 0 if tq == kt else (tq * 128 - (kt * 128 + 127))
                            if sl * dmin <= ALIBI_FAR:
                                s1 = sq_ + 1
                        if s1 > s0:
                            glist.append(dict(g=g, i=i, kt=kt, s0=s0, s1=s1))
                    started = set()
                    for it in glist:
                        it["startbank"] = {}
                        for sq_ in range(it["s0"], it["s1"]):
                            bk = sq_ // 2
                            if bk not in started:
                                started.add(bk)
                                it["startbank"][sq_] = True
                    glist[-1]["last"] = True
                    assert glist[-1]["kt"] == 4 * g + 3 and started == {0, 1}
                    iters.extend(glist)
            SB = [0, 1, 6]
            PIPE = 2

            def emit_S(n):
                it = iters[n]
                g, i, kt, s0, s1 = it["g"], it["i"], it["kt"], it["s0"], it["s1"]
                j = kt - 4 * g
                sbk = SB[n % 3]
                P.add("pe", lambda e: e.matmul(
                    ps[sbk][:, s0 * 128:s1 * 128], lhsT=KT[i][0:KR, kt * 128:(kt + 1) * 128],
                    rhs=QT[i][0:KR, g * 512 + s0 * 128:g * 512 + s1 * 128], start=True, stop=(j < 0)),
                    reads=[("KT", i, kt // 4), ("KTaug", i), ("QT", i, g), ("QTaug", i), ("QTsel", i)],
                    writes=[psr(sbk)])
                if j >= 0:
                    P.add("pe", lambda e: e.matmul(ps[sbk][:, j * 128:(j + 1) * 128], lhsT=ident[:],
                                                   rhs=cmask[:], start=False, stop=True),
                          reads=["ident", "cmask"], writes=[psr(sbk)])

            def emit_rest(n):
                it = iters[n]
                g, i, kt, s0, s1 = it["g"], it["i"], it["kt"], it["s0"], it["s1"]
                sbk = SB[n % 3]
                slot = n % NPT
                oset = (g * 2 + i) % 2
                ob = [2 + 2 * oset, 3 + 2 * oset]
                P.add("act", lambda e: e.activation(
                    out=PT[slot][:, s0 * 128:s1 * 128], in_=ps[sbk][:, s0 * 128:s1 * 128], func=AF.Exp),
                    writes=[psr(sbk), ("PT", slot)])
                for s in range(s0, s1):
                    bank = ob[s // 2]
                    col = (s % 2) * dvw
                    vlo = i * 65 if moba else 0
                    P.add("pe", lambda e, s=s, bank=bank, col=col, vlo=vlo: e.matmul(
                        ps[bank][:, col:col + dvw], lhsT=PT[slot][:, s * 128:(s + 1) * 128],
                        rhs=Vt[:, kt, vlo:vlo + dvw], start=bool(it["startbank"].get(s, False)), stop=(kt == 4 * g + s),
                        skip_group_check=True),
                        reads=[("PT", slot), "Vt"], writes=[psr(bank)])
                if it.get("last"):
                    pend.append((n + EPI_DELAY, (u, g, i, ob, dvw, moba)))

            NI = len(iters)
            pend = []
            EPI_DELAY = 2
            for n in range(min(PIPE, NI)):
                emit_S(n)
            for n in range(NI):
                if n + PIPE < NI:
                    emit_S(n + PIPE)
                emit_rest(n)
                while pend and pend[0][0] <= n:
                    epilogue(*pend.pop(0)[1])
            while pend:
                epilogue(*pend.pop(0)[1])

        def epilogue(u, g, i, ob, dvw, moba):
            mb = g % 2
            if (not moba) and i == 1:
                k4 = cnt["s4"] % 16
                cnt["s4"] += 1
                c4 = 4 * k4
            for s in range(4):
                t = 4 * g + s
                bank = ob[s // 2]
                col = (s % 2) * dvw
                c0 = scol()
                if moba:
                    P.add("dve", lambda e, bank=bank, col=col, c0=c0: e.reciprocal(out=small[:, c0:c0 + 1],
                                                                                   in_=ps[bank][:, col + 64:col + 65]),
                          writes=[psr(bank), ("small", c0)])
                    P.add("dve", lambda e, bank=bank, col=col, c0=c0, s=s, t=t, mb=mb, i=i: e.scalar_tensor_tensor(
                        out=mo[mb][:, s, i * 64:(i + 1) * 64], in0=ps[bank][:, col:col + 64], scalar=small[:, c0:c0 + 1],
                        in1=Gt[:, t, i * 64:(i + 1) * 64], op0=ALU.mult, op1=ALU.mult),
                        reads=[("small", c0), "Gt"], writes=[psr(bank), ("mo", mb)])
                elif i == 0:
                    P.add("dve", lambda e, bank=bank, col=col, c0=c0: e.reciprocal(out=small[:, c0:c0 + 1],
                                                                                   in_=ps[bank][:, col + 128:col + 129]),
                          writes=[psr(bank), ("small", c0)])
                    P.add("dve", lambda e, bank=bank, col=col, c0=c0, s=s: e.tensor_scalar(
                        out=abuf[:, s, :], in0=ps[bank][:, col:col + 128], scalar1=small[:, c0:c0 + 1], scalar2=None,
                        op0=ALU.mult),
                        reads=[("small", c0)], writes=[psr(bank), ("abuf", s)])
                else:
                    P.add("dve", lambda e, bank=bank, col=col, c0=c0: e.reciprocal(out=small[:, c0:c0 + 1],
                                                                                   in_=ps[bank][:, col + 128:col + 129]),
                          writes=[psr(bank), ("small", c0)])
                    P.add("dve", lambda e, c0=c0: e.tensor_tensor(out=small[:, c0:c0 + 1], in0=small[:, c0:c0 + 1],
                                                                 in1=lamt[:, 4:5], op=ALU.mult),
                          reads=[("small", c0), "neglam"], writes=[("small", c0)])
                    P.add("dve", lambda e, bank=bank, col=col, c0=c0, s=s: e.scalar_tensor_tensor(
                        out=dbuf[:, s, :], in0=ps[bank][:, col:col + 128], scalar=small[:, c0:c0 + 1], in1=abuf[:, s, :],
                        op0=ALU.mult, op1=ALU.add),
                        reads=[("small", c0), ("abuf", s)], writes=[psr(bank), ("dbuf", s)])
                    P.add("dve", lambda e, s=s, c4=c4: e.scalar_tensor_tensor(
                        out=junkf[:], in0=dbuf[:, s, :], scalar=1.0, in1=dbuf[:, s, :], op0=ALU.mult, op1=ALU.mult,
                        accum_out=small4[:, c4 + s:c4 + s + 1]),
                        reads=[("dbuf", s)], writes=["junkf", ("small4", c4)])
            if (not moba) and i == 1:
                P.add("act", lambda e, c4=c4: e.activation(out=small4[:, c4:c4 + 4], in_=small4[:, c4:c4 + 4], func=AF.Ln,
                                                          scale=1.0 / 128, bias=EPS),
                      reads=[("small4", c4)], writes=[("small4", c4)])
                P.add("act", lambda e, c4=c4: e.activation(out=small4[:, c4:c4 + 4], in_=small4[:, c4:c4 + 4], func=AF.Exp,
                                                          scale=-0.5),
                      reads=[("small4", c4)], writes=[("small4", c4)])
                for s in range(4):
                    t = 4 * g + s
                    P.add("dve", lambda e, s=s, t=t, mb=mb, c4=c4: e.scalar_tensor_tensor(
                        out=mo[mb][:, s, :], in0=dbuf[:, s, :], scalar=small4[:, c4 + s:c4 + s + 1], in1=Gt[:, t, :],
                        op0=ALU.mult, op1=ALU.mult),
                        reads=[("dbuf", s), ("small4", c4), "Gt"], writes=[("mo", mb)])
            if i == 1:
                P.add("sp", lambda e, g=g, mb=mb, u=u: e.dma_start(
                    out=mix_d[g * 512:(g + 1) * 512, u * 128:(u + 1) * 128].rearrange("(s p) c -> p s c", p=128),
                    in_=mo[mb][:, :, :]),
                    reads=[("mo", mb)], writes=[("mixd", g)], dma=True)

        for l in range(L):
            layer(l)
        P.add("sp", None, reads=[("outd", t) for t in range(NT)])
        print('sbuf bytes remaining', nc.sbuf_bytes_remaining)
        P.emit(nc, block, sems, dsems)
    return nc


def _host_layout(S, inputs):
    w_in = np.asarray(inputs["w_in"], dtype=np.float32)
    w_out = np.asarray(inputs["w_out"], dtype=np.float32)
    L = w_in.shape[0]
    NT = S // 128
    w_in_r = np.empty((L, 8, 128, NCH, 512), np.float32)
    for l in range(L):
        wl = w_in[l].reshape(NCH, 128, 4096)
        for u in range(8):
            base = 0 if u < 4 else 2048
            idx = u % 4
            for k in range(4):
                c0 = base + k * 512 + idx * 128
                w_in_r[l, u, :, :, k * 128:(k + 1) * 128] = wl[:, :, c0:c0 + 128].transpose(1, 0, 2)
    w_in_r = np.ascontiguousarray(w_in_r.reshape(L, 8, 128, NCH * 512))
    w_out_r = np.ascontiguousarray(w_out.reshape(L, NCH, 128, D).transpose(0, 2, 1, 3).reshape(L, 128, NCH * D))
    gcol = np.stack([np.stack([np.tile(np.asarray(inputs[k], np.float32)[l], 2) for k in
                               ("moba_q_norm", "moba_k_norm", "diff_q_norm", "diff_k_norm")], axis=1)
                     for l in range(L)], axis=0).astype(np.float32)
    lamv = np.concatenate([np.asarray(inputs[k], np.float32) for k in
                           ("lambda_q1", "lambda_k1", "lambda_q2", "lambda_k2")], axis=1)
    pos = np.arange(S)
    ident = np.eye(128, dtype=np.float32)
    kk = np.arange(128)[:, None]
    qq = np.arange(128)[None, :]
    cmask = np.where(kk <= qq, 0.0, NEGM).astype(np.float32)
    bones = np.kron(np.eye(2, dtype=np.float32), np.ones((64, 64), np.float32))
    kaug = np.zeros((20, S), np.float32)
    kaug[0:16] = (pos[None, :] // 256 == np.arange(16)[:, None]).astype(np.float32)
    kaug[16] = pos // 128
    kaug[17] = pos % 128
    kaug[18] = 1.0
    kaug[19] = 1.0
    qaug = np.zeros((12, 4, S), np.float32)
    for h, sl in enumerate(MOBA_SLOPES + DIFF_SLOPES):
        qaug[h, 0] = sl * 128.0
        qaug[h, 1] = sl
        qaug[h, 2] = -sl * 128.0 * (pos // 128)
        qaug[h, 3] = -sl * (pos % 128)
    gb = np.zeros((NT, 16), np.float32)
    for t in range(NT):
        own = t // 2
        gb[t, own] = 1e30
        gb[t, own + 1:] = -1e30
    common = {
        "w_in_r": w_in_r, "w_out_r": w_out_r,
        "norm_g": np.ascontiguousarray(np.asarray(inputs["norm_g"], np.float32)),
        "gcol": np.ascontiguousarray(gcol),
        "subln": np.ascontiguousarray(np.asarray(inputs["diff_subln"], np.float32)),
        "lamv": np.ascontiguousarray(lamv),
        "c_ident": ident, "c_cmask": cmask, "c_bones": bones, "c_kaug": kaug, "c_qaug": qaug,
        "c_gb": gb.reshape(1, NT * 16),
    }
    return L, common


def kernel(x, norm_g, w_in, moba_q_norm, moba_k_norm, diff_q_norm, diff_k_norm,
           lambda_q1, lambda_k1, lambda_q2, lambda_k2, diff_subln, w_out):
    inputs = dict(x=x, norm_g=norm_g, w_in=w_in, moba_q_norm=moba_q_norm, moba_k_norm=moba_k_norm,
                  diff_q_norm=diff_q_norm, diff_k_norm=diff_k_norm, lambda_q1=lambda_q1, lambda_k1=lambda_k1,
                  lambda_q2=lambda_q2, lambda_k2=lambda_k2, diff_subln=diff_subln, w_out=w_out)
    x = np.asarray(x, dtype=np.float32)
    B, S, _ = x.shape
    L, common = _host_layout(S, inputs)
    lambda_inits = [0.8 - 0.6 * math.exp(-0.3 * l) for l in range(L)]
    nc = build(S, L, lambda_inits)
    in_maps = [dict(common, x=np.ascontiguousarray(x[b])) for b in range(B)]
    res = run_bass_kernel_spmd(nc, in_maps, core_ids=list(range(B)))
    return np.stack([np.asarray(r["out"], dtype=np.float32) for r in res.results], axis=0)
```

```python
import math
import numpy as np
import concourse.bass as bass
import concourse.mybir as mybir
from concourse.bass_utils import run_bass_kernel_spmd

F32 = mybir.dt.float32
BF16 = mybir.dt.bfloat16
AF = mybir.ActivationFunctionType
ALU = mybir.AluOpType
AX = mybir.AxisListType

D = 1024
NCH = 8
EPS = 1e-6
NEGM = -30000.0
KR = 84
MOBA_SLOPES = [2.0 ** (-8.0 * i / 8) for i in range(1, 9)]
DIFF_SLOPES = [2.0 ** (-8.0 * i / 4) for i in range(1, 5)]
NDSEM = 8
ALIBI_FAR = 45.0


class Prog:
    def __init__(self):
        self.ops = []
        self.last_w = {}
        self.readers = {}
        self.dma_hist = {"sp": [], "pool": []}

    def add(self, eng, fn, reads=(), writes=(), dma=False):
        idx = len(self.ops)
        raw = set()
        deps = set()
        for r in reads:
            if r in self.last_w:
                raw.add(self.last_w[r])
        for w in writes:
            if w in self.last_w:
                deps.add(self.last_w[w])
            for rd in self.readers.get(w, ()):
                deps.add(rd)
        deps |= raw
        keep = set()
        for j in deps:
            oj = self.ops[j]
            if oj["dma"]:
                keep.add(j)
            elif oj["eng"] != eng:
                keep.add(j)
            elif (j in raw) and eng != "pe" and not dma:
                keep.add(j)
            elif dma:
                keep.add(j)
        op = dict(eng=eng, fn=fn, dma=dma, deps=keep, sig=dma, sem=None, val=None)
        if dma:
            h = self.dma_hist[eng]
            n = len(h)
            if n >= NDSEM:
                op["deps"].add(h[n - NDSEM])
            op["slot"] = n % NDSEM
            op["val"] = 16 * (n // NDSEM + 1)
            h.append(idx)
        for w in writes:
            self.last_w[w] = idx
            self.readers[w] = []
        for r in reads:
            self.readers.setdefault(r, []).append(idx)
        self.ops.append(op)
        return idx

    def emit(self, nc, block, sems, dsems):
        ops = self.ops
        for op in ops:
            for j in op["deps"]:
                ops[j]["sig"] = True
        cnt = {e: 0 for e in sems}
        for op in ops:
            if op["dma"]:
                op["sem"] = dsems[op["eng"]][op["slot"]]
            elif op["sig"]:
                cnt[op["eng"]] += 1
                op["sem"] = sems[op["eng"]]
                op["val"] = cnt[op["eng"]]

        def run(engname, eng):
            waited = {}
            for op in ops:
                if op["eng"] != engname:
                    continue
                need = {}
                for j in op["deps"]:
                    oj = ops[j]
                    key = id(oj["sem"])
                    if waited.get(key, 0) >= oj["val"]:
                        continue
                    if key not in need or need[key][1] < oj["val"]:
                        need[key] = (oj["sem"], oj["val"])
                for key, (sem, val) in need.items():
                    eng.wait_ge(sem, val)
                    waited[key] = val
                if op["fn"] is None:
                    continue
                ins = op["fn"](eng)
                if op["dma"]:
                    ins.then_inc(op["sem"], 16)
                elif op["sig"]:
                    ins.then_inc(op["sem"], 1)

        @block.tensor
        def _(e):
            run("pe", e)

        @block.scalar
        def _(e):
            run("act", e)

        @block.vector
        def _(e):
            run("dve", e)

        @block.gpsimd
        def _(e):
            run("pool", e)

        @block.sync
        def _(e):
            run("sp", e)


def build(S, L, lambda_inits):
    NT = S // 128
    NG = S // 512
    NB = S // 256
    assert S % 512 == 0 and NT <= 32
    nc = bass.Bass("TRN2", target_bir_lowering=False)

    def dram(name, shape, dt, kind):
        return nc.dram_tensor(name, list(shape), dt, kind=kind).ap()

    x_d = dram("x", [S, D], F32, "ExternalInput")
    w_in_d = dram("w_in_r", [L, 8, 128, NCH * 512], F32, "ExternalInput")
    w_out_d = dram("w_out_r", [L, 128, NCH * D], F32, "ExternalInput")
    ng_d = dram("norm_g", [L, D], F32, "ExternalInput")
    gcol_d = dram("gcol", [L, 128, 4], F32, "ExternalInput")
    sub_d = dram("subln", [L, 128], F32, "ExternalInput")
    lamv_d = dram("lamv", [L, 256], F32, "ExternalInput")
    ident_d = dram("c_ident", [128, 128], F32, "ExternalInput")
    cm_d = dram("c_cmask", [128, 128], F32, "ExternalInput")
    bones_d = dram("c_bones", [128, 128], F32, "ExternalInput")
    kaug_d = dram("c_kaug", [20, S], F32, "ExternalInput")
    qaug_d = dram("c_qaug", [12, 4, S], F32, "ExternalInput")
    gb_d = dram("c_gb", [1, NT * 16], F32, "ExternalInput")
    out_d = dram("out", [S, D], F32, "ExternalOutput")
    x1_d = dram("x1_scratch", [S, D], F32, "Internal")
    mix_d = dram("mix_scratch", [S, D], BF16, "Internal")

    from contextlib import ExitStack
    es = ExitStack()

    def sb(name, shape, dt):
        return es.enter_context(nc.sbuf_tensor(name, list(shape), dt))

    def pst(name, shape, dt):
        return es.enter_context(nc.psum_tensor(name, list(shape), dt))

    with es:
        hT = sb("hT", [128, NCH, S], BF16)
        xt = [sb(f"xt{i}", [128, D], F32) for i in range(3)]
        hb = [sb(f"hb{i}", [128, D], BF16) for i in range(2)]
        junk = sb("junk", [128, D], BF16)
        junk2 = sb("junk2", [128, D], BF16)
        gbc = sb("gbc", [128, D], F32)
        wbf = sb("wbf", [128, NCH, 512], BF16)
        wout = sb("wout", [128, NCH, D], BF16)
        QT = [sb(f"QT{i}", [128, S], BF16) for i in range(2)]
        KT = [sb(f"KT{i}", [128, S], BF16) for i in range(2)]
        Vt = sb("Vt", [128, NT, 130], BF16)
        Gt = sb("Gt", [128, NT, 128], BF16)
        gtmp = [sb(f"gtmp{i}", [128, 128], F32) for i in range(2)]
        sq = [sb(f"sq{i}", [128, 512], BF16) for i in range(2)]
        rs = [sb(f"rs{i}", [128, 512], F32) for i in range(2)]
        NPT = 4
        PT = [sb(f"PT{i}", [128, 512], BF16) for i in range(NPT)]
        gm = [sb(f"gm{i}", [128, NT, 16], F32) for i in range(2)]
        top8 = sb("top8", [128, NT, 8], F32)
        thr = sb("thr", [128, NT], F32)
        sel = sb("sel", [128, NT, 16], F32)
        selb = [sb(f"selb{i}", [128, NT, 16], BF16) for i in range(2)]
        km = [sb(f"km{i}", [64, 16], F32) for i in range(2)]
        kmb = [sb(f"kmb{i}", [64, 16], BF16) for i in range(2)]
        mo = [sb(f"mo{i}", [128, 4, 128], BF16) for i in range(2)]
        abuf = sb("abuf", [128, 4, 128], F32)
        dbuf = sb("dbuf", [128, 4, 128], F32)
        junkf = sb("junkf", [128, 128], F32)
        small4 = sb("small4", [128, 64], F32)
        small = sb("small", [128, 64], F32)
        mt = [sb(f"mt{i}", [128, D], BF16) for i in range(2)]
        mT = [sb(f"mT{i}", [128, NCH, 128], BF16) for i in range(2)]
        ident = sb("ident", [128, 128], BF16)
        cmask = sb("cmask", [128, 128], BF16)
        bones = sb("bones", [128, 128], BF16)
        gbt = sb("gbt", [128, NT * 16], F32)
        gcol = sb("gcol_s", [128, 4], F32)
        qcol = sb("qcol_s", [128, 4], F32)
        subbc = sb("subbc", [128, 128], F32)
        lamv = sb("lamv_s", [128, 256], F32)
        lamt = sb("lamt", [128, 8], F32)

        ps = [pst(f"ps{i}", [128, 512], F32) for i in range(7)]
        tp = pst("tp", [128, 1024], BF16)

        sems = {e: es.enter_context(nc.semaphore(f"sem_{e}")) for e in ["pe", "act", "dve", "pool", "sp"]}
        dsems = {q: [es.enter_context(nc.semaphore(f"dsem_{q}{i}")) for i in range(NDSEM)] for q in ["sp", "pool"]}
        block = es.enter_context(nc.Block())

        P = Prog()
        cnt = {"s": 0, "pt": 0, "small": 0, "pj": 0, "s4": 0, "tpb": 0}

        def psr(k):
            return ("ps", k)

        def scol():
            c = cnt["small"] % 64
            cnt["small"] += 1
            return c

        P.add("pool", lambda e: e.dma_start(out=ident[:], in_=ident_d[:, :]), writes=["ident"], dma=True)
        P.add("pool", lambda e: e.dma_start(out=cmask[:], in_=cm_d[:, :]), writes=["cmask"], dma=True)
        P.add("pool", lambda e: e.dma_start(out=bones[:], in_=bones_d[:, :]), writes=["bones"], dma=True)
        P.add("sp", lambda e: e.dma_start(out=gbt[:], in_=gb_d[:, :].to_broadcast([128, NT * 16])), writes=["gbt"], dma=True)
        for i in range(2):
            P.add("pool", lambda e, i=i: e.dma_start(out=KT[i][64:84, :], in_=kaug_d[:, :]),
                  writes=[("KTaug", i)], dma=True)
        for i in range(2):
            P.add("dve", lambda e, i=i: e.memset(kmb[i][:], 0.0), writes=[("kmb", i)])

        def layer(l):
            src = x_d if l == 0 else x1_d
            dst = out_d if l == L - 1 else x1_d
            srcn = "xd" if l == 0 else "x1d"
            dstn = "outd" if l == L - 1 else "x1d"
            li = lambda_inits[l]

            if l == 0:
                P.add("sp", lambda e: e.dma_start(out=gbc[:], in_=ng_d[l:l + 1, :].to_broadcast([128, D])),
                      writes=["gbc"], dma=True)
            P.add("sp", lambda e: e.dma_start(out=gcol[:], in_=gcol_d[l, :, :]), writes=["gcol"], dma=True)
            P.add("sp", lambda e: e.dma_start(out=subbc[:], in_=sub_d[l:l + 1, :].to_broadcast([128, 128])),
                  writes=["subbc"], dma=True)
            P.add("sp", lambda e: e.dma_start(out=lamv[:], in_=lamv_d[l:l + 1, :].to_broadcast([128, 256])),
                  writes=["lamv"], dma=True)
            P.add("pool", lambda e: e.dma_start(out=wout[:].rearrange("p c n -> p (c n)"), in_=w_out_d[l, :, :]),
                  writes=["wout"], dma=True)
            P.add("dve", lambda e: e.tensor_scalar(out=qcol[:], in0=gcol[:], scalar1=0.125, scalar2=None, op0=ALU.mult),
                  reads=["gcol"], writes=["qcol"])
            P.add("dve", lambda e: e.tensor_scalar(out=subbc[:], in0=subbc[:], scalar1=float(1.0 - li), scalar2=None,
                                                   op0=ALU.mult), reads=["subbc"], writes=["subbc"])
            P.add("dve", lambda e: e.tensor_tensor(out=lamv[:, 0:64], in0=lamv[:, 0:64], in1=lamv[:, 64:128], op=ALU.mult),
                  reads=["lamv"], writes=["lamv"])
            P.add("dve", lambda e: e.tensor_tensor(out=lamv[:, 128:192], in0=lamv[:, 128:192], in1=lamv[:, 192:256],
                                                   op=ALU.mult), reads=["lamv"], writes=["lamv"])
            P.add("dve", lambda e: e.tensor_reduce(out=lamt[:, 0:1], in_=lamv[:, 0:64], axis=AX.X, op=ALU.add),
                  reads=["lamv"], writes=["lamt"])
            P.add("dve", lambda e: e.tensor_reduce(out=lamt[:, 1:2], in_=lamv[:, 128:192], axis=AX.X, op=ALU.add),
                  reads=["lamv", "lamt"], writes=["lamt"])
            P.add("act", lambda e: e.activation(out=lamt[:, 2:4], in_=lamt[:, 0:2], func=AF.Exp),
                  reads=["lamt"], writes=["lamt"])
            P.add("dve", lambda e: e.scalar_tensor_tensor(out=lamt[:, 4:5], in0=lamt[:, 3:4], scalar=float(-li),
                                                          in1=lamt[:, 2:3], op0=ALU.add, op1=ALU.subtract),
                  reads=["lamt"], writes=["neglam"])

            cols0 = {}

            def p0_load(t):
                b3 = t % 3
                P.add("sp", lambda e: e.dma_start(out=xt[b3][:], in_=src[t * 128:(t + 1) * 128, :]),
                      reads=[(srcn, t)], writes=[("xt", b3)], dma=True)

            def p0_A(t):
                b3 = t % 3
                c0 = scol()
                cols0[t] = c0
                jk = junk if t % 2 == 0 else junk2
                P.add("act", lambda e: e.activation(out=jk[:], in_=xt[b3][:], func=AF.Square,
                                                    accum_out=small[:, c0:c0 + 1]),
                      reads=[("xt", b3)], writes=[("junk", t % 2), ("small", c0)])

            def p0_B(t):
                b3 = t % 3
                b = t % 2
                c0 = cols0[t]
                P.add("act", lambda e: e.activation(out=small[:, c0:c0 + 1], in_=small[:, c0:c0 + 1], func=AF.Ln,
                                                    scale=1.0 / D, bias=EPS),
                      reads=[("small", c0)], writes=[("small", c0)])
                P.add("act", lambda e: e.activation(out=small[:, c0:c0 + 1], in_=small[:, c0:c0 + 1], func=AF.Exp,
                                                    scale=-0.5),
                      reads=[("small", c0)], writes=[("small", c0)])
                P.add("dve", lambda e: e.scalar_tensor_tensor(out=hb[b][:], in0=xt[b3][:], scalar=small[:, c0:c0 + 1],
                                                              in1=gbc[:], op0=ALU.mult, op1=ALU.mult),
                      reads=[("xt", b3), ("small", c0), "gbc"], writes=[("hb", b)])

            def p0_B2(t):
                b = t % 2
                for c in range(NCH):
                    P.add("pe", lambda e, c=c: e.transpose(tp[:, c * 128:(c + 1) * 128], hb[b][:, c * 128:(c + 1) * 128],
                                                           ident[:]),
                          reads=[("hb", b), "ident"], writes=["tp"])

            def p0_C(t):
                P.add("dve", lambda e: e.tensor_copy(out=hT[:, :, t * 128:(t + 1) * 128],
                                                     in_=tp[:, :].rearrange("p (c n) -> p c n", n=128)),
                      writes=["tp", ("hT", t // 4)])

            if l == 0:
                p0_load(0)
                if NT > 1:
                    p0_load(1)
                p0_A(0)
                for t in range(NT):
                    if t + 2 < NT:
                        p0_load(t + 2)
                    if t + 1 < NT:
                        p0_A(t + 1)
                    p0_B(t)
                    if t >= 1:
                        p0_C(t - 1)
                    p0_B2(t)
                p0_C(NT - 1)

            for u in range(8):
                unit(l, u, li)

            def p2_load(t):
                b = t % 2
                b3 = t % 3
                P.add("sp", lambda e: e.dma_start(out=mt[b][:], in_=mix_d[t * 128:(t + 1) * 128, :]),
                      reads=[("mixd", t // 4)], writes=[("mt", b)], dma=True)
                P.add("sp", lambda e: e.dma_start(out=xt[b3][:], in_=src[t * 128:(t + 1) * 128, :]),
                      reads=[(srcn, t)], writes=[("xt", b3)], dma=True)

            fuse = l < L - 1
            if fuse:
                P.add("sp", lambda e: e.dma_start(out=gbc[:], in_=ng_d[l + 1:l + 2, :].to_broadcast([128, D])),
                      writes=["gbc"], dma=True)
            tpn = ps[5][:, :].bitcast(BF16)
            fcols = {}

            def f_act(t):
                b3 = t % 3
                c0 = scol()
                fcols[t] = c0
                jk = junk if t % 2 == 0 else junk2
                P.add("act", lambda e: e.activation(out=jk[:], in_=xt[b3][:], func=AF.Square,
                                                    accum_out=small[:, c0:c0 + 1]),
                      reads=[("xt", b3)], writes=[("junk", t % 2), ("small", c0)])
                P.add("act", lambda e: e.activation(out=small[:, c0:c0 + 1], in_=small[:, c0:c0 + 1], func=AF.Ln,
                                                    scale=1.0 / D, bias=EPS),
                      reads=[("small", c0)], writes=[("small", c0)])
                P.add("act", lambda e: e.activation(out=small[:, c0:c0 + 1], in_=small[:, c0:c0 + 1], func=AF.Exp,
                                                    scale=-0.5),
                      reads=[("small", c0)], writes=[("small", c0)])

            def f_stt(t):
                b3 = t % 3
                b = t % 2
                c0 = fcols[t]
                P.add("dve", lambda e: e.scalar_tensor_tensor(out=hb[b][:], in0=xt[b3][:], scalar=small[:, c0:c0 + 1],
                                                              in1=gbc[:], op0=ALU.mult, op1=ALU.mult),
                      reads=[("xt", b3), ("small", c0), "gbc"], writes=[("hb", b)])

            def f_tr(t):
                b = t % 2
                for c in range(NCH):
                    P.add("pe", lambda e, c=c: e.transpose(tpn[:, c * 128:(c + 1) * 128], hb[b][:, c * 128:(c + 1) * 128],
                                                           ident[:]),
                          reads=[("hb", b), "ident"], writes=[psr(5)])
                P.add("dve", lambda e: e.tensor_copy(out=hT[:, :, t * 128:(t + 1) * 128],
                                                     in_=tpn.rearrange("p (c n) -> p c n", n=128)),
                      writes=[psr(5), ("hT", t // 4)])

            p2_load(0)
            for t in range(NT):
                b = t % 2
                b3 = t % 3
                if t + 1 < NT:
                    p2_load(t + 1)
                for c in range(NCH):
                    P.add("pe", lambda e, b=b, c=c: e.transpose(tp[:, c * 128:(c + 1) * 128], mt[b][:, c * 128:(c + 1) * 128],
                                                                ident[:]),
                          reads=[("mt", b), "ident"], writes=["tp"])
                P.add("act", lambda e, b=b: e.copy(out=mT[b][:, :, :], in_=tp[:, :].rearrange("p (c n) -> p c n", n=128)),
                      writes=["tp", ("mT", b)])
                if fuse and t >= 1:
                    f_act(t - 1)
                    f_stt(t - 1)
                banks = [6, 0] if t % 2 == 0 else [1, 2]
                for half in range(2):
                    bank = banks[half]
                    for c in range(NCH):
                        P.add("pe", lambda e, b=b, c=c, half=half, bank=bank: e.matmul(
                            ps[bank][:, :], lhsT=mT[b][:, c, :], rhs=wout[:, c, half * 512:(half + 1) * 512],
                            start=(c == 0), stop=(c == NCH - 1)),
                            reads=[("mT", b), "wout"], writes=[psr(bank)])
                if fuse and t >= 1:
                    f_tr(t - 1)
                for half in range(2):
                    bank = banks[half]
                    P.add("dve", lambda e, b3=b3, half=half, bank=bank: e.tensor_tensor(
                        out=xt[b3][:, half * 512:(half + 1) * 512], in0=xt[b3][:, half * 512:(half + 1) * 512],
                        in1=ps[bank][:, :], op=ALU.add),
                        reads=[("xt", b3)], writes=[psr(bank), ("xt", b3)])
                P.add("pool", lambda e, t=t, b3=b3: e.dma_start(out=dst[t * 128:(t + 1) * 128, :], in_=xt[b3][:]),
                      reads=[("xt", b3)], writes=[(dstn, t)], dma=True)
            if fuse:
                f_act(NT - 1)
                f_stt(NT - 1)
                f_tr(NT - 1)

        def unit(l, u, li):
            moba = u < 4
            dvw = 65 if moba else 129
            heads = [2 * u, 2 * u + 1] if moba else [8 + (u - 4), 8 + (u - 4)]
            P.add("pool", lambda e: e.dma_start(out=wbf[:].rearrange("p c n -> p (c n)"), in_=w_in_d[l, u, :, :]),
                  writes=["wbf"], dma=True)
            for i in range(2):
                P.add("pool", lambda e, i=i: e.dma_start(out=QT[i][80:84, :], in_=qaug_d[heads[i], :, :]),
                      writes=[("QTaug", i)], dma=True)
            if moba:
                P.add("dve", lambda e: e.memset(Vt[:, :, 64:65], 1.0), writes=["Vt"])
                P.add("dve", lambda e: e.memset(Vt[:, :, 129:130], 1.0), writes=["Vt"])
            else:
                P.add("dve", lambda e: e.memset(Vt[:, :, 128:129], 1.0), writes=["Vt"])
                if u == 4:
                    for i in range(2):
                        P.add("dve", lambda e, i=i: e.memset(QT[i][64:80, :], 0.0), writes=[("QTsel", i)])

            groups = [(which, g) for which in range(2) for g in range(NG)]
            PJB = [6, 1, 2, 3]
            SSB = [0, 4]

            def emit_proj(n):
                which, g = groups[n]
                pj = PJB[n % 4]
                for c in range(NCH):
                    P.add("pe", lambda e, c=c: e.matmul(
                        ps[pj][:, :], lhsT=wbf[:, c, which * 128:(which + 1) * 128],
                        rhs=hT[:, c, g * 512:(g + 1) * 512], start=(c == 0), stop=(c == NCH - 1)),
                        reads=["wbf", ("hT", g)], writes=[psr(pj)])

            def emit_chainA(n):
                pj = PJB[n % 4]
                b = n % 2
                ssb = SSB[n % 2]
                P.add("act", lambda e: e.activation(out=sq[b][:], in_=ps[pj][:, :], func=AF.Square),
                      writes=[psr(pj), ("sq", b)])
                P.add("pe", lambda e: e.matmul(ps[ssb][:, :], lhsT=bones[:], rhs=sq[b][:], start=True, stop=True),
                      reads=["bones", ("sq", b)], writes=[psr(ssb)])

            def emit_chainB(n):
                which, g = groups[n]
                pj = PJB[n % 4]
                b = n % 2
                ssb = SSB[n % 2]
                T = QT if which == 0 else KT
                tname = "QT" if which == 0 else "KT"
                colt = qcol if which == 0 else gcol
                cidx = (0 if moba else 2) + which
                P.add("act", lambda e: e.activation(out=rs[b][:], in_=ps[ssb][:, :], func=AF.Ln, scale=1.0 / 64, bias=EPS),
                      writes=[psr(ssb), ("rs", b)])
                P.add("act", lambda e: e.activation(out=rs[b][:], in_=rs[b][:], func=AF.Exp, scale=-0.5),
                      reads=[("rs", b)], writes=[("rs", b)])
                for i in range(2):
                    if moba and which == 1:
                        for hh in range(2):
                            P.add("dve", lambda e, i=i, hh=hh: e.scalar_tensor_tensor(
                                out=T[i][0:64, g * 512 + hh * 256:g * 512 + (hh + 1) * 256],
                                in0=ps[pj][i * 64:(i + 1) * 64, hh * 256:(hh + 1) * 256],
                                scalar=colt[i * 64:(i + 1) * 64, cidx:cidx + 1],
                                in1=rs[b][i * 64:(i + 1) * 64, hh * 256:(hh + 1) * 256],
                                op0=ALU.mult, op1=ALU.mult, accum_out=km[i][:, 2 * g + hh:2 * g + hh + 1]),
                                reads=[("rs", b), "qcol", "gcol"], writes=[psr(pj), (tname, i, g), ("km", i)])
                    else:
                        P.add("dve", lambda e, i=i: e.scalar_tensor_tensor(
                            out=T[i][0:64, g * 512:(g + 1) * 512], in0=ps[pj][i * 64:(i + 1) * 64, :],
                            scalar=colt[i * 64:(i + 1) * 64, cidx:cidx + 1], in1=rs[b][i * 64:(i + 1) * 64, :],
                            op0=ALU.mult, op1=ALU.mult),
                            reads=[("rs", b), "qcol", "gcol"], writes=[psr(pj), (tname, i, g)])

            NGR = len(groups)
            emit_proj(0)
            if NGR > 1:
                emit_proj(1)
            emit_chainA(0)
            for n in range(NGR):
                if n + 2 < NGR:
                    emit_proj(n + 2)
                if n + 1 < NGR:
                    emit_chainA(n + 1)
                emit_chainB(n)

            side = []

            def sel_stage1(i):
                P.add("dve", lambda e: e.tensor_scalar(out=kmb[i][:, 0:NB], in0=km[i][:, 0:NB], scalar1=1.0 / 256,
                                                       scalar2=None, op0=ALU.mult),
                      reads=[("km", i)], writes=[("kmb", i)])

            def sel_gate(i):
                gbank = 6 if i == 0 else 0
                for t in range(NT):
                    P.add("pe", lambda e, t=t: e.matmul(ps[gbank][:, t * 16:(t + 1) * 16],
                                                        lhsT=QT[i][0:64, t * 128:(t + 1) * 128], rhs=kmb[i][:, :],
                                                        start=True, stop=True),
                          reads=[("QT", i, t // 4), ("kmb", i)], writes=[psr(gbank)])
                P.add("dve", lambda e: e.tensor_tensor(out=gm[i][:].rearrange("p t n -> p (t n)"),
                                                       in0=ps[gbank][:, 0:NT * 16], in1=gbt[:], op=ALU.add),
                      reads=["gbt"], writes=[psr(gbank), ("gm", i)])

            def sel_chain(i):
                th = []
                for t in range(NT):
                    th.append(lambda t=t: P.add("dve", lambda e: e.max(out=top8[:, t, :], in_=gm[i][:, t, :]),
                                                reads=[("gm", i)], writes=["top8"]))
                th.append(lambda: P.add("dve", lambda e: e.tensor_scalar(out=thr[:], in0=top8[:, :, 3], scalar1=-1e29,
                                                                         scalar2=None, op0=ALU.max),
                                        reads=["top8"], writes=["thr"]))
                th.append(lambda: P.add("dve", lambda e: e.tensor_tensor(
                    out=sel[:], in0=gm[i][:], in1=thr[:].unsqueeze(2).to_broadcast([128, NT, 16]), op=ALU.is_ge),
                    reads=[("gm", i), "thr"], writes=["sel"]))
                th.append(lambda: P.add("dve", lambda e: e.tensor_scalar(out=selb[i][:], in0=sel[:], scalar1=-NEGM,
                                                                         scalar2=NEGM, op0=ALU.mult, op1=ALU.add),
                                        reads=["sel"], writes=[("selb", i)]))
                return th

            def sel_final_thunks(i):
                th = []
                for k, t0 in enumerate(range(0, NT, 8)):
                    def f(k=k, t0=t0):
                        w = cnt["tpb"] % 3
                        cnt["tpb"] += 1
                        if w == 0:
                            buf = tp[0:16, :]
                            res = "tp"
                        else:
                            buf = ps[3 + w][0:16, :].bitcast(BF16)
                            res = psr(3 + w)
                        for t in range(t0, t0 + 8):
                            P.add("pe", lambda e, t=t: e.transpose(buf[:, (t - t0) * 128:(t - t0 + 1) * 128],
                                                                   selb[i][:, t, :], ident[:]),
                                  reads=[("selb", i), "ident"], writes=[res])
                        if k % 2 == 0:
                            P.add("act", lambda e: e.copy(out=QT[i][64:80, t0 * 128:(t0 + 8) * 128], in_=buf),
                                  writes=[res, ("QTsel", i)])
                        else:
                            P.add("dve", lambda e: e.tensor_copy(out=QT[i][64:80, t0 * 128:(t0 + 8) * 128], in_=buf),
                                  writes=[res, ("QTsel", i)])
                    th.append(f)
                return th

            if moba:
                for i in range(2):
                    sel_stage1(i)

            for t in range(NT):
                bank = [2, 3, 1][t % 3]
                b = t % 2
                if moba and t == min(4, NT - 1):
                    for i in range(2):
                        sel_gate(i)
                        side.extend(sel_chain(i))
                        side.extend(sel_final_thunks(i))
                for c in range(NCH):
                    P.add("pe", lambda e, c=c, t=t, bank=bank: e.matmul(
                        ps[bank][:, 0:256], lhsT=hT[:, c, t * 128:(t + 1) * 128], rhs=wbf[:, c, 256:512],
                        start=(c == 0), stop=(c == NCH - 1)),
                        reads=["wbf", ("hT", t // 4)], writes=[psr(bank)])
                if moba:
                    P.add("dve", lambda e, t=t, bank=bank: e.tensor_copy(
                        out=Vt[:, t, :].rearrange("p (i c) -> p i c", c=65)[:, :, 0:64],
                        in_=ps[bank][:, 0:128].rearrange("p (i c) -> p i c", c=64)),
                        writes=[psr(bank), "Vt"])
                    P.add("act", lambda e, t=t, bank=bank: e.activation(out=Gt[:, t, :], in_=ps[bank][:, 128:256], func=AF.Silu),
                          writes=[psr(bank), "Gt"])
                else:
                    P.add("dve", lambda e, t=t, bank=bank: e.tensor_copy(out=Vt[:, t, 0:128], in_=ps[bank][:, 0:128]),
                          writes=[psr(bank), "Vt"])
                    P.add("act", lambda e, b=b, bank=bank: e.activation(out=gtmp[b][:], in_=ps[bank][:, 128:256], func=AF.Silu),
                          writes=[psr(bank), ("gtmp", b)])
                    P.add("dve", lambda e, t=t, b=b: e.tensor_tensor(out=Gt[:, t, :], in0=gtmp[b][:], in1=subbc[:], op=ALU.mult),
                          reads=[("gtmp", b), "subbc"], writes=["Gt"])
                for _ in range(4):
                    if side:
                        side.pop(0)()
            while side:
                side.pop(0)()

            slopes_all = MOBA_SLOPES + DIFF_SLOPES
            iters = []
            for g in range(NG):
                for i in range(2):
                    sl = slopes_all[heads[i]]
                    glist = []
                    for kt in range(0, 4 * (g + 1)):
                        j = kt - 4 * g
                        s0 = max(0, j)
                        s1 = s0
                        for sq_ in range(s0, 4):
                            tq = 4 * g + sq_
                            dmin = 0 if tq == kt else (tq * 128 - (kt * 128 + 127))
                            if sl * dmin <= ALIBI_FAR:
                                s1 = sq_ + 1
                        if s1 > s0:
                            glist.append(dict(g=g, i=i, kt=kt, s0=s0, s1=s1))
                    started = set()
                    for it in glist:
                        it["startbank"] = {}
                        for sq_ in range(it["s0"], it["s1"]):
                            bk = sq_ // 2
                            if bk not in started:
                                started.add(bk)
                                it["startbank"][sq_] = True
                    glist[-1]["last"] = True
                    assert glist[-1]["kt"] == 4 * g + 3 and started == {0, 1}
                    iters.extend(glist)
            SBAP = [ps[0][:, :], ps[1][:, :], ps[6][:, :], tp[:, :].bitcast(F32)]
            SBRES = [psr(0), psr(1), psr(6), "tp"]
            PIPE = 3

            def emit_S(n):
                it = iters[n]
                g, i, kt, s0, s1 = it["g"], it["i"], it["kt"], it["s0"], it["s1"]
                j = kt - 4 * g
                sbk = n % 4
                P.add("pe", lambda e: e.matmul(
                    SBAP[sbk][:, s0 * 128:s1 * 128], lhsT=KT[i][0:KR, kt * 128:(kt + 1) * 128],
                    rhs=QT[i][0:KR, g * 512 + s0 * 128:g * 512 + s1 * 128], start=True, stop=(j < 0)),
                    reads=[("KT", i, kt // 4), ("KTaug", i), ("QT", i, g), ("QTaug", i), ("QTsel", i)],
                    writes=[SBRES[sbk]])
                if j >= 0:
                    P.add("pe", lambda e: e.matmul(SBAP[sbk][:, j * 128:(j + 1) * 128], lhsT=ident[:],
                                                   rhs=cmask[:], start=False, stop=True),
                          reads=["ident", "cmask"], writes=[SBRES[sbk]])

            def emit_rest(n):
                it = iters[n]
                g, i, kt, s0, s1 = it["g"], it["i"], it["kt"], it["s0"], it["s1"]
                sbk = n % 4
                slot = n % NPT
                oset = (g * 2 + i) % 2
                ob = [2 + 2 * oset, 3 + 2 * oset]
                P.add("act", lambda e: e.activation(
                    out=PT[slot][:, s0 * 128:s1 * 128], in_=SBAP[sbk][:, s0 * 128:s1 * 128], func=AF.Exp),
                    writes=[SBRES[sbk], ("PT", slot)])
                for s in range(s0, s1):
                    bank = ob[s // 2]
                    col = (s % 2) * dvw
                    vlo = i * 65 if moba else 0
                    P.add("pe", lambda e, s=s, bank=bank, col=col, vlo=vlo: e.matmul(
                        ps[bank][:, col:col + dvw], lhsT=PT[slot][:, s * 128:(s + 1) * 128],
                        rhs=Vt[:, kt, vlo:vlo + dvw], start=bool(it["startbank"].get(s, False)), stop=(kt == 4 * g + s),
                        skip_group_check=True),
                        reads=[("PT", slot), "Vt"], writes=[psr(bank)])
                if it.get("last"):
                    pend.append((n + EPI_DELAY, (u, g, i, ob, dvw, moba)))

            NI = len(iters)
            pend = []
            EPI_DELAY = 2
            for n in range(min(PIPE, NI)):
                emit_S(n)
            for n in range(NI):
                if n + PIPE < NI:
                    emit_S(n + PIPE)
                emit_rest(n)
                while pend and pend[0][0] <= n:
                    epilogue(*pend.pop(0)[1])
            while pend:
                epilogue(*pend.pop(0)[1])

        def epilogue(u, g, i, ob, dvw, moba):
            mb = g % 2
            if (not moba) and i == 1:
                k4 = cnt["s4"] % 16
                cnt["s4"] += 1
                c4 = 4 * k4
            for s in range(4):
                t = 4 * g + s
                bank = ob[s // 2]
                col = (s % 2) * dvw
                c0 = scol()
                if moba:
                    P.add("dve", lambda e, bank=bank, col=col, c0=c0: e.reciprocal(out=small[:, c0:c0 + 1],
                                                                                   in_=ps[bank][:, col + 64:col + 65]),
                          writes=[psr(bank), ("small", c0)])
                    P.add("dve", lambda e, bank=bank, col=col, c0=c0, s=s, t=t, mb=mb, i=i: e.scalar_tensor_tensor(
                        out=mo[mb][:, s, i * 64:(i + 1) * 64], in0=ps[bank][:, col:col + 64], scalar=small[:, c0:c0 + 1],
                        in1=Gt[:, t, i * 64:(i + 1) * 64], op0=ALU.mult, op1=ALU.mult),
                        reads=[("small", c0), "Gt"], writes=[psr(bank), ("mo", mb)])
                elif i == 0:
                    P.add("dve", lambda e, bank=bank, col=col, c0=c0: e.reciprocal(out=small[:, c0:c0 + 1],
                                                                                   in_=ps[bank][:, col + 128:col + 129]),
                          writes=[psr(bank), ("small", c0)])
                    P.add("dve", lambda e, bank=bank, col=col, c0=c0, s=s: e.tensor_scalar(
                        out=abuf[:, s, :], in0=ps[bank][:, col:col + 128], scalar1=small[:, c0:c0 + 1], scalar2=None,
                        op0=ALU.mult),
                        reads=[("small", c0)], writes=[psr(bank), ("abuf", s)])
                else:
                    P.add("dve", lambda e, bank=bank, col=col, c0=c0: e.reciprocal(out=small[:, c0:c0 + 1],
                                                                                   in_=ps[bank][:, col + 128:col + 129]),
                          writes=[psr(bank), ("small", c0)])
                    P.add("dve", lambda e, c0=c0: e.tensor_tensor(out=small[:, c0:c0 + 1], in0=small[:, c0:c0 + 1],
                                                                 in1=lamt[:, 4:5], op=ALU.mult),
                          reads=[("small", c0), "neglam"], writes=[("small", c0)])
                    P.add("dve", lambda e, bank=bank, col=col, c0=c0, s=s: e.scalar_tensor_tensor(
                        out=dbuf[:, s, :], in0=ps[bank][:, col:col + 128], scalar=small[:, c0:c0 + 1], in1=abuf[:, s, :],
                        op0=ALU.mult, op1=ALU.add),
                        reads=[("small", c0), ("abuf", s)], writes=[psr(bank), ("dbuf", s)])
                    P.add("dve", lambda e, s=s, c4=c4: e.scalar_tensor_tensor(
                        out=junkf[:], in0=dbuf[:, s, :], scalar=1.0, in1=dbuf[:, s, :], op0=ALU.mult, op1=ALU.mult,
                        accum_out=small4[:, c4 + s:c4 + s + 1]),
                        reads=[("dbuf", s)], writes=["junkf", ("small4", c4)])
            if (not moba) and i == 1:
                P.add("act", lambda e, c4=c4: e.activation(out=small4[:, c4:c4 + 4], in_=small4[:, c4:c4 + 4], func=AF.Ln,
                                                          scale=1.0 / 128, bias=EPS),
                      reads=[("small4", c4)], writes=[("small4", c4)])
                P.add("act", lambda e, c4=c4: e.activation(out=small4[:, c4:c4 + 4], in_=small4[:, c4:c4 + 4], func=AF.Exp,
                                                          scale=-0.5),
                      reads=[("small4", c4)], writes=[("small4", c4)])
                for s in range(4):
                    t = 4 * g + s
                    P.add("dve", lambda e, s=s, t=t, mb=mb, c4=c4: e.scalar_tensor_tensor(
                        out=mo[mb][:, s, :], in0=dbuf[:, s, :], scalar=small4[:, c4 + s:c4 + s + 1], in1=Gt[:, t, :],
                        op0=ALU.mult, op1=ALU.mult),
                        reads=[("dbuf", s), ("small4", c4), "Gt"], writes=[("mo", mb)])
            if i == 1:
                P.add("sp", lambda e, g=g, mb=mb, u=u: e.dma_start(
                    out=mix_d[g * 512:(g + 1) * 512, u * 128:(u + 1) * 128].rearrange("(s p) c -> p s c", p=128),
                    in_=mo[mb][:, :, :]),
                    reads=[("mo", mb)], writes=[("mixd", g)], dma=True)

        for l in range(L):
            layer(l)
        P.add("sp", None, reads=[("outd", t) for t in range(NT)])
        print('sbuf bytes remaining', nc.sbuf_bytes_remaining)
        P.emit(nc, block, sems, dsems)
    return nc


def _host_layout(S, inputs):
    w_in = np.asarray(inputs["w_in"], dtype=np.float32)
    w_out = np.asarray(inputs["w_out"], dtype=np.float32)
    L = w_in.shape[0]
    NT = S // 128
    w_in_r = np.empty((L, 8, 128, NCH, 512), np.float32)
    for l in range(L):
        wl = w_in[l].reshape(NCH, 128, 4096)
        for u in range(8):
            base = 0 if u < 4 else 2048
            idx = u % 4
            for k in range(4):
                c0 = base + k * 512 + idx * 128
                w_in_r[l, u, :, :, k * 128:(k + 1) * 128] = wl[:, :, c0:c0 + 128].transpose(1, 0, 2)
    w_in_r = np.ascontiguousarray(w_in_r.reshape(L, 8, 128, NCH * 512))
    w_out_r = np.ascontiguousarray(w_out.reshape(L, NCH, 128, D).transpose(0, 2, 1, 3).reshape(L, 128, NCH * D))
    gcol = np.stack([np.stack([np.tile(np.asarray(inputs[k], np.float32)[l], 2) for k in
                               ("moba_q_norm", "moba_k_norm", "diff_q_norm", "diff_k_norm")], axis=1)
                     for l in range(L)], axis=0).astype(np.float32)
    lamv = np.concatenate([np.asarray(inputs[k], np.float32) for k in
                           ("lambda_q1", "lambda_k1", "lambda_q2", "lambda_k2")], axis=1)
    pos = np.arange(S)
    ident = np.eye(128, dtype=np.float32)
    kk = np.arange(128)[:, None]
    qq = np.arange(128)[None, :]
    cmask = np.where(kk <= qq, 0.0, NEGM).astype(np.float32)
    bones = np.kron(np.eye(2, dtype=np.float32), np.ones((64, 64), np.float32))
    kaug = np.zeros((20, S), np.float32)
    kaug[0:16] = (pos[None, :] // 256 == np.arange(16)[:, None]).astype(np.float32)
    kaug[16] = pos // 128
    kaug[17] = pos % 128
    kaug[18] = 1.0
    kaug[19] = 1.0
    qaug = np.zeros((12, 4, S), np.float32)
    for h, sl in enumerate(MOBA_SLOPES + DIFF_SLOPES):
        qaug[h, 0] = sl * 128.0
        qaug[h, 1] = sl
        qaug[h, 2] = -sl * 128.0 * (pos // 128)
        qaug[h, 3] = -sl * (pos % 128)
    gb = np.zeros((NT, 16), np.float32)
    for t in range(NT):
        own = t // 2
        gb[t, own] = 1e30
        gb[t, own + 1:] = -1e30
    common = {
        "w_in_r": w_in_r, "w_out_r": w_out_r,
        "norm_g": np.ascontiguousarray(np.asarray(inputs["norm_g"], np.float32)),
        "gcol": np.ascontiguousarray(gcol),
        "subln": np.ascontiguousarray(np.asarray(inputs["diff_subln"], np.float32)),
        "lamv": np.ascontiguousarray(lamv),
        "c_ident": ident, "c_cmask": cmask, "c_bones": bones, "c_kaug": kaug, "c_qaug": qaug,
        "c_gb": gb.reshape(1, NT * 16),
    }
    return L, common


def kernel(x, norm_g, w_in, moba_q_norm, moba_k_norm, diff_q_norm, diff_k_norm,
           lambda_q1, lambda_k1, lambda_q2, lambda_k2, diff_subln, w_out):
    inputs = dict(x=x, norm_g=norm_g, w_in=w_in, moba_q_norm=moba_q_norm, moba_k_norm=moba_k_norm,
                  diff_q_norm=diff_q_norm, diff_k_norm=diff_k_norm, lambda_q1=lambda_q1, lambda_k1=lambda_k1,
                  lambda_q2=lambda_q2, lambda_k2=lambda_k2, diff_subln=diff_subln, w_out=w_out)
    x = np.asarray(x, dtype=np.float32)
    B, S, _ = x.shape
    L, common = _host_layout(S, inputs)
    lambda_inits = [0.8 - 0.6 * math.exp(-0.3 * l) for l in range(L)]
    nc = build(S, L, lambda_inits)
    in_maps = [dict(common, x=np.ascontiguousarray(x[b])) for b in range(B)]
    res = run_bass_kernel_spmd(nc, in_maps, core_ids=list(range(B)))
    return np.stack([np.asarray(r["out"], dtype=np.float32) for r in res.results], axis=0)
```

```python
import math
import numpy as np
import concourse.bass as bass
import concourse.mybir as mybir
from concourse.bass_utils import run_bass_kernel_spmd

F32 = mybir.dt.float32
BF16 = mybir.dt.bfloat16
AF = mybir.ActivationFunctionType
ALU = mybir.AluOpType
AX = mybir.AxisListType

D = 1024
NCH = 8
EPS = 1e-6
NEGM = -30000.0
KR = 84
MOBA_SLOPES = [2.0 ** (-8.0 * i / 8) for i in range(1, 9)]
DIFF_SLOPES = [2.0 ** (-8.0 * i / 4) for i in range(1, 5)]
NDSEM = 8
ALIBI_FAR = 45.0


class Prog:
    def __init__(self):
        self.ops = []
        self.last_w = {}
        self.readers = {}
        self.dma_hist = {"sp": [], "pool": []}

    def add(self, eng, fn, reads=(), writes=(), dma=False):
        idx = len(self.ops)
        raw = set()
        deps = set()
        for r in reads:
            if r in self.last_w:
                raw.add(self.last_w[r])
        for w in writes:
            if w in self.last_w:
                deps.add(self.last_w[w])
            for rd in self.readers.get(w, ()):
                deps.add(rd)
        deps |= raw
        keep = set()
        for j in deps:
            oj = self.ops[j]
            if oj["dma"]:
                keep.add(j)
            elif oj["eng"] != eng:
                keep.add(j)
            elif (j in raw) and eng != "pe" and not dma:
                keep.add(j)
            elif dma:
                keep.add(j)
        op = dict(eng=eng, fn=fn, dma=dma, deps=keep, sig=dma, sem=None, val=None)
        if dma:
            h = self.dma_hist[eng]
            n = len(h)
            if n >= NDSEM:
                op["deps"].add(h[n - NDSEM])
            op["slot"] = n % NDSEM
            op["val"] = 16 * (n // NDSEM + 1)
            h.append(idx)
        for w in writes:
            self.last_w[w] = idx
            self.readers[w] = []
        for r in reads:
            self.readers.setdefault(r, []).append(idx)
        self.ops.append(op)
        return idx

    def emit(self, nc, block, sems, dsems):
        ops = self.ops
        for op in ops:
            for j in op["deps"]:
                ops[j]["sig"] = True
        cnt = {e: 0 for e in sems}
        for op in ops:
            if op["dma"]:
                op["sem"] = dsems[op["eng"]][op["slot"]]
            elif op["sig"]:
                cnt[op["eng"]] += 1
                op["sem"] = sems[op["eng"]]
                op["val"] = cnt[op["eng"]]

        def run(engname, eng):
            waited = {}
            for op in ops:
                if op["eng"] != engname:
                    continue
                need = {}
                for j in op["deps"]:
                    oj = ops[j]
                    key = id(oj["sem"])
                    if waited.get(key, 0) >= oj["val"]:
                        continue
                    if key not in need or need[key][1] < oj["val"]:
                        need[key] = (oj["sem"], oj["val"])
                for key, (sem, val) in need.items():
                    eng.wait_ge(sem, val)
                    waited[key] = val
                if op["fn"] is None:
                    continue
                ins = op["fn"](eng)
                if op["dma"]:
                    ins.then_inc(op["sem"], 16)
                elif op["sig"]:
                    ins.then_inc(op["sem"], 1)

        @block.tensor
        def _(e):
            run("pe", e)

        @block.scalar
        def _(e):
            run("act", e)

        @block.vector
        def _(e):
            run("dve", e)

        @block.gpsimd
        def _(e):
            run("pool", e)

        @block.sync
        def _(e):
            run("sp", e)


def build(S, L, lambda_inits):
    NT = S // 128
    NG = S // 512
    NB = S // 256
    assert S % 512 == 0 and NT <= 32
    nc = bass.Bass("TRN2", target_bir_lowering=False)

    def dram(name, shape, dt, kind):
        return nc.dram_tensor(name, list(shape), dt, kind=kind).ap()

    x_d = dram("x", [S, D], F32, "ExternalInput")
    w_in_d = dram("w_in_r", [L, 8, 128, NCH * 512], F32, "ExternalInput")
    w_out_d = dram("w_out_r", [L, 128, NCH * D], F32, "ExternalInput")
    ng_d = dram("norm_g", [L, D], F32, "ExternalInput")
    gcol_d = dram("gcol", [L, 128, 4], F32, "ExternalInput")
    sub_d = dram("subln", [L, 128], F32, "ExternalInput")
    lamv_d = dram("lamv", [L, 256], F32, "ExternalInput")
    ident_d = dram("c_ident", [128, 128], F32, "ExternalInput")
    cm_d = dram("c_cmask", [128, 128], F32, "ExternalInput")
    bones_d = dram("c_bones", [128, 128], F32, "ExternalInput")
    kaug_d = dram("c_kaug", [20, S], F32, "ExternalInput")
    qaug_d = dram("c_qaug", [12, 4, S], F32, "ExternalInput")
    gb_d = dram("c_gb", [1, NT * 16], F32, "ExternalInput")
    out_d = dram("out", [S, D], F32, "ExternalOutput")
    x1_d = dram("x1_scratch", [S, D], F32, "Internal")
    mix_d = dram("mix_scratch", [S, D], BF16, "Internal")

    from contextlib import ExitStack
    es = ExitStack()

    def sb(name, shape, dt):
        return es.enter_context(nc.sbuf_tensor(name, list(shape), dt))

    def pst(name, shape, dt):
        return es.enter_context(nc.psum_tensor(name, list(shape), dt))

    with es:
        hT = sb("hT", [128, NCH, S], BF16)
        xt = [sb(f"xt{i}", [128, D], F32) for i in range(3)]
        hb = [sb(f"hb{i}", [128, D], BF16) for i in range(2)]
        junk = sb("junk", [128, D], BF16)
        junk2 = sb("junk2", [128, D], BF16)
        gbc = sb("gbc", [128, D], F32)
        wbf = sb("wbf", [128, NCH, 512], BF16)
        wout = sb("wout", [128, NCH, D], BF16)
        QT = [sb(f"QT{i}", [128, S], BF16) for i in range(2)]
        KT = [sb(f"KT{i}", [128, S], BF16) for i in range(2)]
        Vt = sb("Vt", [128, NT, 130], BF16)
        Gt = sb("Gt", [128, NT, 128], BF16)
        gtmp = [sb(f"gtmp{i}", [128, 128], F32) for i in range(2)]
        sq = [sb(f"sq{i}", [128, 512], BF16) for i in range(2)]
        rs = [sb(f"rs{i}", [128, 512], F32) for i in range(2)]
        NPT = 6
        PT = [sb(f"PT{i}", [128, 512], BF16) for i in range(NPT)]
        gm = [sb(f"gm{i}", [128, NT, 16], F32) for i in range(2)]
        top8 = sb("top8", [128, NT, 8], F32)
        thr = sb("thr", [128, NT], F32)
        sel = sb("sel", [128, NT, 16], F32)
        selb = [sb(f"selb{i}", [128, NT, 16], BF16) for i in range(2)]
        km = [sb(f"km{i}", [64, 16], F32) for i in range(2)]
        kmb = [sb(f"kmb{i}", [64, 16], BF16) for i in range(2)]
        mo = [sb(f"mo{i}", [128, 4, 128], BF16) for i in range(2)]
        abuf = sb("abuf", [128, 4, 128], F32)
        dbuf = sb("dbuf", [128, 4, 128], F32)
        junkf = sb("junkf", [128, 128], F32)
        small4 = sb("small4", [128, 64], F32)
        small = sb("small", [128, 64], F32)
        mt = [sb(f"mt{i}", [128, D], BF16) for i in range(2)]
        mT = [sb(f"mT{i}", [128, NCH, 128], BF16) for i in range(2)]
        ident = sb("ident", [128, 128], BF16)
        cmask = sb("cmask", [128, 128], BF16)
        bones = sb("bones", [128, 128], BF16)
        gbt = sb("gbt", [128, NT * 16], F32)
        gcol = sb("gcol_s", [128, 4], F32)
        qcol = sb("qcol_s", [128, 4], F32)
        subbc = sb("subbc", [128, 128], F32)
        lamv = sb("lamv_s", [128, 256], F32)
        lamt = sb("lamt", [128, 8], F32)

        ps = [pst(f"ps{i}", [128, 512], F32) for i in range(7)]
        tp = pst("tp", [128, 1024], BF16)

        sems = {e: es.enter_context(nc.semaphore(f"sem_{e}")) for e in ["pe", "act", "dve", "pool", "sp"]}
        dsems = {q: [es.enter_context(nc.semaphore(f"dsem_{q}{i}")) for i in range(NDSEM)] for q in ["sp", "pool"]}
        block = es.enter_context(nc.Block())

        P = Prog()
        cnt = {"s": 0, "pt": 0, "small": 0, "pj": 0, "s4": 0, "tpb": 0}

        def psr(k):
            return ("ps", k)

        def scol():
            c = cnt["small"] % 64
            cnt["small"] += 1
            return c

        P.add("pool", lambda e: e.dma_start(out=ident[:], in_=ident_d[:, :]), writes=["ident"], dma=True)
        P.add("pool", lambda e: e.dma_start(out=cmask[:], in_=cm_d[:, :]), writes=["cmask"], dma=True)
        P.add("pool", lambda e: e.dma_start(out=bones[:], in_=bones_d[:, :]), writes=["bones"], dma=True)
        P.add("sp", lambda e: e.dma_start(out=gbt[:], in_=gb_d[:, :].to_broadcast([128, NT * 16])), writes=["gbt"], dma=True)
        for i in range(2):
            P.add("pool", lambda e, i=i: e.dma_start(out=KT[i][64:84, :], in_=kaug_d[:, :]),
                  writes=[("KTaug", i)], dma=True)
        for i in range(2):
            P.add("dve", lambda e, i=i: e.memset(kmb[i][:], 0.0), writes=[("kmb", i)])

        def layer(l):
            src = x_d if l == 0 else x1_d
            dst = out_d if l == L - 1 else x1_d
            srcn = "xd" if l == 0 else "x1d"
            dstn = "outd" if l == L - 1 else "x1d"
            li = lambda_inits[l]

            if l == 0:
                P.add("sp", lambda e: e.dma_start(out=gbc[:], in_=ng_d[l:l + 1, :].to_broadcast([128, D])),
                      writes=["gbc"], dma=True)
            P.add("sp", lambda e: e.dma_start(out=gcol[:], in_=gcol_d[l, :, :]), writes=["gcol"], dma=True)
            P.add("sp", lambda e: e.dma_start(out=subbc[:], in_=sub_d[l:l + 1, :].to_broadcast([128, 128])),
                  writes=["subbc"], dma=True)
            P.add("sp", lambda e: e.dma_start(out=lamv[:], in_=lamv_d[l:l + 1, :].to_broadcast([128, 256])),
                  writes=["lamv"], dma=True)
            P.add("pool", lambda e: e.dma_start(out=wout[:].rearrange("p c n -> p (c n)"), in_=w_out_d[l, :, :]),
                  writes=["wout"], dma=True)
            P.add("dve", lambda e: e.tensor_scalar(out=qcol[:], in0=gcol[:], scalar1=0.125, scalar2=None, op0=ALU.mult),
                  reads=["gcol"], writes=["qcol"])
            P.add("dve", lambda e: e.tensor_scalar(out=subbc[:], in0=subbc[:], scalar1=float(1.0 - li), scalar2=None,
                                                   op0=ALU.mult), reads=["subbc"], writes=["subbc"])
            P.add("dve", lambda e: e.tensor_tensor(out=lamv[:, 0:64], in0=lamv[:, 0:64], in1=lamv[:, 64:128], op=ALU.mult),
                  reads=["lamv"], writes=["lamv"])
            P.add("dve", lambda e: e.tensor_tensor(out=lamv[:, 128:192], in0=lamv[:, 128:192], in1=lamv[:, 192:256],
                                                   op=ALU.mult), reads=["lamv"], writes=["lamv"])
            P.add("dve", lambda e: e.tensor_reduce(out=lamt[:, 0:1], in_=lamv[:, 0:64], axis=AX.X, op=ALU.add),
                  reads=["lamv"], writes=["lamt"])
            P.add("dve", lambda e: e.tensor_reduce(out=lamt[:, 1:2], in_=lamv[:, 128:192], axis=AX.X, op=ALU.add),
                  reads=["lamv", "lamt"], writes=["lamt"])
            P.add("act", lambda e: e.activation(out=lamt[:, 2:4], in_=lamt[:, 0:2], func=AF.Exp),
                  reads=["lamt"], writes=["lamt"])
            P.add("dve", lambda e: e.scalar_tensor_tensor(out=lamt[:, 4:5], in0=lamt[:, 3:4], scalar=float(-li),
                                                          in1=lamt[:, 2:3], op0=ALU.add, op1=ALU.subtract),
                  reads=["lamt"], writes=["neglam"])

            cols0 = {}

            def p0_load(t):
                b3 = t % 3
                P.add("sp", lambda e: e.dma_start(out=xt[b3][:], in_=src[t * 128:(t + 1) * 128, :]),
                      reads=[(srcn, t)], writes=[("xt", b3)], dma=True)

            def p0_A(t):
                b3 = t % 3
                c0 = scol()
                cols0[t] = c0
                jk = junk if t % 2 == 0 else junk2
                P.add("act", lambda e: e.activation(out=jk[:], in_=xt[b3][:], func=AF.Square,
                                                    accum_out=small[:, c0:c0 + 1]),
                      reads=[("xt", b3)], writes=[("junk", t % 2), ("small", c0)])

            def p0_B(t):
                b3 = t % 3
                b = t % 2
                c0 = cols0[t]
                P.add("act", lambda e: e.activation(out=small[:, c0:c0 + 1], in_=small[:, c0:c0 + 1], func=AF.Ln,
                                                    scale=1.0 / D, bias=EPS),
                      reads=[("small", c0)], writes=[("small", c0)])
                P.add("act", lambda e: e.activation(out=small[:, c0:c0 + 1], in_=small[:, c0:c0 + 1], func=AF.Exp,
                                                    scale=-0.5),
                      reads=[("small", c0)], writes=[("small", c0)])
                P.add("dve", lambda e: e.scalar_tensor_tensor(out=hb[b][:], in0=xt[b3][:], scalar=small[:, c0:c0 + 1],
                                                              in1=gbc[:], op0=ALU.mult, op1=ALU.mult),
                      reads=[("xt", b3), ("small", c0), "gbc"], writes=[("hb", b)])

            def p0_B2(t):
                b = t % 2
                for c in range(NCH):
                    P.add("pe", lambda e, c=c: e.transpose(tp[:, c * 128:(c + 1) * 128], hb[b][:, c * 128:(c + 1) * 128],
                                                           ident[:]),
                          reads=[("hb", b), "ident"], writes=["tp"])

            def p0_C(t):
                P.add("dve", lambda e: e.tensor_copy(out=hT[:, :, t * 128:(t + 1) * 128],
                                                     in_=tp[:, :].rearrange("p (c n) -> p c n", n=128)),
                      writes=["tp", ("hT", t // 4)])

            if l == 0:
                p0_load(0)
                if NT > 1:
                    p0_load(1)
                p0_A(0)
                for t in range(NT):
                    if t + 2 < NT:
                        p0_load(t + 2)
                    if t + 1 < NT:
                        p0_A(t + 1)
                    p0_B(t)
                    if t >= 1:
                        p0_C(t - 1)
                    p0_B2(t)
                p0_C(NT - 1)

            for u in range(8):
                unit(l, u, li)

            def p2_load(t):
                b = t % 2
                b3 = t % 3
                P.add("sp", lambda e: e.dma_start(out=mt[b][:], in_=mix_d[t * 128:(t + 1) * 128, :]),
                      reads=[("mixd", t // 4)], writes=[("mt", b)], dma=True)
                P.add("sp", lambda e: e.dma_start(out=xt[b3][:], in_=src[t * 128:(t + 1) * 128, :]),
                      reads=[(srcn, t)], writes=[("xt", b3)], dma=True)

            fuse = l < L - 1
            if fuse:
                P.add("sp", lambda e: e.dma_start(out=gbc[:], in_=ng_d[l + 1:l + 2, :].to_broadcast([128, D])),
                      writes=["gbc"], dma=True)
            tpn = ps[5][:, :].bitcast(BF16)
            fcols = {}

            def f_act(t):
                b3 = t % 3
                c0 = scol()
                fcols[t] = c0
                jk = junk if t % 2 == 0 else junk2
                P.add("act", lambda e: e.activation(out=jk[:], in_=xt[b3][:], func=AF.Square,
                                                    accum_out=small[:, c0:c0 + 1]),
                      reads=[("xt", b3)], writes=[("junk", t % 2), ("small", c0)])
                P.add("act", lambda e: e.activation(out=small[:, c0:c0 + 1], in_=small[:, c0:c0 + 1], func=AF.Ln,
                                                    scale=1.0 / D, bias=EPS),
                      reads=[("small", c0)], writes=[("small", c0)])
                P.add("act", lambda e: e.activation(out=small[:, c0:c0 + 1], in_=small[:, c0:c0 + 1], func=AF.Exp,
                                                    scale=-0.5),
                      reads=[("small", c0)], writes=[("small", c0)])

            def f_stt(t):
                b3 = t % 3
                b = t % 2
                c0 = fcols[t]
                P.add("dve", lambda e: e.scalar_tensor_tensor(out=hb[b][:], in0=xt[b3][:], scalar=small[:, c0:c0 + 1],
                                                              in1=gbc[:], op0=ALU.mult, op1=ALU.mult),
                      reads=[("xt", b3), ("small", c0), "gbc"], writes=[("hb", b)])

            def f_tr(t):
                b = t % 2
                for c in range(NCH):
                    P.add("pe", lambda e, c=c: e.transpose(tpn[:, c * 128:(c + 1) * 128], hb[b][:, c * 128:(c + 1) * 128],
                                                           ident[:]),
                          reads=[("hb", b), "ident"], writes=[psr(5)])
                P.add("dve", lambda e: e.tensor_copy(out=hT[:, :, t * 128:(t + 1) * 128],
                                                     in_=tpn.rearrange("p (c n) -> p c n", n=128)),
                      writes=[psr(5), ("hT", t // 4)])

            p2_load(0)
            for t in range(NT):
                b = t % 2
                b3 = t % 3
                if t + 1 < NT:
                    p2_load(t + 1)
                for c in range(NCH):
                    P.add("pe", lambda e, b=b, c=c: e.transpose(tp[:, c * 128:(c + 1) * 128], mt[b][:, c * 128:(c + 1) * 128],
                                                                ident[:]),
                          reads=[("mt", b), "ident"], writes=["tp"])
                P.add("act", lambda e, b=b: e.copy(out=mT[b][:, :, :], in_=tp[:, :].rearrange("p (c n) -> p c n", n=128)),
                      writes=["tp", ("mT", b)])
                if fuse and t >= 1:
                    f_act(t - 1)
                    f_stt(t - 1)
                banks = [6, 0] if t % 2 == 0 else [1, 2]
                for half in range(2):
                    bank = banks[half]
                    for c in range(NCH):
                        P.add("pe", lambda e, b=b, c=c, half=half, bank=bank: e.matmul(
                            ps[bank][:, :], lhsT=mT[b][:, c, :], rhs=wout[:, c, half * 512:(half + 1) * 512],
                            start=(c == 0), stop=(c == NCH - 1)),
                            reads=[("mT", b), "wout"], writes=[psr(bank)])
                if fuse and t >= 1:
                    f_tr(t - 1)
                for half in range(2):
                    bank = banks[half]
                    P.add("dve", lambda e, b3=b3, half=half, bank=bank: e.tensor_tensor(
                        out=xt[b3][:, half * 512:(half + 1) * 512], in0=xt[b3][:, half * 512:(half + 1) * 512],
                        in1=ps[bank][:, :], op=ALU.add),
                        reads=[("xt", b3)], writes=[psr(bank), ("xt", b3)])
                P.add("pool", lambda e, t=t, b3=b3: e.dma_start(out=dst[t * 128:(t + 1) * 128, :], in_=xt[b3][:]),
                      reads=[("xt", b3)], writes=[(dstn, t)], dma=True)
            if fuse:
                f_act(NT - 1)
                f_stt(NT - 1)
                f_tr(NT - 1)

        def unit(l, u, li):
            moba = u < 4
            dvw = 65 if moba else 129
            heads = [2 * u, 2 * u + 1] if moba else [8 + (u - 4), 8 + (u - 4)]
            P.add("pool", lambda e: e.dma_start(out=wbf[:].rearrange("p c n -> p (c n)"), in_=w_in_d[l, u, :, :]),
                  writes=["wbf"], dma=True)
            for i in range(2):
                P.add("pool", lambda e, i=i: e.dma_start(out=QT[i][80:84, :], in_=qaug_d[heads[i], :, :]),
                      writes=[("QTaug", i)], dma=True)
            if moba:
                P.add("dve", lambda e: e.memset(Vt[:, :, 64:65], 1.0), writes=["Vt"])
                P.add("dve", lambda e: e.memset(Vt[:, :, 129:130], 1.0), writes=["Vt"])
            else:
                P.add("dve", lambda e: e.memset(Vt[:, :, 128:129], 1.0), writes=["Vt"])
                if u == 4:
                    for i in range(2):
                        P.add("dve", lambda e, i=i: e.memset(QT[i][64:80, :], 0.0), writes=[("QTsel", i)])

            groups = [(which, g) for which in range(2) for g in range(NG)]
            PJB = [6, 1, 2, 3]
            SSB = [0, 4]

            def emit_proj(n):
                which, g = groups[n]
                pj = PJB[n % 4]
                for c in range(NCH):
                    P.add("pe", lambda e, c=c: e.matmul(
                        ps[pj][:, :], lhsT=wbf[:, c, which * 128:(which + 1) * 128],
                        rhs=hT[:, c, g * 512:(g + 1) * 512], start=(c == 0), stop=(c == NCH - 1)),
                        reads=["wbf", ("hT", g)], writes=[psr(pj)])

            def emit_chainA(n):
                pj = PJB[n % 4]
                b = n % 2
                ssb = SSB[n % 2]
                P.add("act", lambda e: e.activation(out=sq[b][:], in_=ps[pj][:, :], func=AF.Square),
                      writes=[psr(pj), ("sq", b)])
                P.add("pe", lambda e: e.matmul(ps[ssb][:, :], lhsT=bones[:], rhs=sq[b][:], start=True, stop=True),
                      reads=["bones", ("sq", b)], writes=[psr(ssb)])

            def emit_chainB(n):
                which, g = groups[n]
                pj = PJB[n % 4]
                b = n % 2
                ssb = SSB[n % 2]
                T = QT if which == 0 else KT
                tname = "QT" if which == 0 else "KT"
                colt = qcol if which == 0 else gcol
                cidx = (0 if moba else 2) + which
                P.add("act", lambda e: e.activation(out=rs[b][:], in_=ps[ssb][:, :], func=AF.Ln, scale=1.0 / 64, bias=EPS),
                      writes=[psr(ssb), ("rs", b)])
                P.add("act", lambda e: e.activation(out=rs[b][:], in_=rs[b][:], func=AF.Exp, scale=-0.5),
                      reads=[("rs", b)], writes=[("rs", b)])
                for i in range(2):
                    if moba and which == 1:
                        for hh in range(2):
                            P.add("dve", lambda e, i=i, hh=hh: e.scalar_tensor_tensor(
                                out=T[i][0:64, g * 512 + hh * 256:g * 512 + (hh + 1) * 256],
                                in0=ps[pj][i * 64:(i + 1) * 64, hh * 256:(hh + 1) * 256],
                                scalar=colt[i * 64:(i + 1) * 64, cidx:cidx + 1],
                                in1=rs[b][i * 64:(i + 1) * 64, hh * 256:(hh + 1) * 256],
                                op0=ALU.mult, op1=ALU.mult, accum_out=km[i][:, 2 * g + hh:2 * g + hh + 1]),
                                reads=[("rs", b), "qcol", "gcol"], writes=[psr(pj), (tname, i, g), ("km", i)])
                    else:
                        P.add("dve", lambda e, i=i: e.scalar_tensor_tensor(
                            out=T[i][0:64, g * 512:(g + 1) * 512], in0=ps[pj][i * 64:(i + 1) * 64, :],
                            scalar=colt[i * 64:(i + 1) * 64, cidx:cidx + 1], in1=rs[b][i * 64:(i + 1) * 64, :],
                            op0=ALU.mult, op1=ALU.mult),
                            reads=[("rs", b), "qcol", "gcol"], writes=[psr(pj), (tname, i, g)])

            NGR = len(groups)
            emit_proj(0)
            if NGR > 1:
                emit_proj(1)
            emit_chainA(0)
            for n in range(NGR):
                if n + 2 < NGR:
                    emit_proj(n + 2)
                if n + 1 < NGR:
                    emit_chainA(n + 1)
                emit_chainB(n)

            side = []

            def sel_stage1(i):
                P.add("dve", lambda e: e.tensor_scalar(out=kmb[i][:, 0:NB], in0=km[i][:, 0:NB], scalar1=1.0 / 256,
                                                       scalar2=None, op0=ALU.mult),
                      reads=[("km", i)], writes=[("kmb", i)])

            def sel_gate(i):
                gbank = 6 if i == 0 else 0
                for t in range(NT):
                    P.add("pe", lambda e, t=t: e.matmul(ps[gbank][:, t * 16:(t + 1) * 16],
                                                        lhsT=QT[i][0:64, t * 128:(t + 1) * 128], rhs=kmb[i][:, :],
                                                        start=True, stop=True),
                          reads=[("QT", i, t // 4), ("kmb", i)], writes=[psr(gbank)])
                P.add("dve", lambda e: e.tensor_tensor(out=gm[i][:].rearrange("p t n -> p (t n)"),
                                                       in0=ps[gbank][:, 0:NT * 16], in1=gbt[:], op=ALU.add),
                      reads=["gbt"], writes=[psr(gbank), ("gm", i)])

            def sel_chain(i):
                th = []
                for t in range(NT):
                    th.append(lambda t=t: P.add("dve", lambda e: e.max(out=top8[:, t, :], in_=gm[i][:, t, :]),
                                                reads=[("gm", i)], writes=["top8"]))
                th.append(lambda: P.add("dve", lambda e: e.tensor_scalar(out=thr[:], in0=top8[:, :, 3], scalar1=-1e29,
                                                                         scalar2=None, op0=ALU.max),
                                        reads=["top8"], writes=["thr"]))
                th.append(lambda: P.add("dve", lambda e: e.tensor_tensor(
                    out=sel[:], in0=gm[i][:], in1=thr[:].unsqueeze(2).to_broadcast([128, NT, 16]), op=ALU.is_ge),
                    reads=[("gm", i), "thr"], writes=["sel"]))
                th.append(lambda: P.add("dve", lambda e: e.tensor_scalar(out=selb[i][:], in0=sel[:], scalar1=-NEGM,
                                                                         scalar2=NEGM, op0=ALU.mult, op1=ALU.add),
                                        reads=["sel"], writes=[("selb", i)]))
                return th

            def sel_final_thunks(i):
                th = []
                for k, t0 in enumerate(range(0, NT, 8)):
                    def f(k=k, t0=t0):
                        w = cnt["tpb"] % 3
                        cnt["tpb"] += 1
                        if w == 0:
                            buf = tp[0:16, :]
                            res = "tp"
                        else:
                            buf = ps[3 + w][0:16, :].bitcast(BF16)
                            res = psr(3 + w)
                        for t in range(t0, t0 + 8):
                            P.add("pe", lambda e, t=t: e.transpose(buf[:, (t - t0) * 128:(t - t0 + 1) * 128],
                                                                   selb[i][:, t, :], ident[:]),
                                  reads=[("selb", i), "ident"], writes=[res])
                        if k % 2 == 0:
                            P.add("act", lambda e: e.copy(out=QT[i][64:80, t0 * 128:(t0 + 8) * 128], in_=buf),
                                  writes=[res, ("QTsel", i)])
                        else:
                            P.add("dve", lambda e: e.tensor_copy(out=QT[i][64:80, t0 * 128:(t0 + 8) * 128], in_=buf),
                                  writes=[res, ("QTsel", i)])
                    th.append(f)
                return th

            if moba:
                for i in range(2):
                    sel_stage1(i)

            for t in range(NT):
                bank = [2, 3, 1][t % 3]
                b = t % 2
                if moba and t == min(4, NT - 1):
                    for i in range(2):
                        sel_gate(i)
                        side.extend(sel_chain(i))
                        side.extend(sel_final_thunks(i))
                for c in range(NCH):
                    P.add("pe", lambda e, c=c, t=t, bank=bank: e.matmul(
                        ps[bank][:, 0:256], lhsT=hT[:, c, t * 128:(t + 1) * 128], rhs=wbf[:, c, 256:512],
                        start=(c == 0), stop=(c == NCH - 1)),
                        reads=["wbf", ("hT", t // 4)], writes=[psr(bank)])
                if moba:
                    P.add("dve", lambda e, t=t, bank=bank: e.tensor_copy(
                        out=Vt[:, t, :].rearrange("p (i c) -> p i c", c=65)[:, :, 0:64],
                        in_=ps[bank][:, 0:128].rearrange("p (i c) -> p i c", c=64)),
                        writes=[psr(bank), "Vt"])
                    P.add("act", lambda e, t=t, bank=bank: e.activation(out=Gt[:, t, :], in_=ps[bank][:, 128:256], func=AF.Silu),
                          writes=[psr(bank), "Gt"])
                else:
                    P.add("dve", lambda e, t=t, bank=bank: e.tensor_copy(out=Vt[:, t, 0:128], in_=ps[bank][:, 0:128]),
                          writes=[psr(bank), "Vt"])
                    P.add("act", lambda e, b=b, bank=bank: e.activation(out=gtmp[b][:], in_=ps[bank][:, 128:256], func=AF.Silu),
                          writes=[psr(bank), ("gtmp", b)])
                    P.add("dve", lambda e, t=t, b=b: e.tensor_tensor(out=Gt[:, t, :], in0=gtmp[b][:], in1=subbc[:], op=ALU.mult),
                          reads=[("gtmp", b), "subbc"], writes=["Gt"])
                for _ in range(4):
                    if side:
                        side.pop(0)()
            while side:
                side.pop(0)()

            slopes_all = MOBA_SLOPES + DIFF_SLOPES
            iters = []
            for g in range(NG):
                for i in range(2):
                    sl = slopes_all[heads[i]]
                    glist = []
                    for kt in range(0, 4 * (g + 1)):
                        j = kt - 4 * g
                        s0 = max(0, j)
                        s1 = s0
                        for sq_ in range(s0, 4):
                            tq = 4 * g + sq_
                            dmin = 0 if tq == kt else (tq * 128 - (kt * 128 + 127))
                            if sl * dmin <= ALIBI_FAR:
                                s1 = sq_ + 1
                        if s1 > s0:
                            glist.append(dict(g=g, i=i, kt=kt, s0=s0, s1=s1))
                    started = set()
                    for it in glist:
                        it["startbank"] = {}
                        for sq_ in range(it["s0"], it["s1"]):
                            bk = sq_ // 2
                            if bk not in started:
                                started.add(bk)
                                it["startbank"][sq_] = True
                    glist[-1]["last"] = True
                    assert glist[-1]["kt"] == 4 * g + 3 and started == {0, 1}
                    iters.extend(glist)
            SBAP = [ps[0][:, :], ps[1][:, :], ps[6][:, :], tp[:, :].bitcast(F32)]
            SBRES = [psr(0), psr(1), psr(6), "tp"]
            PIPE = 3

            def emit_S(n):
                it = iters[n]
                g, i, kt, s0, s1 = it["g"], it["i"], it["kt"], it["s0"], it["s1"]
                j = kt - 4 * g
                sbk = n % 4
                P.add("pe", lambda e: e.matmul(
                    SBAP[sbk][:, s0 * 128:s1 * 128], lhsT=KT[i][0:KR, kt * 128:(kt + 1) * 128],
                    rhs=QT[i][0:KR, g * 512 + s0 * 128:g * 512 + s1 * 128], start=True, stop=(j < 0)),
                    reads=[("KT", i, kt // 4), ("KTaug", i), ("QT", i, g), ("QTaug", i), ("QTsel", i)],
                    writes=[SBRES[sbk]])
                if j >= 0:
                    P.add("pe", lambda e: e.matmul(SBAP[sbk][:, j * 128:(j + 1) * 128], lhsT=ident[:],
                                                   rhs=cmask[:], start=False, stop=True),
                          reads=["ident", "cmask"], writes=[SBRES[sbk]])

            def emit_rest(n):
                it = iters[n]
                g, i, kt, s0, s1 = it["g"], it["i"], it["kt"], it["s0"], it["s1"]
                sbk = n % 4
                slot = n % NPT
                oset = (g * 2 + i) % 2
                ob = [2 + 2 * oset, 3 + 2 * oset]
                P.add("act", lambda e: e.activation(
                    out=PT[slot][:, s0 * 128:s1 * 128], in_=SBAP[sbk][:, s0 * 128:s1 * 128], func=AF.Exp),
                    writes=[SBRES[sbk], ("PT", slot)])
                for s in range(s0, s1):
                    bank = ob[s // 2]
                    col = (s % 2) * dvw
                    vlo = i * 65 if moba else 0
                    P.add("pe", lambda e, s=s, bank=bank, col=col, vlo=vlo: e.matmul(
                        ps[bank][:, col:col + dvw], lhsT=PT[slot][:, s * 128:(s + 1) * 128],
                        rhs=Vt[:, kt, vlo:vlo + dvw], start=bool(it["startbank"].get(s, False)), stop=(kt == 4 * g + s),
                        skip_group_check=True),
                        reads=[("PT", slot), "Vt"], writes=[psr(bank)])
                if it.get("last"):
                    pend.append((n + EPI_DELAY, (u, g, i, ob, dvw, moba)))

            NI = len(iters)
            pend = []
            EPI_DELAY = 3
            for n in range(min(PIPE, NI)):
                emit_S(n)
            for n in range(NI):
                if n + PIPE < NI:
                    emit_S(n + PIPE)
                emit_rest(n)
                while pend and pend[0][0] <= n:
                    epilogue(*pend.pop(0)[1])
            while pend:
                epilogue(*pend.pop(0)[1])

        def epilogue(u, g, i, ob, dvw, moba):
            mb = g % 2
            if (not moba) and i == 1:
                k4 = cnt["s4"] % 16
                cnt["s4"] += 1
                c4 = 4 * k4
            for s in range(4):
                t = 4 * g + s
                bank = ob[s // 2]
                col = (s % 2) * dvw
                c0 = scol()
                if moba:
                    P.add("dve", lambda e, bank=bank, col=col, c0=c0: e.reciprocal(out=small[:, c0:c0 + 1],
                                                                                   in_=ps[bank][:, col + 64:col + 65]),
                          writes=[psr(bank), ("small", c0)])
                    P.add("dve", lambda e, bank=bank, col=col, c0=c0, s=s, t=t, mb=mb, i=i: e.scalar_tensor_tensor(
                        out=mo[mb][:, s, i * 64:(i + 1) * 64], in0=ps[bank][:, col:col + 64], scalar=small[:, c0:c0 + 1],
                        in1=Gt[:, t, i * 64:(i + 1) * 64], op0=ALU.mult, op1=ALU.mult),
                        reads=[("small", c0), "Gt"], writes=[psr(bank), ("mo", mb)])
                elif i == 0:
                    P.add("dve", lambda e, bank=bank, col=col, c0=c0: e.reciprocal(out=small[:, c0:c0 + 1],
                                                                                   in_=ps[bank][:, col + 128:col + 129]),
                          writes=[psr(bank), ("small", c0)])
                    P.add("dve", lambda e, bank=bank, col=col, c0=c0, s=s: e.tensor_scalar(
                        out=abuf[:, s, :], in0=ps[bank][:, col:col + 128], scalar1=small[:, c0:c0 + 1], scalar2=None,
                        op0=ALU.mult),
                        reads=[("small", c0)], writes=[psr(bank), ("abuf", s)])
                else:
                    P.add("dve", lambda e, bank=bank, col=col, c0=c0: e.reciprocal(out=small[:, c0:c0 + 1],
                                                                                   in_=ps[bank][:, col + 128:col + 129]),
                          writes=[psr(bank), ("small", c0)])
                    P.add("dve", lambda e, c0=c0: e.tensor_tensor(out=small[:, c0:c0 + 1], in0=small[:, c0:c0 + 1],
                                                                 in1=lamt[:, 4:5], op=ALU.mult),
                          reads=[("small", c0), "neglam"], writes=[("small", c0)])
                    P.add("dve", lambda e, bank=bank, col=col, c0=c0, s=s: e.scalar_tensor_tensor(
                        out=dbuf[:, s, :], in0=ps[bank][:, col:col + 128], scalar=small[:, c0:c0 + 1], in1=abuf[:, s, :],
                        op0=ALU.mult, op1=ALU.add),
                        reads=[("small", c0), ("abuf", s)], writes=[psr(bank), ("dbuf", s)])
                    P.add("dve", lambda e, s=s, c4=c4: e.scalar_tensor_tensor(
                        out=junkf[:], in0=dbuf[:, s, :], scalar=1.0, in1=dbuf[:, s, :], op0=ALU.mult, op1=ALU.mult,
                        accum_out=small4[:, c4 + s:c4 + s + 1]),
                        reads=[("dbuf", s)], writes=["junkf", ("small4", c4)])
            if (not moba) and i == 1:
                P.add("act", lambda e, c4=c4: e.activation(out=small4[:, c4:c4 + 4], in_=small4[:, c4:c4 + 4], func=AF.Ln,
                                                          scale=1.0 / 128, bias=EPS),
                      reads=[("small4", c4)], writes=[("small4", c4)])
                P.add("act", lambda e, c4=c4: e.activation(out=small4[:, c4:c4 + 4], in_=small4[:, c4:c4 + 4], func=AF.Exp,
                                                          scale=-0.5),
                      reads=[("small4", c4)], writes=[("small4", c4)])
                for s in range(4):
                    t = 4 * g + s
                    P.add("dve", lambda e, s=s, t=t, mb=mb, c4=c4: e.scalar_tensor_tensor(
                        out=mo[mb][:, s, :], in0=dbuf[:, s, :], scalar=small4[:, c4 + s:c4 + s + 1], in1=Gt[:, t, :],
                        op0=ALU.mult, op1=ALU.mult),
                        reads=[("dbuf", s), ("small4", c4), "Gt"], writes=[("mo", mb)])
            if i == 1:
                P.add("sp", lambda e, g=g, mb=mb, u=u: e.dma_start(
                    out=mix_d[g * 512:(g + 1) * 512, u * 128:(u + 1) * 128].rearrange("(s p) c -> p s c", p=128),
                    in_=mo[mb][:, :, :]),
                    reads=[("mo", mb)], writes=[("mixd", g)], dma=True)

        for l in range(L):
            layer(l)
        P.add("sp", None, reads=[("outd", t) for t in range(NT)])
        print('sbuf bytes remaining', nc.sbuf_bytes_remaining)
        P.emit(nc, block, sems, dsems)
    return nc


def _host_layout(S, inputs):
    w_in = np.asarray(inputs["w_in"], dtype=np.float32)
    w_out = np.asarray(inputs["w_out"], dtype=np.float32)
    L = w_in.shape[0]
    NT = S // 128
    w_in_r = np.empty((L, 8, 128, NCH, 512), np.float32)
    for l in range(L):
        wl = w_in[l].reshape(NCH, 128, 4096)
        for u in range(8):
            base = 0 if u < 4 else 2048
            idx = u % 4
            for k in range(4):
                c0 = base + k * 512 + idx * 128
                w_in_r[l, u, :, :, k * 128:(k + 1) * 128] = wl[:, :, c0:c0 + 128].transpose(1, 0, 2)
    w_in_r = np.ascontiguousarray(w_in_r.reshape(L, 8, 128, NCH * 512))
    w_out_r = np.ascontiguousarray(w_out.reshape(L, NCH, 128, D).transpose(0, 2, 1, 3).reshape(L, 128, NCH * D))
    gcol = np.stack([np.stack([np.tile(np.asarray(inputs[k], np.float32)[l], 2) for k in
                               ("moba_q_norm", "moba_k_norm", "diff_q_norm", "diff_k_norm")], axis=1)
                     for l in range(L)], axis=0).astype(np.float32)
    lamv = np.concatenate([np.asarray(inputs[k], np.float32) for k in
                           ("lambda_q1", "lambda_k1", "lambda_q2", "lambda_k2")], axis=1)
    pos = np.arange(S)
    ident = np.eye(128, dtype=np.float32)
    kk = np.arange(128)[:, None]
    qq = np.arange(128)[None, :]
    cmask = np.where(kk <= qq, 0.0, NEGM).astype(np.float32)
    bones = np.kron(np.eye(2, dtype=np.float32), np.ones((64, 64), np.float32))
    kaug = np.zeros((20, S), np.float32)
    kaug[0:16] = (pos[None, :] // 256 == np.arange(16)[:, None]).astype(np.float32)
    kaug[16] = pos // 128
    kaug[17] = pos % 128
    kaug[18] = 1.0
    kaug[19] = 1.0
    qaug = np.zeros((12, 4, S), np.float32)
    for h, sl in enumerate(MOBA_SLOPES + DIFF_SLOPES):
        qaug[h, 0] = sl * 128.0
        qaug[h, 1] = sl
        qaug[h, 2] = -sl * 128.0 * (pos // 128)
        qaug[h, 3] = -sl * (pos % 128)
    gb = np.zeros((NT, 16), np.float32)
    for t in range(NT):
        own = t // 2
        gb[t, own] = 1e30
        gb[t, own + 1:] = -1e30
    common = {
        "w_in_r": w_in_r, "w_out_r": w_out_r,
        "norm_g": np.ascontiguousarray(np.asarray(inputs["norm_g"], np.float32)),
        "gcol": np.ascontiguousarray(gcol),
        "subln": np.ascontiguousarray(np.asarray(inputs["diff_subln"], np.float32)),
        "lamv": np.ascontiguousarray(lamv),
        "c_ident": ident, "c_cmask": cmask, "c_bones": bones, "c_kaug": kaug, "c_qaug": qaug,
        "c_gb": gb.reshape(1, NT * 16),
    }
    return L, common


def kernel(x, norm_g, w_in, moba_q_norm, moba_k_norm, diff_q_norm, diff_k_norm,
           lambda_q1, lambda_k1, lambda_q2, lambda_k2, diff_subln, w_out):
    inputs = dict(x=x, norm_g=norm_g, w_in=w_in, moba_q_norm=moba_q_norm, moba_k_norm=moba_k_norm,
                  diff_q_norm=diff_q_norm, diff_k_norm=diff_k_norm, lambda_q1=lambda_q1, lambda_k1=lambda_k1,
                  lambda_q2=lambda_q2, lambda_k2=lambda_k2, diff_subln=diff_subln, w_out=w_out)
    x = np.asarray(x, dtype=np.float32)
    B, S, _ = x.shape
    L, common = _host_layout(S, inputs)
    lambda_inits = [0.8 - 0.6 * math.exp(-0.3 * l) for l in range(L)]
    nc = build(S, L, lambda_inits)
    in_maps = [dict(common, x=np.ascontiguousarray(x[b])) for b in range(B)]
    res = run_bass_kernel_spmd(nc, in_maps, core_ids=list(range(B)))
    return np.stack([np.asarray(r["out"], dtype=np.float32) for r in res.results], axis=0)
```
